# Optimizing a Trainium2 kernel written in Bass

```python
import math
import jax
import jax.numpy as jnp
from jax import lax
import numpy as np

D_MODEL = 2048
BATCH = 4
SEQ = 4096
DEPTH = 2

MEM_LEN = 256
MAX_POS_OFFSET = 4096

S5_WIDTH = D_MODEL // 4
S5_CH_PER_GROUP = 16
S5_GROUPS = S5_WIDTH // S5_CH_PER_GROUP
S5_STATE = 64
S5_LAMBDA_RE_MAX = -1e-4

MLA_HEADS = D_MODEL // 256
MLA_NOPE = 128
MLA_ROPE = 64
MLA_V = 128
MLA_Q_RANK = D_MODEL // 4
MLA_KV_RANK = D_MODEL // 4
MLA_WIDTH = MLA_HEADS * MLA_V
ROPE_THETA = 10000.0
Q_BLOCK = 128

GDN_HEADS = D_MODEL // 512
GDN_DK = 128
GDN_DV = 128
GDN_WIDTH = GDN_HEADS * GDN_DV
GDN_QKV = 2 * GDN_HEADS * GDN_DK + GDN_WIDTH
GDN_CONV = 4
GDN_CHUNK = 64

XA_HEADS = 4
XA_DH = 128
XA_WIDTH = XA_HEADS * XA_DH

MOE_GROUPS = 4
MOE_PER_GROUP = 8
MOE_EXPERTS = MOE_GROUPS * MOE_PER_GROUP
MOE_TOPK = 2
MOE_FF = D_MODEL // 4
MOE_ROW_BLOCK = 128

DN_ALPHA = (2 * DEPTH) ** 0.25
DN_BETA = (8 * DEPTH) ** -0.25

IN_SIZES = (S5_WIDTH, MLA_Q_RANK, MLA_KV_RANK, MLA_ROPE,
            GDN_HEADS * GDN_DK, GDN_HEADS * GDN_DK, GDN_WIDTH, GDN_WIDTH,
            GDN_HEADS, GDN_HEADS)
IN_COLS = sum(IN_SIZES)
MIX_WIDTH = S5_WIDTH + MLA_WIDTH + GDN_WIDTH

kernel_name = 'hybrid_s5_mla_gdn_hmoe_deepnorm'


def _rms(x, g, eps=1e-6):
    xf = x.astype(jnp.float32)
    y = xf * lax.rsqrt(jnp.mean(xf * xf, axis=-1, keepdims=True) + eps)
    return (y * g.astype(jnp.float32)).astype(x.dtype)


def _layernorm(x, g, b, eps=1e-5):
    xf = x.astype(jnp.float32)
    mu = jnp.mean(xf, axis=-1, keepdims=True)
    var = jnp.mean(jnp.square(xf - mu), axis=-1, keepdims=True)
    y = (xf - mu) * lax.rsqrt(var + eps)
    return (y * g.astype(jnp.float32) + b.astype(jnp.float32)).astype(x.dtype)


def _l2norm(x, eps=1e-6):
    xf = x.astype(jnp.float32)
    return xf * lax.rsqrt(jnp.sum(xf * xf, axis=-1, keepdims=True) + eps)


def _rope(x, positions):
    rdim = x.shape[-1]
    half = rdim // 2
    inv_freq = 1.0 / (ROPE_THETA ** (jnp.arange(half, dtype=jnp.float32) * (2.0 / rdim)))
    ang = positions.astype(jnp.float32)[..., None] * inv_freq
    ang = ang.reshape(ang.shape[:2] + (1,) * (x.ndim - 3) + (half,))
    cos, sin = jnp.cos(ang), jnp.sin(ang)
    xf = x.astype(jnp.float32)
    x1, x2 = xf[..., :half], xf[..., half:]
    return jnp.concatenate([x1 * cos - x2 * sin, x2 * cos + x1 * sin], axis=-1).astype(x.dtype)


def _split_in_proj(proj):
    idx = np.cumsum(np.array(IN_SIZES))[:-1].tolist()
    return jnp.split(proj, idx, axis=-1)


def _cmul(ar, ai, br, bi):
    return ar * br - ai * bi, ar * bi + ai * br


def _s5_scan(u, lam_re, lam_im, log_step, b_re, b_im, c_re, c_im, d_skip):
    f32 = jnp.float32
    lr = jnp.minimum(lam_re.astype(f32), S5_LAMBDA_RE_MAX)
    li = lam_im.astype(f32)
    dt = jnp.exp(log_step.astype(f32))[:, None]
    mag = jnp.exp(lr * dt)
    ab_re, ab_im = mag * jnp.cos(li * dt), mag * jnp.sin(li * dt)
    den = lr * lr + li * li
    nr, ni = ab_re - 1.0, ab_im
    fr = (nr * lr + ni * li) / den
    fi = (ni * lr - nr * li) / den
    br, bi = b_re.astype(f32), b_im.astype(f32)
    bb_re = fr[..., None] * br - fi[..., None] * bi
    bb_im = fr[..., None] * bi + fi[..., None] * br
    uf = u.astype(f32)
    bu_re = jnp.einsum('bsgh,gph->bsgp', uf, bb_re)
    bu_im = jnp.einsum('bsgh,gph->bsgp', uf, bb_im)
    a_re = jnp.broadcast_to(ab_re, bu_re.shape)
    a_im = jnp.broadcast_to(ab_im, bu_im.shape)

    def combine(e1, e2):
        a1r, a1i, b1r, b1i = e1
        a2r, a2i, b2r, b2i = e2
        ar, ai = _cmul(a2r, a2i, a1r, a1i)
        tr, ti = _cmul(a2r, a2i, b1r, b1i)
        return ar, ai, tr + b2r, ti + b2i

    _, _, xr, xi = lax.associative_scan(combine, (a_re, a_im, bu_re, bu_im), axis=1)
    y = (jnp.einsum('bsgp,ghp->bsgh', xr, c_re.astype(f32))
         - jnp.einsum('bsgp,ghp->bsgh', xi, c_im.astype(f32)))
    y = y + d_skip.astype(f32) * uf
    return y.astype(u.dtype)


def _s5_group(u, lam_re, lam_im, log_step, b_re, b_im, c_re, c_im, d_skip, w_glu, b_glu, g_out):
    bsz, seq, _ = u.shape
    y = _s5_scan(u.reshape(bsz, seq, S5_GROUPS, S5_CH_PER_GROUP),
                 lam_re, lam_im, log_step, b_re, b_im, c_re, c_im, d_skip)
    y = jax.nn.gelu(y.reshape(bsz, seq, S5_WIDTH))
    y = y * jax.nn.sigmoid(y @ w_glu + b_glu)
    return _rms(y, g_out)


def _causal_attention(q, k, v, scale):
    bsz, seq, heads, dk = q.shape
    nb = seq // Q_BLOCK
    qb = jnp.moveaxis(q.reshape(bsz, nb, Q_BLOCK, heads, dk), 1, 0)
    kpos = jnp.arange(seq)

    def one_block(args):
        qi, i = args
        s = jnp.einsum('bqhd,bkhd->bhqk', qi, k).astype(jnp.float32) * scale
        qpos = i * Q_BLOCK + jnp.arange(Q_BLOCK)
        s = jnp.where(kpos[None, :] <= qpos[:, None], s, -jnp.inf)
        p = jax.nn.softmax(s, axis=-1).astype(v.dtype)
        return jnp.einsum('bhqk,bkhd->bqhd', p, v)

    out = lax.map(one_block, (qb, jnp.arange(nb)))
    return jnp.moveaxis(out, 0, 1).reshape(bsz, seq, heads, v.shape[-1])


def _mla_group(cq, ckv, kr, positions, q_norm, w_uq, kv_norm, w_ukv, g_out):
    bsz, seq, _ = cq.shape
    q = (_rms(cq, q_norm) @ w_uq).reshape(bsz, seq, MLA_HEADS, MLA_NOPE + MLA_ROPE)
    q = jnp.concatenate([q[..., :MLA_NOPE], _rope(q[..., MLA_NOPE:], positions)], axis=-1)
    kv = (_rms(ckv, kv_norm) @ w_ukv).reshape(bsz, seq, MLA_HEADS, MLA_NOPE + MLA_V)
    k_pe = jnp.broadcast_to(_rope(kr, positions)[:, :, None, :], (bsz, seq, MLA_HEADS, MLA_ROPE))
    k = jnp.concatenate([kv[..., :MLA_NOPE], k_pe], axis=-1)
    v = kv[..., MLA_NOPE:]
    o = _causal_attention(q, k, v, (MLA_NOPE + MLA_ROPE) ** -0.5)
    return _rms(o.reshape(bsz, seq, MLA_WIDTH), g_out)


def _causal_dwconv(x, w):
    taps, ch = w.shape
    return lax.conv_general_dilated(x, w.astype(x.dtype)[:, None, :], window_strides=(1,),
                                    padding=((taps - 1, 0),),
                                    dimension_numbers=('NWC', 'WIO', 'NWC'),
                                    feature_group_count=ch)


def _gated_delta_chunked(q, k, v, g, beta):
    f32 = jnp.float32
    bsz, seq, heads, dk = q.shape
    dv = v.shape[-1]
    c = GDN_CHUNK
    n = seq // c

    def to_chunks(t):
        t = t.astype(f32).reshape((bsz, n, c, heads) + t.shape[3:])
        return jnp.moveaxis(t, 3, 1)

    q = to_chunks(q) * (dk ** -0.5)
    k, v, g, beta = to_chunks(k), to_chunks(v), to_chunks(g), to_chunks(beta)
    gc = jnp.cumsum(g, axis=-1)
    tri = jnp.tril(jnp.ones((c, c), dtype=bool))
    strict = jnp.tril(jnp.ones((c, c), dtype=bool), -1)
    decay = jnp.where(tri, jnp.exp(jnp.where(tri, gc[..., :, None] - gc[..., None, :], 0.0)), 0.0)
    kb = k * beta[..., None]
    vb = v * beta[..., None]
    lmat = jnp.where(strict, jnp.einsum('bhncd,bhnjd->bhncj', kb, k) * decay, 0.0)
    eye = jnp.eye(c, dtype=f32)
    tmat = lax.linalg.triangular_solve(eye + lmat, jnp.broadcast_to(eye, lmat.shape),
                                       left_side=True, lower=True, unit_diagonal=True)
    u = tmat @ vb
    w = tmat @ (kb * jnp.exp(gc)[..., None])
    a_intra = jnp.where(tri, jnp.einsum('bhncd,bhnjd->bhncj', q, k) * decay, 0.0)
    g_last = gc[..., -1]
    k_end = k * jnp.exp(g_last[..., None] - gc)[..., None]
    q_dec = q * jnp.exp(gc)[..., None]
    xs = tuple(jnp.moveaxis(t, 2, 0) for t in (q_dec, w, u, a_intra, k_end, g_last))

    def step(state, inp):
        qd, wi, ui, ai, ke, gl = inp
        v_new = ui - jnp.einsum('bhcd,bhde->bhce', wi, state)
        o = jnp.einsum('bhcd,bhde->bhce', qd, state) + jnp.einsum('bhcj,bhje->bhce', ai, v_new)
        state = state * jnp.exp(gl)[..., None, None] + jnp.einsum('bhcd,bhce->bhde', ke, v_new)
        return state, o

    s0 = jnp.zeros((bsz, heads, dk, dv), f32)
    _, o = lax.scan(step, s0, xs)
    o = jnp.moveaxis(o, 0, 2)
    return jnp.moveaxis(o, 1, 3).reshape(bsz, seq, heads, dv)


def _gdn_group(gq, gk, gv, gz, ga, gb, w_conv, a_log, dt_bias, g_out):
    f32 = jnp.float32
    bsz, seq, _ = gq.shape
    nqk = GDN_HEADS * GDN_DK
    qkv = jax.nn.silu(_causal_dwconv(jnp.concatenate([gq, gk, gv], axis=-1), w_conv))
    q = _l2norm(qkv[..., :nqk].reshape(bsz, seq, GDN_HEADS, GDN_DK))
    k = _l2norm(qkv[..., nqk:2 * nqk].reshape(bsz, seq, GDN_HEADS, GDN_DK))
    v = qkv[..., 2 * nqk:].reshape(bsz, seq, GDN_HEADS, GDN_DV)
    beta = jax.nn.sigmoid(gb.astype(f32))
    g = -jnp.exp(a_log.astype(f32)) * jax.nn.softplus(ga.astype(f32) + dt_bias.astype(f32))
    o = _gated_delta_chunked(q, k, v, g, beta)
    o = _rms(o, g_out) * jax.nn.silu(gz.astype(f32).reshape(bsz, seq, GDN_HEADS, GDN_DV))
    return o.reshape(bsz, seq, GDN_WIDTH).astype(gq.dtype)


def _memory_xattn(x, mem, w_q, w_k, w_v, w_o):
    bsz, seq, _ = x.shape
    mlen = mem.shape[1]
    q = (x @ w_q).reshape(bsz, seq, XA_HEADS, XA_DH)
    k = (mem @ w_k).reshape(bsz, mlen, XA_HEADS, XA_DH)
    v = (mem @ w_v).reshape(bsz, mlen, XA_HEADS, XA_DH)
    s = jnp.einsum('bshd,bmhd->bhsm', q, k).astype(jnp.float32) * (XA_DH ** -0.5)
    p = jax.nn.softmax(s, axis=-1).astype(v.dtype)
    o = jnp.einsum('bhsm,bmhd->bshd', p, v).reshape(bsz, seq, XA_WIDTH)
    return o @ w_o


def _hier_moe(x, w_group, b_group, w_expert, b_expert, w_gate_up, w_down):
    f32 = jnp.float32
    bsz, seq, dm = x.shape
    ntok = bsz * seq
    xt = x.reshape(ntok, dm)
    pg = jax.nn.softmax((xt @ w_group + b_group).astype(f32), axis=-1)
    pg_top, gsel = lax.top_k(pg, 1)
    le = (xt @ w_expert + b_expert).astype(f32).reshape(ntok, MOE_GROUPS, MOE_PER_GROUP)
    idx = jnp.broadcast_to(gsel[:, :, None], (ntok, 1, MOE_PER_GROUP))
    le = jnp.take_along_axis(le, idx, axis=1)[:, 0]
    pe_top, esel = lax.top_k(jax.nn.softmax(le, axis=-1), MOE_TOPK)
    gate = pg_top * pe_top / jnp.sum(pe_top, axis=-1, keepdims=True)
    eid = gsel * MOE_PER_GROUP + esel

    m = ntok * MOE_TOPK
    flat_e = eid.reshape(m)
    flat_tok = jnp.repeat(jnp.arange(ntok, dtype=jnp.int32), MOE_TOPK)
    flat_g = gate.reshape(m)
    order = jnp.argsort(flat_e)
    se = flat_e[order]
    counts = jnp.zeros((MOE_EXPERTS,), jnp.int32).at[flat_e].add(1)
    starts = jnp.cumsum(counts) - counts
    pcounts = (counts + MOE_ROW_BLOCK - 1) // MOE_ROW_BLOCK * MOE_ROW_BLOCK
    pends = jnp.cumsum(pcounts)
    pstarts = pends - pcounts
    dest = pstarts[se] + (jnp.arange(m, dtype=jnp.int32) - starts[se])
    nblk = (m + MOE_ROW_BLOCK - 1) // MOE_ROW_BLOCK + MOE_EXPERTS
    rows = nblk * MOE_ROW_BLOCK
    row_tok = jnp.full((rows,), ntok, jnp.int32).at[dest].set(flat_tok[order])
    row_gate = jnp.zeros((rows,), f32).at[dest].set(flat_g[order])
    blk_start = jnp.arange(nblk, dtype=jnp.int32) * MOE_ROW_BLOCK
    blk_exp = jnp.minimum(jnp.searchsorted(pends, blk_start, side='right'), MOE_EXPERTS - 1)
    xpad = jnp.concatenate([xt, jnp.zeros((1, dm), xt.dtype)], axis=0)
    xr = xpad[row_tok].reshape(nblk, MOE_ROW_BLOCK, dm)

    def expert_block(args):
        xb, e = args
        gu = xb @ w_gate_up[e]
        h = jax.nn.silu(gu[:, :MOE_FF]) * gu[:, MOE_FF:]
        return h @ w_down[e]

    yr = lax.map(expert_block, (xr, blk_exp)).reshape(rows, dm)
    yr = yr * row_gate[:, None].astype(yr.dtype)
    y = jax.ops.segment_sum(yr, row_tok, num_segments=ntok + 1)[:ntok]
    return y.reshape(bsz, seq, dm)


def setup_inputs(seed: int = 0) -> dict:
    key = jax.random.key(seed)
    keys = iter(jax.random.split(key, 64))
    f32 = jnp.float32
    L = DEPTH

    def normal(shape, scale):
        return scale * jax.random.normal(next(keys), shape, f32)

    def gain(shape):
        return 1.0 + normal(shape, 0.02)

    def log_uniform(shape, lo, hi):
        return jax.random.uniform(next(keys), shape, f32, math.log(lo), math.log(hi))

    x = normal((BATCH, SEQ, D_MODEL), 1.0)
    mem = normal((BATCH, MEM_LEN, D_MODEL), 1.0)
    positions = (jax.random.randint(next(keys), (BATCH, 1), 0, MAX_POS_OFFSET, dtype=jnp.int32)
                 + jnp.arange(SEQ, dtype=jnp.int32)[None, :])
    w_in = normal((L, D_MODEL, IN_COLS), D_MODEL ** -0.5)
    s5_lambda_re = -0.5 + normal((L, S5_GROUPS, S5_STATE), 0.01)
    s5_lambda_im = math.pi * jnp.arange(S5_STATE, dtype=f32) + normal((L, S5_GROUPS, S5_STATE), 0.01)
    s5_log_step = log_uniform((L, S5_GROUPS), 1e-3, 1e-1)
    b_scale = (2 * S5_CH_PER_GROUP) ** -0.5
    s5_b_re = normal((L, S5_GROUPS, S5_STATE, S5_CH_PER_GROUP), b_scale)
    s5_b_im = normal((L, S5_GROUPS, S5_STATE, S5_CH_PER_GROUP), b_scale)
    c_scale = S5_STATE ** -0.5
    s5_c_re = normal((L, S5_GROUPS, S5_CH_PER_GROUP, S5_STATE), c_scale)
    s5_c_im = normal((L, S5_GROUPS, S5_CH_PER_GROUP, S5_STATE), c_scale)
    s5_d = normal((L, S5_GROUPS, S5_CH_PER_GROUP), 1.0)
    s5_w_glu = normal((L, S5_WIDTH, S5_WIDTH), S5_WIDTH ** -0.5)
    s5_b_glu = normal((L, S5_WIDTH), 0.02)
    s5_out_norm = gain((L, S5_WIDTH))
    mla_q_norm = gain((L, MLA_Q_RANK))
    mla_w_uq = normal((L, MLA_Q_RANK, MLA_HEADS * (MLA_NOPE + MLA_ROPE)), MLA_Q_RANK ** -0.5)
    mla_kv_norm = gain((L, MLA_KV_RANK))
    mla_w_ukv = normal((L, MLA_KV_RANK, MLA_HEADS * (MLA_NOPE + MLA_V)), MLA_KV_RANK ** -0.5)
    mla_out_norm = gain((L, MLA_WIDTH))
    gdn_conv = normal((L, GDN_CONV, GDN_QKV), GDN_CONV ** -0.5)
    gdn_a_log = jnp.log(jax.random.uniform(next(keys), (L, GDN_HEADS), f32, 1.0, 16.0))
    dt = jnp.exp(log_uniform((L, GDN_HEADS), 1e-3, 1e-1))
    gdn_dt_bias = dt + jnp.log(-jnp.expm1(-dt))
    gdn_out_norm = gain((L, GDN_DV))
    w_out = normal((L, MIX_WIDTH, D_MODEL), DN_BETA * MIX_WIDTH ** -0.5)
    ln1_g = gain((L, D_MODEL))
    ln1_b = normal((L, D_MODEL), 0.02)
    xa_w_q = normal((L, D_MODEL, XA_WIDTH), D_MODEL ** -0.5)
    xa_w_k = normal((L, D_MODEL, XA_WIDTH), D_MODEL ** -0.5)
    xa_w_v = normal((L, D_MODEL, XA_WIDTH), D_MODEL ** -0.5)
    xa_w_o = normal((L, XA_WIDTH, D_MODEL), DN_BETA * XA_WIDTH ** -0.5)
    ln2_g = gain((L, D_MODEL))
    ln2_b = normal((L, D_MODEL), 0.02)
    moe_w_group = normal((L, D_MODEL, MOE_GROUPS), D_MODEL ** -0.5)
    moe_b_group = normal((L, MOE_GROUPS), 0.01)
    moe_w_expert = normal((L, D_MODEL, MOE_EXPERTS), D_MODEL ** -0.5)
    moe_b_expert = normal((L, MOE_EXPERTS), 0.01)
    moe_w_gate_up = normal((L, MOE_EXPERTS, D_MODEL, 2 * MOE_FF), D_MODEL ** -0.5)
    moe_w_down = normal((L, MOE_EXPERTS, MOE_FF, D_MODEL), DN_BETA * MOE_FF ** -0.5)
    ln3_g = gain((L, D_MODEL))
    ln3_b = normal((L, D_MODEL), 0.02)
    return {'x': x, 'mem': mem, 'positions': positions, 'w_in': w_in,
            's5_lambda_re': s5_lambda_re, 's5_lambda_im': s5_lambda_im, 's5_log_step': s5_log_step,
            's5_b_re': s5_b_re, 's5_b_im': s5_b_im, 's5_c_re': s5_c_re, 's5_c_im': s5_c_im,
            's5_d': s5_d, 's5_w_glu': s5_w_glu, 's5_b_glu': s5_b_glu, 's5_out_norm': s5_out_norm,
            'mla_q_norm': mla_q_norm, 'mla_w_uq': mla_w_uq, 'mla_kv_norm': mla_kv_norm,
            'mla_w_ukv': mla_w_ukv, 'mla_out_norm': mla_out_norm,
            'gdn_conv': gdn_conv, 'gdn_a_log': gdn_a_log, 'gdn_dt_bias': gdn_dt_bias,
            'gdn_out_norm': gdn_out_norm, 'w_out': w_out, 'ln1_g': ln1_g, 'ln1_b': ln1_b,
            'xa_w_q': xa_w_q, 'xa_w_k': xa_w_k, 'xa_w_v': xa_w_v, 'xa_w_o': xa_w_o,
            'ln2_g': ln2_g, 'ln2_b': ln2_b,
            'moe_w_group': moe_w_group, 'moe_b_group': moe_b_group,
            'moe_w_expert': moe_w_expert, 'moe_b_expert': moe_b_expert,
            'moe_w_gate_up': moe_w_gate_up, 'moe_w_down': moe_w_down,
            'ln3_g': ln3_g, 'ln3_b': ln3_b}


def reference(x, mem, positions, w_in, s5_lambda_re, s5_lambda_im, s5_log_step, s5_b_re, s5_b_im,
              s5_c_re, s5_c_im, s5_d, s5_w_glu, s5_b_glu, s5_out_norm, mla_q_norm, mla_w_uq,
              mla_kv_norm, mla_w_ukv, mla_out_norm, gdn_conv, gdn_a_log, gdn_dt_bias, gdn_out_norm,
              w_out, ln1_g, ln1_b, xa_w_q, xa_w_k, xa_w_v, xa_w_o, ln2_g, ln2_b,
              moe_w_group, moe_b_group, moe_w_expert, moe_b_expert, moe_w_gate_up, moe_w_down,
              ln3_g, ln3_b):
    for l in range(DEPTH):
        proj = x @ w_in[l]
        u, cq, ckv, kr, gq, gk, gv, gz, ga, gb = _split_in_proj(proj)
        y_s5 = _s5_group(u, s5_lambda_re[l], s5_lambda_im[l], s5_log_step[l], s5_b_re[l],
                         s5_b_im[l], s5_c_re[l], s5_c_im[l], s5_d[l], s5_w_glu[l], s5_b_glu[l],
                         s5_out_norm[l])
        y_mla = _mla_group(cq, ckv, kr, positions, mla_q_norm[l], mla_w_uq[l], mla_kv_norm[l],
                           mla_w_ukv[l], mla_out_norm[l])
        y_gdn = _gdn_group(gq, gk, gv, gz, ga, gb, gdn_conv[l], gdn_a_log[l], gdn_dt_bias[l],
                           gdn_out_norm[l])
        mixed = jnp.concatenate([y_s5, y_mla, y_gdn], axis=-1) @ w_out[l]
        x = _layernorm(DN_ALPHA * x + mixed, ln1_g[l], ln1_b[l])
        xa = _memory_xattn(x, mem, xa_w_q[l], xa_w_k[l], xa_w_v[l], xa_w_o[l])
        x = _layernorm(DN_ALPHA * x + xa, ln2_g[l], ln2_b[l])
        ff = _hier_moe(x, moe_w_group[l], moe_b_group[l], moe_w_expert[l], moe_b_expert[l],
                       moe_w_gate_up[l], moe_w_down[l])
        x = _layernorm(DN_ALPHA * x + ff, ln3_g[l], ln3_b[l])
    return x
```

```python
import math
from contextlib import ExitStack
from concourse.bass_utils import run_bass_kernel_spmd
import numpy as np
import concourse.bass as bass
import concourse.mybir as mybir

F32 = mybir.dt.float32
BF16 = mybir.dt.bfloat16
I32 = mybir.dt.int32
U32 = mybir.dt.uint32
AF = mybir.ActivationFunctionType
ALU = mybir.AluOpType
AX = mybir.AxisListType


class Buf:
    def __init__(self, k, name, ap):
        self.k = k
        self.name = name
        self.ap = ap
        self.w = None
        self.r = {}
        self.dw = None
        self.dr = None

    def __getitem__(self, idx):
        return self.ap[idx]


class Eng:
    def __init__(self, k, name, eng, sem):
        self.k = k
        self.name = name
        self.eng = eng
        self.sem = sem
        self.count = 0
        self.seen = {}


class MK:
    def __init__(self, nc, stack, same_engine_sync=True, pe_self_sync=False):
        self.nc = nc
        self.stack = stack
        self.sems = {}
        self.semval = {}
        self.same_engine_sync = same_engine_sync
        self.pe_self_sync = pe_self_sync
        self.engs = {}
        for name in ["tensor", "vector", "scalar", "gpsimd", "sync"]:
            s = self._newsem("e_" + name)
            self.engs[name] = Eng(self, name, getattr(nc, name), s)
        self.pe = self.engs["tensor"]
        self.dve = self.engs["vector"]
        self.act = self.engs["scalar"]
        self.pool = self.engs["gpsimd"]
        self.sp = self.engs["sync"]
        self.nbuf = 0
        self.ninst = 0

    def _newsem(self, key):
        h = self.stack.enter_context(self.nc.semaphore(key))
        self.sems[key] = h
        self.semval[key] = 0
        return key

    def sbuf(self, name, shape, dt, stack=None):
        t = (stack or self.stack).enter_context(self.nc.sbuf_tensor(name, list(shape), dt))
        return Buf(self, name, t)

    def psum(self, name, shape, dt):
        t = self.stack.enter_context(self.nc.psum_tensor(name, list(shape), dt))
        return Buf(self, name, t)

    def dram(self, name, shape, dt, kind="Internal"):
        t = self.nc.dram_tensor(name, list(shape), dt, kind=kind)
        return Buf(self, name, t.ap())

    def view(self, name, ap):
        return Buf(self, name, ap)

    def _wait(self, E, deps):
        for (sk, val) in deps:
            if sk == E.sem and (not self.same_engine_sync or (E.name == "tensor" and not self.pe_self_sync)):
                continue
            if E.seen.get(sk, 0) < val:
                E.eng.wait_ge(self.sems[sk], val)
                E.seen[sk] = val

    def _deps(self, reads, writes, skip=None):
        deps = []
        for b in reads:
            if b.w is not None:
                deps.append(b.w)
        for b in writes:
            if b.w is not None and b.w[0] != skip:
                deps.append(b.w)
            for sk, v in b.r.items():
                deps.append((sk, v))
        return deps

    def op(self, E, fn, reads=(), writes=()):
        if isinstance(E, str):
            E = self.engs[E]
        self._wait(E, self._deps(reads, writes))
        inst = fn(E.eng)
        E.count += 1
        inst.then_inc(self.sems[E.sem], 1)
        tok = (E.sem, E.count)
        for b in writes:
            b.w = tok
            b.r = {}
        for b in reads:
            if b not in writes:
                b.r[E.sem] = E.count
        self.ninst += 1
        return inst

    def dma(self, Q, out_ap, in_ap, reads=(), writes=(), indirect=None, **kw):
        if isinstance(Q, str):
            Q = self.engs[Q]
        assert len(writes) == 1
        wb = writes[0]
        if wb.dw is None:
            wb.dw = self._newsem("dw_" + wb.name)
        self._wait(Q, self._deps(reads, writes, skip=wb.dw))
        if Q.name == "gpsimd":
            prev = getattr(self, "_last_swdge", None)
            if prev is not None:
                self._wait(Q, [prev])
        if indirect is None:
            inst = Q.eng.dma_start(out=out_ap, in_=in_ap, **kw)
        else:
            inst = indirect(Q.eng)
        inst.then_inc(self.sems[wb.dw], 16)
        self.semval[wb.dw] += 16
        tok = (wb.dw, self.semval[wb.dw])
        if Q.name == "gpsimd":
            self._last_swdge = tok
        wb.w = tok
        wb.r = {}
        for b in reads:
            b.r[wb.dw] = self.semval[wb.dw]
        self.ninst += 1
        return inst

    def barrier(self):
        for E in self.engs.values():
            deps = [(o.sem, o.count) for o in self.engs.values() if o.count > 0]
            deps += [(sk, v) for sk, v in self.semval.items() if sk.startswith("dw_") and v > 0]
            self._wait(E, deps)

    def finish(self, bufs):
        for b in bufs:
            if b.w is not None:
                self._wait(self.sp, [b.w])


NCORES = 8
D = 2048
SEQ = 4096
BATCH = 4
TPC = 2048
IN_COLS = 3656
DN_ALPHA = 4.0 ** 0.25


def new_nc():
    return bass.Bass("TRN2", target_bir_lowering=False)


def run(nc, in_maps):
    res = run_bass_kernel_spmd(nc, in_maps, core_ids=list(range(NCORES)))
    return res.results


def build_gemm(T, K, N):
    nc = new_nc()
    xT = nc.dram_tensor("xT", [K, T], F32, kind="ExternalInput").ap()
    w = nc.dram_tensor("w", [K, N], F32, kind="ExternalInput").ap()
    out = nc.dram_tensor("out", [T, N], F32, kind="ExternalOutput").ap()
    KC = K // 128
    TT = T // 128
    with ExitStack() as st:
        k = MK(nc, st)
        xs = k.sbuf("xs", [128, KC, T], BF16)
        k.dma("gpsimd", xs[:], xT.rearrange("(c p) t -> p c t", p=128), writes=[xs])
        wb = [k.sbuf(f"wb{i}", [128, KC, 512], BF16) for i in range(2)]
        ps = [k.psum(f"ps{i}", [128, 512], F32) for i in range(4)]
        ob = [k.sbuf(f"ob{i}", [128, 512], F32) for i in range(4)]
        od = k.view("od", out)
        nblk = (N + 511) // 512
        it = 0
        for nb in range(nblk):
            n0 = nb * 512
            nw = min(512, N - n0)
            W = wb[nb % 2]
            k.dma("gpsimd", W[:, :, :nw], w[:, n0:n0 + nw].rearrange("(c p) n -> p c n", p=128), writes=[W])
            for tt in range(TT):
                P = ps[it % 4]
                O = ob[it % 4]
                for c in range(KC):
                    k.op("tensor", lambda e: e.matmul(P[:, :nw], lhsT=xs[:, c, tt * 128:(tt + 1) * 128], rhs=W[:, c, :nw],
                                                      start=(c == 0), stop=(c == KC - 1)), reads=[xs, W], writes=[P])
                if it % 2 == 0:
                    k.op("vector", lambda e: e.tensor_copy(out=O[:, :nw], in_=P[:, :nw]), reads=[P], writes=[O])
                else:
                    k.op("scalar", lambda e: e.activation(out=O[:, :nw], in_=P[:, :nw], func=AF.Copy), reads=[P], writes=[O])
                k.dma("sync", out[tt * 128:(tt + 1) * 128, n0:n0 + nw], O[:, :nw], reads=[O], writes=[od])
                it += 1
        k.finish([od])
    return nc


def make_ident(k, n=128, dt=F32, name="ident"):
    t = k.sbuf(name, [n, n], dt)
    k.op("gpsimd", lambda e: e.memset(t[:], 0.0), writes=[t])
    k.op("gpsimd", lambda e: e.affine_select(out=t[:], in_=t[:], pattern=[[-1, n]], compare_op=ALU.not_equal,
                                             fill=1.0, base=0, channel_multiplier=1), reads=[t], writes=[t])
    return t


def make_tri(k, name, n, keep_q_ge_k=True, dt=F32, strict=False):
    t = k.sbuf(name, [n, n], dt)
    k.op("gpsimd", lambda e: e.memset(t[:], 1.0), writes=[t])
    k.op("gpsimd", lambda e: e.affine_select(out=t[:], in_=t[:], pattern=[[1, n]], compare_op=ALU.is_ge,
                                             fill=0.0, base=(-1 if strict else 0), channel_multiplier=-1),
         reads=[t], writes=[t])
    return t


MLA_SCALE = 192.0 ** -0.5


def build_mla(stage=9):
    S = SEQ
    nc = new_nc()
    di = lambda n, shp, dt=F32: nc.dram_tensor(n, shp, dt, kind="ExternalInput").ap()
    cqT = di("cqT", [512, S]); ckvT = di("ckvT", [512, S])
    kr1 = di("kr1", [128, S]); kr2 = di("kr2", [128, S])
    msgn = di("msgn", [128, 1])
    pos = di("pos", [1, S], I32)
    invf = di("invf", [128, 1])
    qn = di("qn", [128, 4]); kvn = di("kvn", [128, 4])
    wqn = di("wqn", [512, 256]); wq1 = di("wq1", [512, 128]); wq2 = di("wq2", [512, 128])
    wk = di("wk", [512, 256]); wv = di("wv", [512, 256])
    out = nc.dram_tensor("o", [S, 256], F32, kind="ExternalOutput").ap()
    NB = S // 512
    with ExitStack() as st:
        k = MK(nc, st)
        od = k.view("od", out)
        ps = [k.psum(f"ps{i}", [128, 512], F32) for i in range(8)]
        ones_f = k.sbuf("ones_f", [128, 128], F32)
        k.op("vector", lambda e: e.memset(ones_f[:], 1.0), writes=[ones_f])
        ones_b = k.sbuf("ones_b", [128, 128], BF16)
        k.op("vector", lambda e: e.memset(ones_b[:], 1.0), writes=[ones_b])
        trimask = make_tri(k, "trimask", 128, dt=BF16)
        Wqn = k.sbuf("Wqn", [128, 4, 256], BF16); Wq1 = k.sbuf("Wq1", [128, 4, 128], BF16)
        Wq2 = k.sbuf("Wq2", [128, 4, 128], BF16); Wk = k.sbuf("Wk", [128, 4, 256], BF16)
        Wv = k.sbuf("Wv", [128, 4, 256], BF16)
        for Wt, wd in ((Wqn, wqn), (Wq1, wq1), (Wq2, wq2), (Wk, wk), (Wv, wv)):
            k.dma("gpsimd", Wt[:], wd.rearrange("(c p) n -> p c n", p=128), writes=[Wt])
        qn_s = k.sbuf("qn_s", [128, 4], F32); kvn_s = k.sbuf("kvn_s", [128, 4], F32)
        k.dma("sync", qn_s[:], qn, writes=[qn_s]); k.dma("sync", kvn_s[:], kvn, writes=[kvn_s])
        invf_s = k.sbuf("invf_s", [128, 1], F32)
        k.dma("sync", invf_s[:], invf, writes=[invf_s])
        msgn_s = k.sbuf("msgn_s", [128, 1], F32)
        k.dma("sync", msgn_s[:], msgn, writes=[msgn_s])
        mone_s = k.sbuf("mone_s", [128, 1], F32)
        k.op("vector", lambda e: e.memset(mone_s[:], -1.0), writes=[mone_s])
        cosT = k.sbuf("cosT", [128, S], BF16); sinT = k.sbuf("sinT", [128, S], BF16)
        negpi = k.sbuf("negpi", [128, 1], F32)
        k.op("vector", lambda e: e.memset(negpi[:], -math.pi), writes=[negpi])
        with ExitStack() as tst:
            posi = k.sbuf("posi", [128, S], I32, stack=tst)
            k.dma("sync", posi[:], pos.to_broadcast([128, S]), writes=[posi])
            ang = k.sbuf("ang", [128, S], F32, stack=tst)
            k.op("vector", lambda e: e.tensor_copy(out=ang[:], in_=posi[:]), reads=[posi], writes=[ang])
            k.op("vector", lambda e: e.tensor_scalar(out=ang[:], in0=ang[:], scalar1=invf_s[:, 0:1], scalar2=1.0 / (2 * math.pi),
                                                     op0=ALU.mult, op1=ALU.mult), reads=[ang, invf_s], writes=[ang])
            tmpf = k.sbuf("tmpf", [128, S], F32, stack=tst)
            tmpg = k.sbuf("tmpg", [128, S], F32, stack=tst)

            def sin_of_turns(dst, shift, mul):
                k.op("vector", lambda e: e.tensor_scalar(out=tmpf[:], in0=ang[:], scalar1=shift, scalar2=None, op0=ALU.add),
                     reads=[ang], writes=[tmpf])
                k.op("vector", lambda e: e.tensor_copy(out=posi[:], in_=tmpf[:]), reads=[tmpf], writes=[posi])
                k.op("vector", lambda e: e.tensor_copy(out=tmpg[:], in_=posi[:]), reads=[posi], writes=[tmpg])
                k.op("vector", lambda e: e.tensor_tensor(out=tmpf[:], in0=tmpf[:], in1=tmpg[:], op=ALU.subtract),
                     reads=[tmpf, tmpg], writes=[tmpf])
                k.op("vector", lambda e: e.tensor_scalar(out=tmpg[:], in0=tmpf[:], scalar1=0.0, scalar2=None, op0=ALU.is_lt),
                     reads=[tmpf], writes=[tmpg])
                k.op("vector", lambda e: e.tensor_tensor(out=tmpf[:], in0=tmpf[:], in1=tmpg[:], op=ALU.add),
                     reads=[tmpf, tmpg], writes=[tmpf])
                k.op("scalar", lambda e: e.activation(out=tmpg[:], in_=tmpf[:], func=AF.Sin, bias=negpi[:, 0:1], scale=2 * math.pi),
                     reads=[tmpf, negpi], writes=[tmpg])
                k.op("vector", lambda e: e.tensor_scalar(out=dst[:], in0=tmpg[:], scalar1=mul[:, 0:1], scalar2=None, op0=ALU.mult),
                     reads=[tmpg, mul], writes=[dst])

            sin_of_turns(sinT, 0.0, msgn_s)
            sin_of_turns(cosT, 0.25, mone_s)
            k.barrier()
        if stage == 0:
            k.dma("sync", out[0:128, 0:128], ones_f[:], reads=[ones_f, sinT, cosT], writes=[od]); k.finish([od]); return nc
        QN = [k.sbuf(f"QN{h}", [128, S], BF16) for h in range(2)]
        KN = [k.sbuf(f"KN{h}", [128, S], BF16) for h in range(2)]
        QR = [k.sbuf(f"QR{i}", [128, S], BF16) for i in range(1)]
        KR = k.sbuf("KR", [128, S], BF16)
        Va = k.sbuf("Va", [128, S // 128, 2, 129], BF16)
        k.op("gpsimd", lambda e: e.memset(Va[:], 1.0), writes=[Va])
        arow = k.sbuf("arow", [128, S], BF16)
        krow = k.sbuf("krow", [128, 2, 128], BF16)
        kmax = k.sbuf("kmax", [128, 2, NB], F32)
        krs = k.sbuf("krs", [128, 2, 512], F32)
        t1 = k.sbuf("t1", [128, 512], F32); t2 = k.sbuf("t2", [128, 512], F32)

        def rope(dst, x, xs, sl, rd):
            k.op("vector", lambda e: e.tensor_tensor(out=t1[:], in0=x, in1=cosT[:, sl], op=ALU.mult), reads=rd + [cosT], writes=[t1])
            k.op("vector", lambda e: e.tensor_tensor(out=t2[:], in0=xs, in1=sinT[:, sl], op=ALU.mult), reads=rd + [sinT], writes=[t2])
            k.op("vector", lambda e: e.tensor_tensor(out=dst[:, sl], in0=t1[:], in1=t2[:], op=ALU.add), reads=[t1, t2], writes=[dst])

        xin = [k.sbuf("xin0", [128, 4, 512], F32)]
        sq = k.sbuf("sq", [128, 4, 512], F32)
        xin.append(sq)
        rstd = k.sbuf("rstd", [128, 512], F32)
        xn0 = k.sbuf("xn0", [128, 4, 512], BF16)
        xn = [xn0, xn0]
        sqb = k.sbuf("sqb", [128, 512], F32)

        def rmsnorm_block(src_dram, sl, gam, XI, XN):
            k.dma("sync", XI[:], src_dram[:, sl].rearrange("(c p) t -> p c t", p=128), writes=[XI])
            k.op("scalar", lambda e: e.activation(out=sq[:], in_=XI[:], func=AF.Square), reads=[XI], writes=[sq])
            P = ps[7]
            for c in range(4):
                k.op("tensor", lambda e: e.matmul(P[:], lhsT=ones_f[:], rhs=sq[:, c, :], start=(c == 0), stop=(c == 3)),
                     reads=[ones_f, sq], writes=[P])
            k.op("scalar", lambda e: e.activation(out=rstd[:], in_=P[:], func=AF.Sqrt, bias=eps_t[:, 0:1], scale=1.0 / 512),
                 reads=[P, eps_t], writes=[rstd])
            k.op("vector", lambda e: e.reciprocal(out=rstd[:], in_=rstd[:]), reads=[rstd], writes=[rstd])
            for c in range(4):
                k.op("vector", lambda e: e.scalar_tensor_tensor(out=XN[:, c, :], in0=XI[:, c, :], scalar=gam[:, c:c + 1], in1=rstd[:],
                                                                op0=ALU.mult, op1=ALU.mult), reads=[XI, gam, rstd], writes=[XN])

        eps_t = k.sbuf("eps_t", [128, 1], F32)
        k.op("vector", lambda e: e.memset(eps_t[:], 1e-6), writes=[eps_t])

        for tb in range(NB):
            sl = slice(tb * 512, (tb + 1) * 512)
            k.dma("sync", krs[:, 0, :], kr1[:, sl], writes=[krs])
            k.dma("sync", krs[:, 1, :], kr2[:, sl], writes=[krs])
            rope(KR, krs[:, 0, :], krs[:, 1, :], sl, [krs])
            if stage == 1.1:
                k.barrier(); k.dma("sync", out[0:128, 0:128], ones_f[:], reads=[ones_f], writes=[od]); k.finish([od]); return nc
            XI, XN = xin[0], xn[0]
            rmsnorm_block(cqT, sl, qn_s, XI, XN)
            if stage == 1.2:
                k.barrier(); k.dma("sync", out[0:128, 0:128], ones_f[:], reads=[ones_f], writes=[od]); k.finish([od]); return nc
            for h in range(2):
                P = ps[h]
                for c in range(4):
                    k.op("tensor", lambda e: e.matmul(P[:], lhsT=Wqn[:, c, h * 128:(h + 1) * 128], rhs=XN[:, c, :], start=(c == 0), stop=(c == 3)),
                         reads=[Wqn, XN], writes=[P])
                k.op("scalar", lambda e: e.activation(out=QN[h][:, sl], in_=P[:], func=AF.Copy, scale=MLA_SCALE), reads=[P], writes=[QN[h]])
                k.op("scalar", lambda e: e.activation(out=XI[:, h, :], in_=P[:], func=AF.Square, scale=MLA_SCALE), reads=[P], writes=[XI])
            if stage == 1.3:
                k.barrier(); k.dma("sync", out[0:128, 0:128], ones_f[:], reads=[ones_f], writes=[od]); k.finish([od]); return nc
            qa = xin[1]
            for pr in range(1):
                P1, P2 = ps[4], ps[5]
                for c in range(4):
                    k.op("tensor", lambda e: e.matmul(P1[:], lhsT=Wq1[:, c, pr * 128:(pr + 1) * 128], rhs=XN[:, c, :], start=(c == 0), stop=(c == 3)), reads=[Wq1, XN], writes=[P1])
                for c in range(4):
                    k.op("tensor", lambda e: e.matmul(P2[:], lhsT=Wq2[:, c, pr * 128:(pr + 1) * 128], rhs=XN[:, c, :], start=(c == 0), stop=(c == 3)), reads=[Wq2, XN], writes=[P2])
                k.op("scalar", lambda e: e.activation(out=qa[:, 0, :], in_=P1[:], func=AF.Copy, scale=MLA_SCALE), reads=[P1], writes=[qa])
                k.op("scalar", lambda e: e.activation(out=qa[:, 1, :], in_=P2[:], func=AF.Copy, scale=MLA_SCALE), reads=[P2], writes=[qa])
                rope(QR[pr], qa[:, 0, :], qa[:, 1, :], sl, [qa])
                k.op("scalar", lambda e: e.activation(out=qa[:, 2 + pr, :], in_=qa[:, 0, :], func=AF.Square), reads=[qa], writes=[qa])
            if stage == 1.4:
                k.barrier(); k.dma("sync", out[0:128, 0:128], ones_f[:], reads=[ones_f], writes=[od]); k.finish([od]); return nc
            for h in range(2):
                P = ps[6]
                b64 = (h % 2) * 64
                k.op("tensor", lambda e: e.matmul(P[:], lhsT=ones_f[:], rhs=XI[:, h, :], start=True, stop=False), reads=[ones_f, XI], writes=[P])
                k.op("tensor", lambda e: e.matmul(P[:], lhsT=ones_f[b64:b64 + 64, :], rhs=qa[b64:b64 + 64, 2, :], start=False, stop=True),
                     reads=[ones_f, qa], writes=[P])
                hp = h * 32
                k.op("scalar", lambda e: e.activation(out=arow[hp:hp + 1, tb * 512:tb * 512 + 512], in_=P[hp:hp + 1, :], func=AF.Sqrt), reads=[P], writes=[arow])
            if stage == 1.5:
                k.barrier(); k.dma("sync", out[0:128, 0:128], ones_f[:], reads=[ones_f], writes=[od]); k.finish([od]); return nc
            XI, XN = xin[0], xn[1]
            rmsnorm_block(ckvT, sl, kvn_s, XI, XN)
            if stage == 1.55:
                k.barrier(); k.dma("sync", out[0:128, 0:128], ones_f[:], reads=[ones_f], writes=[od]); k.finish([od]); return nc
            for h in range(2):
                P = ps[h]
                for c in range(4):
                    k.op("tensor", lambda e: e.matmul(P[:], lhsT=Wk[:, c, h * 128:(h + 1) * 128], rhs=XN[:, c, :], start=(c == 0), stop=(c == 3)),
                         reads=[Wk, XN], writes=[P])
                k.op("vector", lambda e: e.tensor_copy(out=KN[h][:, sl], in_=P[:]), reads=[P], writes=[KN[h]])
                if stage == 1.56:
                    continue
                k.op("scalar", lambda e: e.activation(out=qa[:, h, :], in_=KN[h][:, sl], func=AF.Square), reads=[KN[h]], writes=[qa])
            if stage in (1.6, 1.61):
                k.barrier(); k.dma("sync", out[0:128, 0:128], ones_f[:], reads=[ones_f], writes=[od]); k.finish([od]); return nc
            if stage == 1.56:
                k.barrier(); k.dma("sync", out[0:128, 0:128], ones_f[:], reads=[ones_f], writes=[od]); k.finish([od]); return nc
            k.op("vector", lambda e: e.tensor_tensor(out=sqb[:], in0=KR[:, sl], in1=KR[:, sl], op=ALU.mult), reads=[KR], writes=[sqb])
            for h in range(2):
                P = ps[6]
                k.op("tensor", lambda e: e.matmul(P[:], lhsT=ones_f[:], rhs=qa[:, h, :], start=True, stop=False), reads=[ones_f, qa], writes=[P])
                k.op("tensor", lambda e: e.matmul(P[:], lhsT=ones_f[0:64, :], rhs=sqb[0:64, :], start=False, stop=True), reads=[ones_f, sqb], writes=[P])
                k.op("vector", lambda e: e.tensor_reduce(out=kmax[:, h, tb:tb + 1], in_=P[:], axis=AX.X, op=ALU.max), reads=[P], writes=[kmax])
            if stage == 1.7:
                k.barrier(); k.dma("sync", out[0:128, 0:128], ones_f[:], reads=[ones_f], writes=[od]); k.finish([od]); return nc
            for tt in range(4):
                P = ps[tt]
                for c in range(4):
                    k.op("tensor", lambda e: e.matmul(P[:, 0:256], lhsT=XN[:, c, tt * 128:(tt + 1) * 128], rhs=Wv[:, c, :], start=(c == 0), stop=(c == 3)),
                         reads=[XN, Wv], writes=[P])
                k.op("vector", lambda e: e.tensor_copy(out=Va[:, tb * 4 + tt, :, 0:128], in_=P[:, 0:256].rearrange("p (h d) -> p h d", h=2)),
                     reads=[P], writes=[Va])
        if stage == 1:
            k.dma("sync", out[0:128, 0:128], ones_f[:], reads=[ones_f, Va, KR, arow] + QN + KN + QR, writes=[od]); k.finish([od]); return nc
        km = k.sbuf("km", [128, 2], F32)
        for h in range(2):
            k.op("vector", lambda e: e.tensor_reduce(out=km[:, h:h + 1], in_=kmax[:, h, :], axis=AX.X, op=ALU.max), reads=[kmax], writes=[km])
        k.op("scalar", lambda e: e.activation(out=km[:], in_=km[:], func=AF.Sqrt), reads=[km], writes=[km])
        k.op("vector", lambda e: e.tensor_scalar(out=km[:], in0=km[:], scalar1=-1.0, scalar2=None, op0=ALU.mult), reads=[km], writes=[km])
        k.op("vector", lambda e: e.memset(krow[:], 1.0), writes=[krow])
        for h in range(2):
            hp = h * 32
            k.op("vector", lambda e: e.tensor_scalar(out=krow[hp:hp + 1, h, :], in0=krow[hp:hp + 1, h, :], scalar1=km[hp:hp + 1, h:h + 1],
                                                     scalar2=None, op0=ALU.mult), reads=[krow, km], writes=[krow])
        if stage == 2:
            k.dma("sync", out[0:128, 0:128], ones_f[:], reads=[ones_f, krow], writes=[od]); k.finish([od]); return nc
        PT = [k.sbuf(f"PT{i}", [128, 512], BF16) for i in range(3)]
        ot = [k.sbuf(f"ot{i}", [128, 128], F32) for i in range(2)]
        rc = [k.sbuf(f"rc{i}", [128, 1], F32) for i in range(2)]
        it = 0
        oi = 0
        for h in range(2):
            hs = slice((h % 2) * 64, (h % 2) * 64 + 64)
            hp = h * 32
            for qt in range(NB):
                qs = slice(qt * 512, (qt + 1) * 512)
                nkb = 4 * qt + 4
                for kb in range(nkb):
                    ks = slice(kb * 128, (kb + 1) * 128)
                    SP = ps[4 + it % 2]
                    Pt = PT[it % 3]
                    it += 1
                    k.op("tensor", lambda e: e.matmul(SP[:], lhsT=KN[h][:, ks], rhs=QN[h][:, qs], start=True, stop=False), reads=[KN[h], QN[h]], writes=[SP])
                    k.op("tensor", lambda e: e.matmul(SP[:], lhsT=KR[hs, ks], rhs=QR[h // 2][hs, qs], start=False, stop=False), reads=[KR, QR[h // 2]], writes=[SP])
                    k.op("tensor", lambda e: e.matmul(SP[:], lhsT=krow[hp:hp + 1, h, :], rhs=arow[hp:hp + 1, qt * 512:qt * 512 + 512], start=False, stop=True), reads=[krow, arow], writes=[SP])
                    k.op("scalar", lambda e: e.activation(out=Pt[:], in_=SP[:], func=AF.Exp), reads=[SP], writes=[Pt])
                    r = kb - 4 * qt
                    if r >= 0:
                        k.op("vector", lambda e: e.tensor_tensor(out=Pt[:, r * 128:(r + 1) * 128], in0=Pt[:, r * 128:(r + 1) * 128], in1=trimask[:], op=ALU.mult),
                             reads=[Pt, trimask], writes=[Pt])
                    for s in range(4):
                        if r >= 0 and s < r:
                            continue
                        O = ps[s]
                        k.op("tensor", lambda e: e.matmul(O[:, 0:129], lhsT=Pt[:, s * 128:(s + 1) * 128], rhs=Va[:, kb, h, :],
                                                          start=(kb == 0), stop=(kb == 4 * qt + s)), reads=[Pt, Va], writes=[O])
                for s in range(4):
                    O = ps[s]
                    R = rc[oi % 2]; OT = ot[oi % 2]; oi += 1
                    k.op("vector", lambda e: e.reciprocal(out=R[:], in_=O[:, 128:129]), reads=[O], writes=[R])
                    k.op("vector", lambda e: e.tensor_scalar(out=OT[:], in0=O[:, 0:128], scalar1=R[:, 0:1], scalar2=None, op0=ALU.mult),
                         reads=[O, R], writes=[OT])
                    q0 = qt * 512 + s * 128
                    k.dma("sync", out[q0:q0 + 128, h * 128:(h + 1) * 128], OT[:], reads=[OT], writes=[od])
        k.finish([od])
    return nc


C_U, C_CQ, C_CKV, C_KR, C_GQ, C_GK, C_GV, C_GZ, C_GA, C_GB = 0, 512, 1024, 1536, 1600, 2112, 2624, 3136, 3648, 3652


def _c(a):
    return np.ascontiguousarray(a)


def mla_inputs(proj, p, l, part):
    invf = (1.0 / (10000.0 ** (np.arange(32, dtype=np.float32) * (2.0 / 64)))).astype(np.float32)
    ins = []
    for c in range(NCORES):
        b, hh = c // 2, c % 2
        pb = proj[b]
        kr = pb[:, C_KR:C_KR + 64]
        heads = range(4 * hh + 2 * part, 4 * hh + 2 * part + 2)
        wuq = p["mla_w_uq"][l]; wukv = p["mla_w_ukv"][l]
        ins.append({
            "cqT": _c(pb[:, C_CQ:C_CQ + 512].T), "ckvT": _c(pb[:, C_CKV:C_CKV + 512].T),
            "kr1": _c(np.tile(kr.T, (2, 1))), "kr2": _c(np.tile(np.concatenate([kr[:, 32:], kr[:, :32]], 1).T, (2, 1))),
            "msgn": _c(np.tile(np.concatenate([np.ones(32), -np.ones(32)]), 2)[:, None].astype(np.float32)),
            "pos": _c(p["positions"][b][None].astype(np.int32)),
            "invf": _c(np.tile(invf, 4)[:, None]),
            "qn": _c(p["mla_q_norm"][l].reshape(4, 128).T), "kvn": _c(p["mla_kv_norm"][l].reshape(4, 128).T),
            "wqn": _c(np.concatenate([wuq[:, h * 192:h * 192 + 128] for h in heads], 1)),
            "wq1": _c(np.concatenate([wuq[:, h * 192 + 128:h * 192 + 192] for h in heads], 1)),
            "wq2": _c(np.concatenate([np.concatenate([wuq[:, h * 192 + 160:h * 192 + 192], wuq[:, h * 192 + 128:h * 192 + 160]], 1) for h in heads], 1)),
            "wk": _c(np.concatenate([wukv[:, h * 256:h * 256 + 128] for h in heads], 1)),
            "wv": _c(np.concatenate([wukv[:, h * 256 + 128:h * 256 + 256] for h in heads], 1)),
        })
    return ins


def mla_gather(o, res, part):
    for c in range(NCORES):
        b, hh = c // 2, c % 2
        c0 = hh * 512 + part * 256
        o[b, :, c0:c0 + 256] = res[c]["o"]
    return o


def build_s5():
    S = SEQ
    L = 128
    NCH = S // L
    nc = new_nc()
    di = lambda n, shp, dt=F32: nc.dram_tensor(n, shp, dt, kind="ExternalInput").ap()
    uT = di("uT", [256, S])
    prow = di("prow", [3, 1024])
    pcol = di("pcol", [128, 3, 8])
    BRc = di("BRc", [2, 128, 512]); BIc = di("BIc", [2, 128, 512])
    CRp = di("CRp", [128, 8, 32]); CIp = di("CIp", [128, 8, 32])
    Dd = di("Dd", [128, 2, 128])
    out = nc.dram_tensor("y", [S, 256], F32, kind="ExternalOutput").ap()
    TWO_PI = 2 * math.pi
    with ExitStack() as st:
        k = MK(nc, st)
        od = k.view("od", out)
        negpi = k.sbuf("negpi", [128, 1], F32)
        k.op("vector", lambda e: e.memset(negpi[:], -math.pi), writes=[negpi])
        us = k.sbuf("us", [128, 2, S], F32)
        k.dma("sync", us[:], uT.rearrange("(c p) t -> p c t", p=128), writes=[us])
        CR = k.sbuf("CR", [128, 8, 32], F32); CI = k.sbuf("CI", [128, 8, 32], F32); DD = k.sbuf("DD", [128, 2, 128], F32)
        k.dma("sync", CR[:], CRp, writes=[CR]); k.dma("sync", CI[:], CIp, writes=[CI]); k.dma("sync", DD[:], Dd, writes=[DD])
        k.op("vector", lambda e: e.tensor_scalar(out=CI[:], in0=CI[:], scalar1=-1.0, scalar2=None, op0=ALU.mult), reads=[CI], writes=[CI])
        triT = make_tri(k, "triT", 128, dt=F32)
        Bblk = k.sbuf("Bblk", [128, 2, 4, 2, 128], F32)
        Wn_re = k.sbuf("Wn_re", [128, 1024], F32); Wn_im = k.sbuf("Wn_im", [128, 1024], F32)
        Wp_re = k.sbuf("Wp_re", [128, 8, L + 1], F32); Wp_im = k.sbuf("Wp_im", [128, 8, L + 1], F32)
        iop = k.sbuf("iop", [128, 1], F32)
        iof = k.sbuf("iof", [128, L + 1], F32)
        k.op("gpsimd", lambda e: e.iota(iop[:], pattern=[[0, 1]], base=0, channel_multiplier=1, allow_small_or_imprecise_dtypes=True), writes=[iop])
        k.op("gpsimd", lambda e: e.iota(iof[:], pattern=[[1, L + 1]], base=0, channel_multiplier=0, allow_small_or_imprecise_dtypes=True), writes=[iof])

        def sincos(k, turns, n, dsin, dcos, tmpa, tmpb, tmpi_):
            for dst, shift in ((dsin, 0.0), (dcos, 0.25)):
                k.op("vector", lambda e: e.tensor_scalar(out=tmpa[:], in0=turns[:], scalar1=shift, scalar2=None, op0=ALU.add), reads=[turns], writes=[tmpa])
                k.op("vector", lambda e: e.tensor_copy(out=tmpi_[:], in_=tmpa[:]), reads=[tmpa], writes=[tmpi_])
                k.op("vector", lambda e: e.tensor_copy(out=tmpb[:], in_=tmpi_[:]), reads=[tmpi_], writes=[tmpb])
                k.op("vector", lambda e: e.tensor_tensor(out=tmpa[:], in0=tmpa[:], in1=tmpb[:], op=ALU.subtract), reads=[tmpa, tmpb], writes=[tmpa])
                k.op("vector", lambda e: e.tensor_scalar(out=tmpb[:], in0=tmpa[:], scalar1=0.0, scalar2=None, op0=ALU.is_lt), reads=[tmpa], writes=[tmpb])
                k.op("vector", lambda e: e.tensor_tensor(out=tmpa[:], in0=tmpa[:], in1=tmpb[:], op=ALU.add), reads=[tmpa, tmpb], writes=[tmpa])
                k.op("scalar", lambda e: e.activation(out=tmpb[:], in_=tmpa[:], func=AF.Sin, bias=negpi[:, 0:1], scale=TWO_PI), reads=[tmpa, negpi], writes=[tmpb])
                k.op("vector", lambda e: e.tensor_scalar(out=dst[:], in0=tmpb[:], scalar1=-1.0, scalar2=None, op0=ALU.mult), reads=[tmpb], writes=[dst])

        with ExitStack() as tst:
            T = lambda n, w=1024, dt=F32: k.sbuf(n, [128, w], dt, stack=tst)
            lr = T("lr"); li = T("li"); dtt = T("dtt")
            k.dma("sync", lr[:], prow[0:1, :].to_broadcast([128, 1024]), writes=[lr])
            k.dma("sync", li[:], prow[1:2, :].to_broadcast([128, 1024]), writes=[li])
            k.dma("sync", dtt[:], prow[2:3, :].to_broadcast([128, 1024]), writes=[dtt])
            k.op("vector", lambda e: e.tensor_scalar(out=lr[:], in0=lr[:], scalar1=-1e-4, scalar2=None, op0=ALU.min), reads=[lr], writes=[lr])
            k.op("scalar", lambda e: e.activation(out=dtt[:], in_=dtt[:], func=AF.Exp), reads=[dtt], writes=[dtt])
            a = T("a"); tu = T("tu")
            k.op("vector", lambda e: e.tensor_tensor(out=a[:], in0=lr[:], in1=dtt[:], op=ALU.mult), reads=[lr, dtt], writes=[a])
            k.op("vector", lambda e: e.tensor_tensor(out=tu[:], in0=li[:], in1=dtt[:], op=ALU.mult), reads=[li, dtt], writes=[tu])
            k.op("vector", lambda e: e.tensor_scalar(out=tu[:], in0=tu[:], scalar1=1.0 / TWO_PI, scalar2=None, op0=ALU.mult), reads=[tu], writes=[tu])
            sn = T("sn"); cs = T("cs"); ta = T("ta"); tb_ = T("tb_"); ti = T("ti", dt=I32)
            sincos(k, tu, 1024, sn, cs, ta, tb_, ti)
            mag = T("mag")
            k.op("scalar", lambda e: e.activation(out=mag[:], in_=a[:], func=AF.Exp), reads=[a], writes=[mag])
            k.op("vector", lambda e: e.tensor_tensor(out=cs[:], in0=cs[:], in1=mag[:], op=ALU.mult), reads=[cs, mag], writes=[cs])
            k.op("vector", lambda e: e.tensor_scalar(out=cs[:], in0=cs[:], scalar1=-1.0, scalar2=None, op0=ALU.add), reads=[cs], writes=[cs])
            k.op("vector", lambda e: e.tensor_tensor(out=sn[:], in0=sn[:], in1=mag[:], op=ALU.mult), reads=[sn, mag], writes=[sn])
            k.op("vector", lambda e: e.tensor_tensor(out=ta[:], in0=lr[:], in1=lr[:], op=ALU.mult), reads=[lr], writes=[ta])
            k.op("vector", lambda e: e.tensor_tensor(out=tb_[:], in0=li[:], in1=li[:], op=ALU.mult), reads=[li], writes=[tb_])
            k.op("vector", lambda e: e.tensor_tensor(out=ta[:], in0=ta[:], in1=tb_[:], op=ALU.add), reads=[ta, tb_], writes=[ta])
            k.op("vector", lambda e: e.reciprocal(out=ta[:], in_=ta[:]), reads=[ta], writes=[ta])
            fr = T("fr"); fi = T("fi")
            k.op("vector", lambda e: e.tensor_tensor(out=fr[:], in0=cs[:], in1=lr[:], op=ALU.mult), reads=[cs, lr], writes=[fr])
            k.op("vector", lambda e: e.tensor_tensor(out=tb_[:], in0=sn[:], in1=li[:], op=ALU.mult), reads=[sn, li], writes=[tb_])
            k.op("vector", lambda e: e.tensor_tensor(out=fr[:], in0=fr[:], in1=tb_[:], op=ALU.add), reads=[fr, tb_], writes=[fr])
            k.op("vector", lambda e: e.tensor_tensor(out=fr[:], in0=fr[:], in1=ta[:], op=ALU.mult), reads=[fr, ta], writes=[fr])
            k.op("vector", lambda e: e.tensor_tensor(out=fi[:], in0=sn[:], in1=lr[:], op=ALU.mult), reads=[sn, lr], writes=[fi])
            k.op("vector", lambda e: e.tensor_tensor(out=tb_[:], in0=cs[:], in1=li[:], op=ALU.mult), reads=[cs, li], writes=[tb_])
            k.op("vector", lambda e: e.tensor_tensor(out=fi[:], in0=fi[:], in1=tb_[:], op=ALU.subtract), reads=[fi, tb_], writes=[fi])
            k.op("vector", lambda e: e.tensor_tensor(out=fi[:], in0=fi[:], in1=ta[:], op=ALU.mult), reads=[fi, ta], writes=[fi])
            br = k.sbuf("br", [128, 2, 512], F32, stack=tst); bi = k.sbuf("bi", [128, 2, 512], F32, stack=tst)
            k.dma("sync", br[:], BRc.rearrange("c p n -> p c n"), writes=[br]); k.dma("sync", bi[:], BIc.rearrange("c p n -> p c n"), writes=[bi])
            for kc in range(2):
                frv = fr[:, kc * 512:(kc + 1) * 512].rearrange("p (a b) -> p a b", a=4)
                fiv = fi[:, kc * 512:(kc + 1) * 512].rearrange("p (a b) -> p a b", a=4)
                brv = br[:, kc, :].rearrange("p (a b) -> p a b", a=4); biv = bi[:, kc, :].rearrange("p (a b) -> p a b", a=4)
                tav = ta[:, 0:512].rearrange("p (a b) -> p a b", a=4); tbv = tb_[:, 0:512].rearrange("p (a b) -> p a b", a=4)
                k.op("vector", lambda e: e.tensor_tensor(out=tav, in0=brv, in1=frv, op=ALU.mult), reads=[br, fr], writes=[ta])
                k.op("vector", lambda e: e.tensor_tensor(out=tbv, in0=biv, in1=fiv, op=ALU.mult), reads=[bi, fi], writes=[tb_])
                k.op("vector", lambda e: e.tensor_tensor(out=Bblk[:, kc, :, 0, :], in0=tav, in1=tbv, op=ALU.subtract), reads=[ta, tb_], writes=[Bblk])
                k.op("vector", lambda e: e.tensor_tensor(out=tav, in0=brv, in1=fiv, op=ALU.mult), reads=[br, fi], writes=[ta])
                k.op("vector", lambda e: e.tensor_tensor(out=tbv, in0=biv, in1=frv, op=ALU.mult), reads=[bi, fr], writes=[tb_])
                k.op("vector", lambda e: e.tensor_tensor(out=Bblk[:, kc, :, 1, :], in0=tav, in1=tbv, op=ALU.add), reads=[ta, tb_], writes=[Bblk])
            k.op("vector", lambda e: e.tensor_scalar(out=ta[:], in0=tu[:], scalar1=iop[:, 0:1], scalar2=None, op0=ALU.mult), reads=[tu, iop], writes=[ta])
            sn2 = fr; cs2 = fi
            sincos(k, ta, 1024, sn2, cs2, mag, tb_, ti)
            nio = k.sbuf("nio", [128, 1], F32, stack=tst)
            k.op("vector", lambda e: e.tensor_scalar(out=nio[:], in0=iop[:], scalar1=-1.0, scalar2=None, op0=ALU.mult), reads=[iop], writes=[nio])
            k.op("scalar", lambda e: e.activation(out=mag[:], in_=a[:], func=AF.Exp, scale=nio[:, 0:1]), reads=[a, nio], writes=[mag])
            k.op("vector", lambda e: e.tensor_tensor(out=Wn_re[:], in0=cs2[:], in1=mag[:], op=ALU.mult), reads=[cs2, mag], writes=[Wn_re])
            k.op("vector", lambda e: e.tensor_tensor(out=Wn_im[:], in0=sn2[:], in1=mag[:], op=ALU.mult), reads=[sn2, mag], writes=[Wn_im])
            k.op("vector", lambda e: e.tensor_scalar(out=Wn_im[:], in0=Wn_im[:], scalar1=-1.0, scalar2=None, op0=ALU.mult), reads=[Wn_im], writes=[Wn_im])
            pc = k.sbuf("pc", [128, 3, 8], F32, stack=tst)
            k.dma("sync", pc[:], pcol, writes=[pc])
            k.op("vector", lambda e: e.tensor_scalar(out=pc[:, 0, :], in0=pc[:, 0, :], scalar1=-1e-4, scalar2=None, op0=ALU.min), reads=[pc], writes=[pc])
            k.op("scalar", lambda e: e.activation(out=pc[:, 2, :], in_=pc[:, 2, :], func=AF.Exp), reads=[pc], writes=[pc])
            ac = k.sbuf("ac", [128, 8], F32, stack=tst); tc_ = k.sbuf("tc_", [128, 8], F32, stack=tst)
            k.op("vector", lambda e: e.tensor_tensor(out=ac[:], in0=pc[:, 0, :], in1=pc[:, 2, :], op=ALU.mult), reads=[pc], writes=[ac])
            k.op("vector", lambda e: e.tensor_tensor(out=tc_[:], in0=pc[:, 1, :], in1=pc[:, 2, :], op=ALU.mult), reads=[pc], writes=[tc_])
            k.op("vector", lambda e: e.tensor_scalar(out=tc_[:], in0=tc_[:], scalar1=1.0 / TWO_PI, scalar2=None, op0=ALU.mult), reads=[tc_], writes=[tc_])
            W = L + 1
            tw = k.sbuf("tw", [128, W], F32, stack=tst); twa = k.sbuf("twa", [128, W], F32, stack=tst); twb = k.sbuf("twb", [128, W], F32, stack=tst)
            twi = k.sbuf("twi", [128, W], I32, stack=tst); tws = k.sbuf("tws", [128, W], F32, stack=tst); twc = k.sbuf("twc", [128, W], F32, stack=tst)
            twm = k.sbuf("twm", [128, W], F32, stack=tst)
            for pr in range(8):
                k.op("vector", lambda e: e.tensor_scalar(out=tw[:], in0=iof[:], scalar1=tc_[:, pr:pr + 1], scalar2=None, op0=ALU.mult), reads=[iof, tc_], writes=[tw])
                sincos(k, tw, W, tws, twc, twa, twb, twi)
                k.op("scalar", lambda e: e.activation(out=twm[:], in_=iof[:], func=AF.Exp, scale=ac[:, pr:pr + 1]), reads=[iof, ac], writes=[twm])
                k.op("vector", lambda e: e.tensor_tensor(out=Wp_re[:, pr, :], in0=twc[:], in1=twm[:], op=ALU.mult), reads=[twc, twm], writes=[Wp_re])
                k.op("vector", lambda e: e.tensor_tensor(out=Wp_im[:, pr, :], in0=tws[:], in1=twm[:], op=ALU.mult), reads=[tws, twm], writes=[Wp_im])
            k.barrier()
        psBU = [k.psum(f"psBU{j}", [128, 512], F32) for j in range(4)]
        psC = [k.psum(f"psC{j}", [128, 512], F32) for j in range(3)]
        psY = k.psum("psY", [128, 512], F32)
        V = k.sbuf("V", [128, 8, 2, 128], F32)
        X = [k.sbuf(f"X{i}", [128, 8, 2, 128], F32) for i in range(2)]
        cre = k.sbuf("cre", [128, 8], F32); cim = k.sbuf("cim", [128, 8], F32)
        k.op("vector", lambda e: e.memset(cre[:], 0.0), writes=[cre]); k.op("vector", lambda e: e.memset(cim[:], 0.0), writes=[cim])
        s1 = k.sbuf("s1", [128, 256], F32); s2 = k.sbuf("s2", [128, 256], F32)
        a1 = k.sbuf("a1", [128, 128], F32); a2 = k.sbuf("a2", [128, 128], F32)
        c1 = k.sbuf("c1", [128, 8], F32); c2 = k.sbuf("c2", [128, 8], F32)
        ys = [k.sbuf(f"ys{i}", [128, 256], F32) for i in range(2)]
        ci = 0
        for n in range(NCH):
            ts_ = slice(n * L, (n + 1) * L)
            Xc = X[n % 2]
            for kc in range(2):
                for hf in range(2):
                    P = psBU[kc * 2 + hf]
                    k.op("tensor", lambda e: e.matmul(P[:], lhsT=us[:, kc, ts_], rhs=Bblk[:, kc, hf * 2:hf * 2 + 2, :, :].rearrange("p a r b -> p (a r b)"),
                                                      start=True, stop=True), reads=[us, Bblk], writes=[P])
            for j in range(4):
                P = psBU[j]
                Pv = P[:].rearrange("p (a r b) -> p a r b", a=2, r=2)
                wr = Wn_re[:, j * 256:(j + 1) * 256].rearrange("p (a b) -> p a b", a=2)
                wi = Wn_im[:, j * 256:(j + 1) * 256].rearrange("p (a b) -> p a b", a=2)
                s1v = s1[:].rearrange("p (a b) -> p a b", a=2); s2v = s2[:].rearrange("p (a b) -> p a b", a=2)
                k.op("vector", lambda e: e.tensor_tensor(out=s1v, in0=Pv[:, :, 0, :], in1=wr, op=ALU.mult), reads=[P, Wn_re], writes=[s1])
                k.op("vector", lambda e: e.tensor_tensor(out=s2v, in0=Pv[:, :, 1, :], in1=wi, op=ALU.mult), reads=[P, Wn_im], writes=[s2])
                k.op("vector", lambda e: e.tensor_tensor(out=V[:, 2 * j:2 * j + 2, 0, :], in0=s1v, in1=s2v, op=ALU.subtract), reads=[s1, s2], writes=[V])
                k.op("vector", lambda e: e.tensor_tensor(out=s1v, in0=Pv[:, :, 0, :], in1=wi, op=ALU.mult), reads=[P, Wn_im], writes=[s1])
                k.op("vector", lambda e: e.tensor_tensor(out=s2v, in0=Pv[:, :, 1, :], in1=wr, op=ALU.mult), reads=[P, Wn_re], writes=[s2])
                k.op("vector", lambda e: e.tensor_tensor(out=V[:, 2 * j:2 * j + 2, 1, :], in0=s1v, in1=s2v, op=ALU.add), reads=[s1, s2], writes=[V])
            for pr in range(8):
                PC = psC[ci % 3]; ci += 1
                for ri in range(2):
                    k.op("tensor", lambda e: e.matmul(PC[:, ri * 128:(ri + 1) * 128], lhsT=V[:, pr, ri, :], rhs=triT[:], start=True, stop=True),
                         reads=[V, triT], writes=[PC])
                cr_, cm_ = PC[:, 0:128], PC[:, 128:256]
                wpr = Wp_re[:, pr, 0:L]; wpi = Wp_im[:, pr, 0:L]
                k.op("vector", lambda e: e.scalar_tensor_tensor(out=a1[:], in0=cr_, scalar=cre[:, pr:pr + 1], in1=wpr, op0=ALU.add, op1=ALU.mult), reads=[PC, cre, Wp_re], writes=[a1])
                k.op("vector", lambda e: e.scalar_tensor_tensor(out=a2[:], in0=cm_, scalar=cim[:, pr:pr + 1], in1=wpi, op0=ALU.add, op1=ALU.mult), reads=[PC, cim, Wp_im], writes=[a2])
                k.op("vector", lambda e: e.tensor_tensor(out=Xc[:, pr, 0, :], in0=a1[:], in1=a2[:], op=ALU.subtract), reads=[a1, a2], writes=[Xc])
                k.op("vector", lambda e: e.scalar_tensor_tensor(out=a1[:], in0=cm_, scalar=cim[:, pr:pr + 1], in1=wpr, op0=ALU.add, op1=ALU.mult), reads=[PC, cim, Wp_re], writes=[a1])
                k.op("vector", lambda e: e.scalar_tensor_tensor(out=a2[:], in0=cr_, scalar=cre[:, pr:pr + 1], in1=wpi, op0=ALU.add, op1=ALU.mult), reads=[PC, cre, Wp_im], writes=[a2])
                k.op("vector", lambda e: e.tensor_tensor(out=Xc[:, pr, 1, :], in0=a1[:], in1=a2[:], op=ALU.add), reads=[a1, a2], writes=[Xc])
            xr = Xc[:, :, 0, L - 1]; xi = Xc[:, :, 1, L - 1]
            l_re = Wp_re[:, :, 1]; l_im = Wp_im[:, :, 1]
            k.op("vector", lambda e: e.tensor_tensor(out=c1[:], in0=xr, in1=l_re, op=ALU.mult), reads=[Xc, Wp_re], writes=[c1])
            k.op("vector", lambda e: e.tensor_tensor(out=c2[:], in0=xi, in1=l_im, op=ALU.mult), reads=[Xc, Wp_im], writes=[c2])
            k.op("vector", lambda e: e.tensor_tensor(out=cre[:], in0=c1[:], in1=c2[:], op=ALU.subtract), reads=[c1, c2], writes=[cre])
            k.op("vector", lambda e: e.tensor_tensor(out=c1[:], in0=xr, in1=l_im, op=ALU.mult), reads=[Xc, Wp_im], writes=[c1])
            k.op("vector", lambda e: e.tensor_tensor(out=c2[:], in0=xi, in1=l_re, op=ALU.mult), reads=[Xc, Wp_re], writes=[c2])
            k.op("vector", lambda e: e.tensor_tensor(out=cim[:], in0=c1[:], in1=c2[:], op=ALU.add), reads=[c1, c2], writes=[cim])
            first = True
            for kc in range(2):
                k.op("tensor", lambda e: e.matmul(psY[:, kc * 128:(kc + 1) * 128], lhsT=us[:, kc, ts_], rhs=DD[:, kc, :], start=first, stop=False),
                     reads=[us, DD], writes=[psY])
                first = False
            for pr in range(8):
                k.op("tensor", lambda e: e.matmul(psY[:, pr * 32:(pr + 1) * 32], lhsT=Xc[:, pr, 0, :], rhs=CR[:, pr, :], start=False, stop=False),
                     reads=[Xc, CR], writes=[psY])
                k.op("tensor", lambda e: e.matmul(psY[:, pr * 32:(pr + 1) * 32], lhsT=Xc[:, pr, 1, :], rhs=CI[:, pr, :], start=False, stop=(pr == 7)),
                     reads=[Xc, CI], writes=[psY])
            Y = ys[n % 2]
            k.op("scalar", lambda e: e.activation(out=Y[:], in_=psY[:, 0:256], func=AF.Copy), reads=[psY], writes=[Y])
            k.dma("sync", out[ts_, :], Y[:], reads=[Y], writes=[od])
        k.finish([od])
    return nc


def s5_inputs(proj, p, l):
    ins = []
    for c in range(NCORES):
        b, gh = c // 2, c % 2
        G = slice(gh * 16, gh * 16 + 16)
        lam_re = p["s5_lambda_re"][l][G]; lam_im = p["s5_lambda_im"][l][G]
        ls = np.repeat(p["s5_log_step"][l][G][:, None], 64, 1)
        rows = np.stack([lam_re.reshape(-1), lam_im.reshape(-1), ls.reshape(-1)], 0).astype(np.float32)
        cols = np.stack([a.reshape(8, 128).T for a in (lam_re, lam_im, ls)], 1).astype(np.float32)
        b_re = p["s5_b_re"][l][G]; b_im = p["s5_b_im"][l][G]
        c_re = p["s5_c_re"][l][G]; c_im = p["s5_c_im"][l][G]
        dsk = p["s5_d"][l][G]
        BR = np.zeros((2, 128, 4, 128), np.float32); BI = np.zeros((2, 128, 4, 128), np.float32)
        CR = np.zeros((128, 8, 32), np.float32); CI = np.zeros((128, 8, 32), np.float32)
        DDm = np.zeros((128, 2, 128), np.float32)
        for g in range(16):
            kc, g8 = g // 8, g % 8
            pr4, gl = g8 // 2, g8 % 2
            BR[kc, g8 * 16:(g8 + 1) * 16, pr4, gl * 64:(gl + 1) * 64] = b_re[g].T
            BI[kc, g8 * 16:(g8 + 1) * 16, pr4, gl * 64:(gl + 1) * 64] = b_im[g].T
            pr = g // 2
            CR[gl * 64:(gl + 1) * 64, pr, gl * 16:(gl + 1) * 16] = c_re[g].T
            CI[gl * 64:(gl + 1) * 64, pr, gl * 16:(gl + 1) * 16] = c_im[g].T
            for h in range(16):
                DDm[g8 * 16 + h, kc, g8 * 16 + h] = dsk[g, h]
        ins.append({"uT": _c(proj[b][:, gh * 256:(gh + 1) * 256].T), "prow": _c(rows), "pcol": _c(cols),
                    "BRc": _c(BR.reshape(2, 128, 512)), "BIc": _c(BI.reshape(2, 128, 512)),
                    "CRp": CR, "CIp": CI, "Dd": DDm})
    return ins


def s5_gather(res):
    y = np.zeros((BATCH, SEQ, 512), np.float32)
    for c in range(NCORES):
        b, gh = c // 2, c % 2
        y[b, :, gh * 256:(gh + 1) * 256] = res[c]["y"]
    return y


def build_gdn():
    S = SEQ
    C = 64
    NCK = S // C
    nc = new_nc()
    di = lambda n, shp, dt=F32: nc.dram_tensor(n, shp, dt, kind="ExternalInput").ap()
    qkvT = di("qkvT", [768, S]); convw = di("convw", [128, 6, 4])
    z = di("z", [S, 256]); garow = di("garow", [2, S]); gbrow = di("gbrow", [2, S])
    hp_ = di("hp", [1, 4])
    gnorm = di("gnorm", [1, 128])
    out = nc.dram_tensor("o", [S, 256], F32, kind="ExternalOutput").ap()
    with ExitStack() as st:
        k = MK(nc, st)
        od = k.view("od", out)
        ps = [k.psum(f"ps{i}", [128, 512], F32) for i in range(8)]
        ident = make_ident(k, 128, F32)
        ones_f = k.sbuf("ones_f", [128, 128], F32)
        k.op("vector", lambda e: e.memset(ones_f[:], 1.0), writes=[ones_f])
        m_incl = make_tri(k, "m_incl", C, dt=F32)
        m_strict = make_tri(k, "m_strict", C, dt=F32, strict=True)
        negm = k.sbuf("negm", [C, C], F32)
        k.op("vector", lambda e: e.tensor_scalar(out=negm[:], in0=m_incl[:], scalar1=-1.0, scalar2=30000.0, op0=ALU.add, op1=ALU.mult),
             reads=[m_incl], writes=[negm])
        cw = k.sbuf("cw", [128, 6, 4], F32); k.dma("sync", cw[:], convw, writes=[cw])
        hp = k.sbuf("hps", [1, 4], F32); k.dma("sync", hp[:], hp_, writes=[hp])
        gn = k.sbuf("gn", [C, 128], F32); k.dma("sync", gn[:], gnorm.to_broadcast([C, 128]), writes=[gn])
        eps6 = k.sbuf("eps6", [128, 1], F32); k.op("vector", lambda e: e.memset(eps6[:], 1e-6), writes=[eps6])
        k.op("scalar", lambda e: e.activation(out=hp[:, 0:2], in_=hp[:, 0:2], func=AF.Exp), reads=[hp], writes=[hp])
        xt = k.sbuf("xt", [128, S + 3], F32)
        y = k.sbuf("yqkv", [128, 3, S], F32)
        Bb = k.sbuf("Bb", [128, S], F32); Beg = k.sbuf("Beg", [128, S], F32); Bend = k.sbuf("Bend", [128, S], F32)
        zc = [k.sbuf(f"zc{i}", [C, 128], F32) for i in range(2)]
        rA = k.sbuf("rA", [1, S], F32); rB = k.sbuf("rB", [1, S], F32); rC = k.sbuf("rC", [1, S], F32)
        negones = k.sbuf("negones", [1, C], F32)
        k.op("vector", lambda e: e.memset(negones[:], -1.0), writes=[negones])
        sqt = k.sbuf("sqt", [128, 512], F32); rst = k.sbuf("rst", [128, 512], F32)
        Sst = k.sbuf("Sst", [128, 128], F32)
        kb = k.sbuf("kb", [128, C], F32); kbg = k.sbuf("kbg", [128, C], F32); qd = k.sbuf("qd", [128, C], F32)
        ke = k.sbuf("ke", [128, C], F32); vb = k.sbuf("vbT", [128, C], F32)
        tok = k.sbuf("tok", [C, 3, 128], F32)
        decT = k.sbuf("decT", [C, C], F32); decTs = k.sbuf("decTs", [C, C], F32); AT = k.sbuf("AT", [C, C], F32)
        Pk = [k.sbuf(f"Pk{i}", [C, C], F32) for i in range(2)]; PTk = [k.sbuf(f"PTk{i}", [C, C], F32) for i in range(2)]
        TT = k.sbuf("TT", [C, C], F32)
        nwT = k.sbuf("nwT", [128, C], F32); vnew = k.sbuf("vnew", [C, 128], F32)
        osb = [k.sbuf(f"osb{i}", [C, 128], F32) for i in range(2)]
        o2 = k.sbuf("o2", [C, 128], F32); ssq = k.sbuf("ssq", [C, 1], F32)
        for j in range(2):
            for t3 in range(3):
                ti = j * 3 + t3
                k.op("vector", lambda e: e.memset(xt[:, 0:3], 0.0), writes=[xt])
                k.dma("sync", xt[:, 3:S + 3], qkvT[ti * 128:(ti + 1) * 128, :], writes=[xt])
                Y = y[:, t3, :]
                k.op("vector", lambda e: e.tensor_scalar(out=Y, in0=xt[:, 3:S + 3], scalar1=cw[:, ti, 3:4], scalar2=None, op0=ALU.mult), reads=[xt, cw], writes=[y])
                for tap in range(3):
                    k.op("vector", lambda e: e.scalar_tensor_tensor(out=Y, in0=xt[:, tap:S + tap], scalar=cw[:, ti, tap:tap + 1], in1=Y, op0=ALU.mult, op1=ALU.add),
                         reads=[xt, cw, y], writes=[y])
                k.op("scalar", lambda e: e.activation(out=Y, in_=Y, func=AF.Silu), reads=[y], writes=[y])
                if t3 < 2:
                    for tb in range(S // 512):
                        sl = slice(tb * 512, (tb + 1) * 512)
                        k.op("scalar", lambda e: e.activation(out=sqt[:], in_=y[:, t3, sl], func=AF.Square), reads=[y], writes=[sqt])
                        k.op("tensor", lambda e: e.matmul(ps[0][:], lhsT=ones_f[:], rhs=sqt[:], start=True, stop=True), reads=[ones_f, sqt], writes=[ps[0]])
                        k.op("scalar", lambda e: e.activation(out=rst[:], in_=ps[0][:], func=AF.Sqrt, bias=eps6[:, 0:1], scale=1.0), reads=[ps[0], eps6], writes=[rst])
                        k.op("vector", lambda e: e.reciprocal(out=rst[:], in_=rst[:]), reads=[rst], writes=[rst])
                        if t3 == 0:
                            k.op("vector", lambda e: e.scalar_tensor_tensor(out=y[:, t3, sl], in0=y[:, t3, sl], scalar=128.0 ** -0.5, in1=rst[:], op0=ALU.mult, op1=ALU.mult),
                                 reads=[y, rst], writes=[y])
                        else:
                            k.op("vector", lambda e: e.tensor_tensor(out=y[:, t3, sl], in0=y[:, t3, sl], in1=rst[:], op=ALU.mult), reads=[y, rst], writes=[y])
            qT, kT, vT = y[:, 0, :], y[:, 1, :], y[:, 2, :]
            k.dma("sync", rA[:], garow[j:j + 1, :], writes=[rA])
            k.op("scalar", lambda e: e.activation(out=rA[:], in_=rA[:], func=AF.Exp, bias=hp[0:1, 2 + j:3 + j], scale=1.0), reads=[rA, hp], writes=[rA])
            k.op("vector", lambda e: e.tensor_scalar(out=rA[:], in0=rA[:], scalar1=1.0, scalar2=None, op0=ALU.add), reads=[rA], writes=[rA])
            k.op("scalar", lambda e: e.activation(out=rA[:], in_=rA[:], func=AF.Ln), reads=[rA], writes=[rA])
            k.op("vector", lambda e: e.tensor_scalar(out=rA[:], in0=rA[:], scalar1=hp[0:1, j:j + 1], scalar2=-1.0, op0=ALU.mult, op1=ALU.mult), reads=[rA, hp], writes=[rA])
            k.op("vector", lambda e: e.memset(rC[:], 1.0), writes=[rC])
            k.op("vector", lambda e: e.memset(rC[:].rearrange("p (n c) -> p n c", c=C)[:, :, 0:1], 0.0), writes=[rC])
            k.op("vector", lambda e: e.tensor_tensor_scan(out=rB[:], data0=rC[:], data1=rA[:], initial=0.0, op0=ALU.mult, op1=ALU.add), reads=[rA, rC], writes=[rB])

            def bcast(row, B):
                for tb in range(S // 512):
                    sl = slice(tb * 512, (tb + 1) * 512)
                    P = ps[1 + tb % 2]
                    k.op("tensor", lambda e: e.matmul(P[:], lhsT=ones_f[0:1, :], rhs=row[0:1, sl], start=True, stop=True), reads=[ones_f, row], writes=[P])
                    k.op("scalar", lambda e: e.activation(out=B[:, sl], in_=P[:], func=AF.Copy), reads=[P], writes=[B])

            k.dma("sync", rA[:], gbrow[j:j + 1, :], writes=[rA])
            k.op("scalar", lambda e: e.activation(out=rA[:], in_=rA[:], func=AF.Sigmoid), reads=[rA], writes=[rA])
            bcast(rA, Bb)
            for n in range(NCK):
                cs = slice(n * C, (n + 1) * C)
                last = n * C + C - 1
                k.op("vector", lambda e: e.tensor_scalar(out=rC[0:1, cs], in0=rB[0:1, cs], scalar1=rB[0:1, last:last + 1], scalar2=-1.0,
                                                         op0=ALU.subtract, op1=ALU.mult), reads=[rB], writes=[rC])
            k.op("scalar", lambda e: e.activation(out=rC[:], in_=rC[:], func=AF.Exp), reads=[rC], writes=[rC])
            bcast(rC, Bend)
            k.op("scalar", lambda e: e.activation(out=rC[:], in_=rB[:], func=AF.Exp), reads=[rB], writes=[rC])
            bcast(rC, Beg)
            k.op("vector", lambda e: e.memset(Sst[:], 0.0), writes=[Sst])
            for n in range(NCK):
                cs = slice(n * C, (n + 1) * C)
                last = n * C + C - 1
                k.op("vector", lambda e: e.tensor_tensor(out=kb[:], in0=kT[:, cs], in1=Bb[:, cs], op=ALU.mult), reads=[y, Bb], writes=[kb])
                k.op("vector", lambda e: e.tensor_tensor(out=kbg[:], in0=kb[:], in1=Beg[:, cs], op=ALU.mult), reads=[kb, Beg], writes=[kbg])
                k.op("vector", lambda e: e.tensor_tensor(out=qd[:], in0=qT[:, cs], in1=Beg[:, cs], op=ALU.mult), reads=[y, Beg], writes=[qd])
                k.op("vector", lambda e: e.tensor_tensor(out=ke[:], in0=kT[:, cs], in1=Bend[:, cs], op=ALU.mult), reads=[y, Bend], writes=[ke])
                k.op("vector", lambda e: e.tensor_tensor(out=vb[:], in0=vT[:, cs], in1=Bb[:, cs], op=ALU.mult), reads=[y, Bb], writes=[vb])
                PD = ps[3]
                k.op("tensor", lambda e: e.matmul(PD[0:C, 0:C], lhsT=ones_f[0:1, 0:C], rhs=rB[0:1, cs], start=True, stop=False), reads=[ones_f, rB], writes=[PD])
                k.op("tensor", lambda e: e.matmul(PD[0:C, 0:C], lhsT=rB[0:1, cs], rhs=negones[0:1, 0:C], start=False, stop=False), reads=[negones, rB], writes=[PD])
                k.op("tensor", lambda e: e.matmul(PD[0:C, 0:C], lhsT=ident[0:C, 0:C], rhs=negm[:], start=False, stop=True), reads=[ident, negm], writes=[PD])
                k.op("scalar", lambda e: e.activation(out=decT[:], in_=PD[0:C, 0:C], func=AF.Exp), reads=[PD], writes=[decT])
                k.op("vector", lambda e: e.tensor_tensor(out=decTs[:], in0=decT[:], in1=m_strict[:], op=ALU.mult), reads=[decT, m_strict], writes=[decTs])
                PK = ps[4]
                k.op("tensor", lambda e: e.matmul(PK[0:C, 0:C], lhsT=kT[:, cs], rhs=kb[:], start=True, stop=True), reads=[y, kb], writes=[PK])
                k.op("tensor", lambda e: e.matmul(PK[0:C, C:2 * C], lhsT=kT[:, cs], rhs=qT[:, cs], start=True, stop=True), reads=[y], writes=[PK])
                k.op("vector", lambda e: e.scalar_tensor_tensor(out=PTk[0][:], in0=PK[0:C, 0:C], scalar=-1.0, in1=decTs[:], op0=ALU.mult, op1=ALU.mult),
                     reads=[PK, decTs], writes=[PTk[0]])
                k.op("vector", lambda e: e.tensor_tensor(out=AT[:], in0=PK[0:C, C:2 * C], in1=decT[:], op=ALU.mult), reads=[PK, decT], writes=[AT])
                PX = ps[5]
                k.op("tensor", lambda e: e.transpose(out=PX[0:C, 0:C], in_=PTk[0][:], identity=ident[0:C, 0:C]), reads=[PTk[0], ident], writes=[PX])
                k.op("scalar", lambda e: e.activation(out=Pk[0][:], in_=PX[0:C, 0:C], func=AF.Copy), reads=[PX], writes=[Pk[0]])
                k.op("vector", lambda e: e.tensor_tensor(out=TT[:], in0=PTk[0][:], in1=ident[0:C, 0:C], op=ALU.add), reads=[PTk[0], ident], writes=[TT])
                for lv in range(1, 6):
                    a_, b_ = (lv - 1) % 2, lv % 2
                    PP = ps[6]
                    k.op("tensor", lambda e: e.matmul(PP[0:C, 0:C], lhsT=PTk[a_][:], rhs=Pk[a_][:], start=True, stop=True), reads=[PTk[a_], Pk[a_]], writes=[PP])
                    if lv < 5:
                        k.op("tensor", lambda e: e.matmul(PP[0:C, C:2 * C], lhsT=Pk[a_][:], rhs=PTk[a_][:], start=True, stop=True), reads=[PTk[a_], Pk[a_]], writes=[PP])
                    k.op("scalar", lambda e: e.activation(out=Pk[b_][:], in_=PP[0:C, 0:C], func=AF.Copy), reads=[PP], writes=[Pk[b_]])
                    if lv < 5:
                        k.op("vector", lambda e: e.tensor_copy(out=PTk[b_][:], in_=PP[0:C, C:2 * C]), reads=[PP], writes=[PTk[b_]])
                    PU = ps[7]
                    k.op("tensor", lambda e: e.matmul(PU[0:C, 0:C], lhsT=Pk[b_][:], rhs=TT[:], start=True, stop=True), reads=[Pk[b_], TT], writes=[PU])
                    k.op("vector", lambda e: e.tensor_tensor(out=TT[:], in0=TT[:], in1=PU[0:C, 0:C], op=ALU.add), reads=[TT, PU], writes=[TT])
                for (ii, src) in ((0, vb), (1, kbg), (2, ke)):
                    PT_ = ps[1 + ii % 2]
                    k.op("tensor", lambda e: e.transpose(out=PT_[0:C, 0:128], in_=src[:], identity=ident[:]), reads=[src, ident], writes=[PT_])
                    k.op("scalar", lambda e: e.activation(out=tok[:, ii, :], in_=PT_[0:C, 0:128], func=AF.Copy), reads=[PT_], writes=[tok])
                PW = ps[3]
                k.op("tensor", lambda e: e.matmul(PW[:, 0:C], lhsT=tok[:, 1, :], rhs=TT[:], start=True, stop=True), reads=[tok, TT], writes=[PW])
                k.op("vector", lambda e: e.tensor_scalar(out=nwT[:], in0=PW[:, 0:C], scalar1=-1.0, scalar2=None, op0=ALU.mult), reads=[PW], writes=[nwT])
                PV = ps[4]
                k.op("tensor", lambda e: e.matmul(PV[0:C, 0:128], lhsT=TT[:], rhs=tok[:, 0, :], start=True, stop=False), reads=[TT, tok], writes=[PV])
                k.op("tensor", lambda e: e.matmul(PV[0:C, 0:128], lhsT=nwT[:], rhs=Sst[:], start=False, stop=True), reads=[nwT, Sst], writes=[PV])
                k.op("scalar", lambda e: e.activation(out=vnew[:], in_=PV[0:C, 0:128], func=AF.Copy), reads=[PV], writes=[vnew])
                PO = ps[5]
                k.op("tensor", lambda e: e.matmul(PO[0:C, 0:128], lhsT=qd[:], rhs=Sst[:], start=True, stop=False), reads=[qd, Sst], writes=[PO])
                k.op("tensor", lambda e: e.matmul(PO[0:C, 0:128], lhsT=AT[:], rhs=vnew[:], start=False, stop=True), reads=[AT, vnew], writes=[PO])
                PS_ = ps[6]
                k.op("tensor", lambda e: e.matmul(PS_[:, 0:128], lhsT=tok[:, 2, :], rhs=vnew[:], start=True, stop=True), reads=[tok, vnew], writes=[PS_])
                k.op("vector", lambda e: e.scalar_tensor_tensor(out=Sst[:], in0=Sst[:], scalar=Beg[:, last:last + 1], in1=PS_[:, 0:128], op0=ALU.mult, op1=ALU.add),
                     reads=[Sst, Beg, PS_], writes=[Sst])
                OS = osb[n % 2]
                k.op("scalar", lambda e: e.activation(out=o2[:], in_=PO[0:C, 0:128], func=AF.Square, accum_out=ssq[:, 0:1]), reads=[PO], writes=[o2, ssq])
                k.op("scalar", lambda e: e.activation(out=ssq[:], in_=ssq[:], func=AF.Sqrt, bias=eps6[0:C, 0:1], scale=1.0 / 128), reads=[ssq, eps6], writes=[ssq])
                k.op("vector", lambda e: e.reciprocal(out=ssq[:], in_=ssq[:]), reads=[ssq], writes=[ssq])
                k.op("vector", lambda e: e.scalar_tensor_tensor(out=OS[:], in0=PO[0:C, 0:128], scalar=ssq[:, 0:1], in1=gn[:], op0=ALU.mult, op1=ALU.mult),
                     reads=[PO, ssq, gn], writes=[OS])
                ZC = zc[n % 2]
                k.dma("sync", ZC[:], z[cs, j * 128:(j + 1) * 128], writes=[ZC])
                k.op("scalar", lambda e: e.activation(out=ZC[:], in_=ZC[:], func=AF.Silu), reads=[ZC], writes=[ZC])
                k.op("vector", lambda e: e.tensor_tensor(out=OS[:], in0=OS[:], in1=ZC[:], op=ALU.mult), reads=[OS, ZC], writes=[OS])
                k.dma("sync", out[cs, j * 128:(j + 1) * 128], OS[:], reads=[OS], writes=[od])
        k.finish([od])
    return nc


def gdn_inputs(proj, p, l):
    ins = []
    cwf = p["gdn_conv"][l]
    for c in range(NCORES):
        b, hh = c // 2, c % 2
        pb = proj[b]
        tiles = []; cws = []
        for j in range(2):
            H = 2 * hh + j
            for base, off in ((C_GQ, 0), (C_GK, 512), (C_GV, 1024)):
                tiles.append(pb[:, base + H * 128: base + (H + 1) * 128].T)
                cws.append(cwf[:, off + H * 128: off + (H + 1) * 128].T)
        ins.append({
            "qkvT": _c(np.concatenate(tiles, 0)), "convw": _c(np.stack(cws, 1)),
            "z": _c(pb[:, C_GZ + hh * 256: C_GZ + (hh + 1) * 256]),
            "garow": _c(pb[:, C_GA + 2 * hh: C_GA + 2 * hh + 2].T), "gbrow": _c(pb[:, C_GB + 2 * hh: C_GB + 2 * hh + 2].T),
            "hp": _c(np.concatenate([p["gdn_a_log"][l][2 * hh:2 * hh + 2], p["gdn_dt_bias"][l][2 * hh:2 * hh + 2]])[None].astype(np.float32)),
            "gnorm": _c(p["gdn_out_norm"][l][None]),
        })
    return ins


def gdn_gather(res):
    y = np.zeros((BATCH, SEQ, 512), np.float32)
    for c in range(NCORES):
        b, hh = c // 2, c % 2
        y[b, :, hh * 256:(hh + 1) * 256] = res[c]["o"]
    return y


def ln_setup(k, g_dram, b_dram):
    gB = k.sbuf("ln_gB", [128, D], F32); bB = k.sbuf("ln_bB", [128, D], F32)
    k.dma("sync", gB[:], g_dram.to_broadcast([128, D]), writes=[gB])
    k.dma("sync", bB[:], b_dram.to_broadcast([128, D]), writes=[bB])
    st_ = {"gB": gB, "bB": bB,
           "junk": k.sbuf("ln_junk", [128, D], F32),
           "s1": k.sbuf("ln_s1", [128, 1], F32), "s2": k.sbuf("ln_s2", [128, 1], F32),
           "m2": k.sbuf("ln_m2", [128, 1], F32), "eps": k.sbuf("ln_eps", [128, 1], F32)}
    k.op("vector", lambda e: e.memset(st_["eps"][:], 1e-5), writes=[st_["eps"]])
    return st_


def ln_rows(k, L, r, dst):
    junk, s1, s2, m2 = L["junk"], L["s1"], L["s2"], L["m2"]
    k.op("scalar", lambda e: e.activation(out=junk[:], in_=r[:], func=AF.Copy, accum_out=s1[:, 0:1]), reads=[r], writes=[junk, s1])
    k.op("scalar", lambda e: e.activation(out=junk[:], in_=r[:], func=AF.Square, accum_out=s2[:, 0:1]), reads=[r], writes=[junk, s2])
    k.op("vector", lambda e: e.tensor_scalar(out=s1[:], in0=s1[:], scalar1=1.0 / D, scalar2=None, op0=ALU.mult), reads=[s1], writes=[s1])
    k.op("vector", lambda e: e.tensor_tensor(out=m2[:], in0=s1[:], in1=s1[:], op=ALU.mult), reads=[s1], writes=[m2])
    k.op("vector", lambda e: e.scalar_tensor_tensor(out=s2[:], in0=s2[:], scalar=1.0 / D, in1=m2[:], op0=ALU.mult, op1=ALU.subtract), reads=[s2, m2], writes=[s2])
    k.op("scalar", lambda e: e.activation(out=s2[:], in_=s2[:], func=AF.Sqrt, bias=L["eps"][:, 0:1], scale=1.0), reads=[s2, L["eps"]], writes=[s2])
    k.op("vector", lambda e: e.reciprocal(out=s2[:], in_=s2[:]), reads=[s2], writes=[s2])
    k.op("vector", lambda e: e.tensor_scalar(out=junk[:], in0=r[:], scalar1=s1[:, 0:1], scalar2=s2[:, 0:1], op0=ALU.subtract, op1=ALU.mult), reads=[r, s1, s2], writes=[junk])
    k.op("vector", lambda e: e.tensor_tensor(out=junk[:], in0=junk[:], in1=L["gB"][:], op=ALU.mult), reads=[junk, L["gB"]], writes=[junk])
    k.op("vector", lambda e: e.tensor_tensor(out=dst[:], in0=junk[:], in1=L["bB"][:], op=ALU.add), reads=[junk, L["bB"]], writes=[dst])


def build_mixout():
    T = TPC
    nc = new_nc()
    di = lambda n, shp, dt=F32: nc.dram_tensor(n, shp, dt, kind="ExternalInput").ap()
    s5T = di("s5T", [512, T]); mlaT = di("mlaT", [1024, T]); gdnT = di("gdnT", [512, T]); x = di("x", [T, D])
    wglu = di("wglu", [512, 512]); bglu = di("bglu", [128, 4]); gs5 = di("gs5", [128, 4]); gmla = di("gmla", [128, 8])
    wout = di("wout", [D, D]); lng = di("lng", [1, D]); lnb = di("lnb", [1, D])
    out = nc.dram_tensor("x1", [T, D], F32, kind="ExternalOutput").ap()
    with ExitStack() as st:
        k = MK(nc, st)
        od = k.view("od", out)
        ps = [k.psum(f"ps{i}", [128, 512], F32) for i in range(8)]
        ones_f = k.sbuf("ones_f", [128, 128], F32)
        k.op("vector", lambda e: e.memset(ones_f[:], 1.0), writes=[ones_f])
        eps6 = k.sbuf("eps6", [128, 1], F32); k.op("vector", lambda e: e.memset(eps6[:], 1e-6), writes=[eps6])
        Wout = k.sbuf("Wout", [128, 16, D], BF16)
        k.dma("gpsimd", Wout[:], wout.rearrange("(c p) n -> p c n", p=128), writes=[Wout])
        Wglu = k.sbuf("Wglu", [128, 4, 512], BF16)
        k.dma("gpsimd", Wglu[:], wglu.rearrange("(c p) n -> p c n", p=128), writes=[Wglu])
        bg = k.sbuf("bg", [128, 4], F32); g5 = k.sbuf("g5", [128, 4], F32); gm = k.sbuf("gm", [128, 8], F32)
        k.dma("sync", bg[:], bglu, writes=[bg]); k.dma("sync", g5[:], gs5, writes=[g5]); k.dma("sync", gm[:], gmla, writes=[gm])
        L = ln_setup(k, lng, lnb)
        cat = k.sbuf("cat", [128, 16, 512], BF16)
        a = k.sbuf("a", [128, 4, 512], F32); xin = k.sbuf("xin", [128, 4, 512], F32); yb = k.sbuf("yb", [128, 4, 512], BF16)
        y2 = k.sbuf("y2", [128, 4, 512], F32)
        mi = k.sbuf("mi", [128, 8, 512], F32)
        rstd = k.sbuf("rstd", [128, 512], F32)
        xt = [k.sbuf(f"xt{i}", [128, D], F32) for i in range(2)]
        r = [k.sbuf(f"r{i}", [128, D], F32) for i in range(2)]
        for tb in range(T // 512):
            sl = slice(tb * 512, (tb + 1) * 512)
            k.dma("sync", xin[:], s5T[:, sl].rearrange("(c p) t -> p c t", p=128), writes=[xin])
            k.op("scalar", lambda e: e.activation(out=a[:], in_=xin[:], func=AF.Square), reads=[xin], writes=[a])
            k.op("vector", lambda e: e.tensor_scalar(out=a[:], in0=a[:], scalar1=0.044715, scalar2=1.0, op0=ALU.mult, op1=ALU.add), reads=[a], writes=[a])
            k.op("vector", lambda e: e.tensor_tensor(out=a[:], in0=a[:], in1=xin[:], op=ALU.mult), reads=[a, xin], writes=[a])
            k.op("scalar", lambda e: e.activation(out=a[:], in_=a[:], func=AF.Tanh, scale=0.7978845608028654), reads=[a], writes=[a])
            k.op("vector", lambda e: e.tensor_scalar(out=a[:], in0=a[:], scalar1=1.0, scalar2=0.5, op0=ALU.add, op1=ALU.mult), reads=[a], writes=[a])
            k.op("vector", lambda e: e.tensor_tensor(out=a[:], in0=a[:], in1=xin[:], op=ALU.mult), reads=[a, xin], writes=[a])
            k.op("vector", lambda e: e.tensor_copy(out=yb[:], in_=a[:]), reads=[a], writes=[yb])
            for oc in range(4):
                P = ps[oc]
                for kc in range(4):
                    k.op("tensor", lambda e: e.matmul(P[:], lhsT=Wglu[:, kc, oc * 128:(oc + 1) * 128], rhs=yb[:, kc, :], start=(kc == 0), stop=(kc == 3)),
                         reads=[Wglu, yb], writes=[P])
                k.op("scalar", lambda e: e.activation(out=y2[:, oc, :], in_=P[:], func=AF.Sigmoid, bias=bg[:, oc:oc + 1], scale=1.0), reads=[P, bg], writes=[y2])
            k.op("vector", lambda e: e.tensor_tensor(out=y2[:], in0=y2[:], in1=a[:], op=ALU.mult), reads=[y2, a], writes=[y2])
            k.op("scalar", lambda e: e.activation(out=a[:], in_=y2[:], func=AF.Square), reads=[y2], writes=[a])
            P = ps[4]
            for kc in range(4):
                k.op("tensor", lambda e: e.matmul(P[:], lhsT=ones_f[:], rhs=a[:, kc, :], start=(kc == 0), stop=(kc == 3)), reads=[ones_f, a], writes=[P])
            k.op("scalar", lambda e: e.activation(out=rstd[:], in_=P[:], func=AF.Sqrt, bias=eps6[:, 0:1], scale=1.0 / 512), reads=[P, eps6], writes=[rstd])
            k.op("vector", lambda e: e.reciprocal(out=rstd[:], in_=rstd[:]), reads=[rstd], writes=[rstd])
            for kc in range(4):
                k.op("vector", lambda e: e.scalar_tensor_tensor(out=cat[:, kc, :], in0=y2[:, kc, :], scalar=g5[:, kc:kc + 1], in1=rstd[:], op0=ALU.mult, op1=ALU.mult),
                     reads=[y2, g5, rstd], writes=[cat])
            k.dma("sync", mi[:], mlaT[:, sl].rearrange("(c p) t -> p c t", p=128), writes=[mi])
            P = ps[5]
            for kc in range(8):
                k.op("scalar", lambda e: e.activation(out=a[:, kc % 4, :], in_=mi[:, kc, :], func=AF.Square), reads=[mi], writes=[a])
                k.op("tensor", lambda e: e.matmul(P[:], lhsT=ones_f[:], rhs=a[:, kc % 4, :], start=(kc == 0), stop=(kc == 7)), reads=[ones_f, a], writes=[P])
            k.op("scalar", lambda e: e.activation(out=rstd[:], in_=P[:], func=AF.Sqrt, bias=eps6[:, 0:1], scale=1.0 / 1024), reads=[P, eps6], writes=[rstd])
            k.op("vector", lambda e: e.reciprocal(out=rstd[:], in_=rstd[:]), reads=[rstd], writes=[rstd])
            for kc in range(8):
                k.op("vector", lambda e: e.scalar_tensor_tensor(out=cat[:, 4 + kc, :], in0=mi[:, kc, :], scalar=gm[:, kc:kc + 1], in1=rstd[:], op0=ALU.mult, op1=ALU.mult),
                     reads=[mi, gm, rstd], writes=[cat])
            k.dma("gpsimd", cat[:, 12:16, :], gdnT[:, sl].rearrange("(c p) t -> p c t", p=128), writes=[cat])
            for tt in range(4):
                t0 = tb * 512 + tt * 128
                X = xt[tt % 2]; R = r[tt % 2]
                k.dma("sync", X[:], x[t0:t0 + 128, :], writes=[X])
                for nb in range(4):
                    P = ps[nb]
                    for kc in range(16):
                        k.op("tensor", lambda e: e.matmul(P[:], lhsT=cat[:, kc, tt * 128:(tt + 1) * 128], rhs=Wout[:, kc, nb * 512:(nb + 1) * 512],
                                                          start=(kc == 0), stop=(kc == 15)), reads=[cat, Wout], writes=[P])
                    k.op("vector", lambda e: e.scalar_tensor_tensor(out=R[:, nb * 512:(nb + 1) * 512], in0=X[:, nb * 512:(nb + 1) * 512], scalar=DN_ALPHA, in1=P[:],
                                                                    op0=ALU.mult, op1=ALU.add), reads=[X, P], writes=[R])
                ln_rows(k, L, R, X)
                k.dma("sync", out[t0:t0 + 128, :], X[:], reads=[X], writes=[od])
        k.finish([od])
    return nc


def mixout_inputs(x, s5scan, o_mla, y_gdn, p, l):
    ins = []
    xt = x.reshape(-1, D); s5 = s5scan.reshape(-1, 512); om = o_mla.reshape(-1, 1024); yg = y_gdn.reshape(-1, 512)
    for c in range(NCORES):
        ts = slice(c * TPC, (c + 1) * TPC)
        ins.append({"s5T": _c(s5[ts].T), "mlaT": _c(om[ts].T), "gdnT": _c(yg[ts].T), "x": _c(xt[ts]),
                    "wglu": _c(p["s5_w_glu"][l]), "bglu": _c(p["s5_b_glu"][l].reshape(4, 128).T), "gs5": _c(p["s5_out_norm"][l].reshape(4, 128).T),
                    "gmla": _c(p["mla_out_norm"][l].reshape(8, 128).T), "wout": _c(p["w_out"][l]),
                    "lng": _c(p["ln1_g"][l][None]), "lnb": _c(p["ln1_b"][l][None])})
    return ins


XA_SCALE = 128.0 ** -0.5


def build_xattn(stage=9):
    T = TPC
    nc = new_nc()
    di = lambda n, shp, dt=F32: nc.dram_tensor(n, shp, dt, kind="ExternalInput").ap()
    x1T = di("x1T", [D, T]); x1 = di("x1", [T, D]); memT = di("memT", [D, 256])
    wq = di("wq", [D, 512]); wk = di("wk", [D, 512]); wv = di("wv", [D, 512]); wo = di("wo", [512, D])
    lng = di("lng", [1, D]); lnb = di("lnb", [1, D])
    out = nc.dram_tensor("x2", [T, D], F32, kind="ExternalOutput").ap()
    with ExitStack() as st:
        k = MK(nc, st, same_engine_sync=(stage != 1.5))
        od = k.view("od", out)
        ps = [k.psum(f"ps{i}", [128, 512], F32) for i in range(8)]
        ones_f = k.sbuf("ones_f", [128, 128], F32); k.op("vector", lambda e: e.memset(ones_f[:], 1.0), writes=[ones_f])
        ones_b = k.sbuf("ones_b", [128, 128], BF16); k.op("vector", lambda e: e.memset(ones_b[:], 1.0), writes=[ones_b])
        XT = k.sbuf("XT", [128, 16, T], BF16)
        k.dma("gpsimd", XT[:], x1T.rearrange("(c p) t -> p c t", p=128), writes=[XT])
        L = ln_setup(k, lng, lnb)
        KT = k.sbuf("KT", [128, 4, 256], BF16); V = k.sbuf("V", [128, 2, 512], BF16)
        kmx = k.sbuf("kmx", [128, 4], F32)
        krow = [k.sbuf(f"krow{h}", [1, 128], BF16) for h in range(4)]
        arow = [k.sbuf(f"arow{h}", [1, 512], BF16) for h in range(4)]
        sq = k.sbuf("sq", [128, 512], F32)
        with ExitStack() as tst:
            Wk = k.sbuf("Wk", [128, 16, 512], BF16, stack=tst); k.dma("gpsimd", Wk[:], wk.rearrange("(c p) n -> p c n", p=128), writes=[Wk])
            Wv = k.sbuf("Wv", [128, 16, 512], BF16, stack=tst); k.dma("gpsimd", Wv[:], wv.rearrange("(c p) n -> p c n", p=128), writes=[Wv])
            MT = k.sbuf("MT", [128, 16, 256], BF16, stack=tst); k.dma("gpsimd", MT[:], memT.rearrange("(c p) t -> p c t", p=128), writes=[MT])
            if stage == 0:
                k.barrier(); k.dma("sync", out[0:128, 0:128], ones_f[:], reads=[ones_f], writes=[od]); k.finish([od]); return nc
            for hp2 in ((1,) if stage == 1.6 else (0, 0) if stage == 1.7 else range(2)):
                P = ps[hp2]
                for hh in range(2):
                    h = 2 * hp2 + hh
                    for kc in range(16):
                        k.op("tensor", lambda e: e.matmul(P[:, hh * 256:(hh + 1) * 256], lhsT=Wk[:, kc, h * 128:(h + 1) * 128], rhs=MT[:, kc, :],
                                                          start=(kc == 0), stop=(kc == 15)), reads=[Wk, MT], writes=[P])
                k.op("vector", lambda e: e.tensor_copy(out=KT[:, 2 * hp2:2 * hp2 + 2, :], in_=P[:].rearrange("p (a b) -> p a b", a=2)), reads=[P], writes=[KT])
                if stage == 0.55:
                    k.barrier(); k.dma("sync", out[0:128, 0:128], ones_f[:], reads=[ones_f], writes=[od]); k.finish([od]); return nc
                KTv = KT[:, 2 * hp2:2 * hp2 + 2, :].rearrange("p a b -> p (a b)")
                k.op("vector", lambda e: e.tensor_tensor(out=sq[:], in0=KTv, in1=KTv, op=ALU.mult), reads=[KT], writes=[sq])
                if stage == 0.6:
                    k.barrier(); k.dma("sync", out[0:128, 0:128], ones_f[:], reads=[ones_f], writes=[od]); k.finish([od]); return nc
                P2 = ps[4]
                k.op("tensor", lambda e: e.matmul(P2[:], lhsT=ones_f[:], rhs=sq[:], start=True, stop=True), reads=[ones_f, sq], writes=[P2])
                if stage == 0.7:
                    k.barrier(); k.dma("sync", out[0:128, 0:128], ones_f[:], reads=[ones_f], writes=[od]); k.finish([od]); return nc
                for hh in range(2):
                    h = 2 * hp2 + hh
                    k.op("vector", lambda e: e.tensor_reduce(out=kmx[:, h:h + 1], in_=P2[:, hh * 256:(hh + 1) * 256], axis=AX.X, op=ALU.max), reads=[P2], writes=[kmx])
                    if stage == 0.8:
                        k.barrier(); k.dma("sync", out[0:128, 0:128], ones_f[:], reads=[ones_f], writes=[od]); k.finish([od]); return nc
                if stage == 0.9:
                    k.barrier(); k.dma("sync", out[0:128, 0:128], ones_f[:], reads=[ones_f], writes=[od]); k.finish([od]); return nc
            if stage in (1, 1.5, 1.6, 1.7):
                k.barrier(); k.dma("sync", out[0:128, 0:128], ones_f[:], reads=[ones_f], writes=[od]); k.finish([od]); return nc
            for mt in range(2):
                P = ps[5 + mt]
                for kc in range(16):
                    k.op("tensor", lambda e: e.matmul(P[:], lhsT=MT[:, kc, mt * 128:(mt + 1) * 128], rhs=Wv[:, kc, :], start=(kc == 0), stop=(kc == 15)),
                         reads=[Wv, MT], writes=[P])
                k.op("vector", lambda e: e.tensor_copy(out=V[:, mt, :], in_=P[:]), reads=[P], writes=[V])
            k.barrier()
        if stage == 2:
            k.barrier(); k.dma("sync", out[0:128, 0:128], ones_f[:], reads=[ones_f], writes=[od]); k.finish([od]); return nc
        k.op("scalar", lambda e: e.activation(out=kmx[:], in_=kmx[:], func=AF.Sqrt), reads=[kmx], writes=[kmx])
        k.op("vector", lambda e: e.tensor_scalar(out=kmx[:], in0=kmx[:], scalar1=-1.0, scalar2=None, op0=ALU.mult), reads=[kmx], writes=[kmx])
        for h in range(4):
            k.op("vector", lambda e: e.memset(krow[h][:], 1.0), writes=[krow[h]])
            k.op("vector", lambda e: e.tensor_scalar(out=krow[h][:], in0=krow[h][:], scalar1=kmx[0:1, h:h + 1], scalar2=None, op0=ALU.mult), reads=[krow[h], kmx], writes=[krow[h]])
        if stage == 3:
            k.barrier(); k.dma("sync", out[0:128, 0:128], ones_f[:], reads=[ones_f], writes=[od]); k.finish([od]); return nc
        Wq = k.sbuf("Wq", [128, 16, 512], BF16); k.dma("gpsimd", Wq[:], wq.rearrange("(c p) n -> p c n", p=128), writes=[Wq])
        Wo = k.sbuf("Wo", [128, 4, D], BF16); k.dma("gpsimd", Wo[:], wo.rearrange("(c p) n -> p c n", p=128), writes=[Wo])
        QT = k.sbuf("QT", [128, 4, 512], BF16)
        PT = [k.sbuf(f"PT{i}", [128, 512], BF16) for i in range(2)]
        OTn = k.sbuf("OTn", [128, 4, 512], BF16)
        rden = k.sbuf("rden", [128, 512], F32)
        X = k.sbuf("Xt", [128, D], F32); R = k.sbuf("Rt", [128, D], F32)
        for tb in range(T // 512):
            sl = slice(tb * 512, (tb + 1) * 512)
            for h in range(4):
                P = ps[h % 2]
                for kc in range(16):
                    k.op("tensor", lambda e: e.matmul(P[:], lhsT=Wq[:, kc, h * 128:(h + 1) * 128], rhs=XT[:, kc, sl], start=(kc == 0), stop=(kc == 15)),
                         reads=[Wq, XT], writes=[P])
                k.op("scalar", lambda e: e.activation(out=QT[:, h, :], in_=P[:], func=AF.Copy, scale=XA_SCALE), reads=[P], writes=[QT])
                k.op("scalar", lambda e: e.activation(out=sq[:], in_=P[:], func=AF.Square, scale=XA_SCALE), reads=[P], writes=[sq])
                P2 = ps[2]
                k.op("tensor", lambda e: e.matmul(P2[:], lhsT=ones_f[:], rhs=sq[:], start=True, stop=True), reads=[ones_f, sq], writes=[P2])
                k.op("scalar", lambda e: e.activation(out=arow[h][:], in_=P2[0:1, :], func=AF.Sqrt), reads=[P2], writes=[arow[h]])
            if stage == 4:
                k.barrier(); k.dma("sync", out[0:128, 0:128], ones_f[:], reads=[ones_f], writes=[od]); k.finish([od]); return nc
            for h in range(4):
                for mt in range(2):
                    SP = ps[3 + mt]
                    k.op("tensor", lambda e: e.matmul(SP[:], lhsT=KT[:, h, mt * 128:(mt + 1) * 128], rhs=QT[:, h, :], start=True, stop=False), reads=[KT, QT], writes=[SP])
                    k.op("tensor", lambda e: e.matmul(SP[:], lhsT=krow[h][0:1, :], rhs=arow[h][0:1, :], start=False, stop=True), reads=[krow[h], arow[h]], writes=[SP])
                    k.op("scalar", lambda e: e.activation(out=PT[mt][:], in_=SP[:], func=AF.Exp), reads=[SP], writes=[PT[mt]])
                PO, PDn = ps[5], ps[6]
                for mt in range(2):
                    k.op("tensor", lambda e: e.matmul(PO[:], lhsT=V[:, mt, h * 128:(h + 1) * 128], rhs=PT[mt][:], start=(mt == 0), stop=(mt == 1)), reads=[V, PT[mt]], writes=[PO])
                for mt in range(2):
                    k.op("tensor", lambda e: e.matmul(PDn[:], lhsT=ones_b[:], rhs=PT[mt][:], start=(mt == 0), stop=(mt == 1)), reads=[ones_b, PT[mt]], writes=[PDn])
                k.op("vector", lambda e: e.reciprocal(out=rden[:], in_=PDn[:]), reads=[PDn], writes=[rden])
                k.op("vector", lambda e: e.tensor_tensor(out=OTn[:, h, :], in0=PO[:], in1=rden[:], op=ALU.mult), reads=[PO, rden], writes=[OTn])
            if stage == 5:
                k.barrier(); k.dma("sync", out[0:128, 0:128], ones_f[:], reads=[ones_f], writes=[od]); k.finish([od]); return nc
            for tt in range(4):
                t0 = tb * 512 + tt * 128
                k.dma("sync", X[:], x1[t0:t0 + 128, :], writes=[X])
                for nb in range(4):
                    P = ps[nb % 2]
                    for h in range(4):
                        k.op("tensor", lambda e: e.matmul(P[:], lhsT=OTn[:, h, tt * 128:(tt + 1) * 128], rhs=Wo[:, h, nb * 512:(nb + 1) * 512], start=(h == 0), stop=(h == 3)),
                             reads=[OTn, Wo], writes=[P])
                    k.op("vector", lambda e: e.scalar_tensor_tensor(out=R[:, nb * 512:(nb + 1) * 512], in0=X[:, nb * 512:(nb + 1) * 512], scalar=DN_ALPHA, in1=P[:],
                                                                    op0=ALU.mult, op1=ALU.add), reads=[X, P], writes=[R])
                ln_rows(k, L, R, X)
                k.dma("sync", out[t0:t0 + 128, :], X[:], reads=[X], writes=[od])
        k.finish([od])
    return nc


def xattn_inputs(x1, p, l):
    ins = []
    xt = x1.reshape(-1, D)
    for c in range(NCORES):
        b = c // 2
        ts = slice(c * TPC, (c + 1) * TPC)
        ins.append({"x1T": _c(xt[ts].T), "x1": _c(xt[ts]), "memT": _c(p["mem"][b].T),
                    "wq": _c(p["xa_w_q"][l]), "wk": _c(p["xa_w_k"][l]), "wv": _c(p["xa_w_v"][l]), "wo": _c(p["xa_w_o"][l]),
                    "lng": _c(p["ln2_g"][l][None]), "lnb": _c(p["ln2_b"][l][None])})
    return ins


def build_moe():
    T = TPC
    HT = T // 2
    nc = new_nc()
    di = lambda n, shp, dt=F32: nc.dram_tensor(n, shp, dt, kind="ExternalInput").ap()
    x2T = di("x2T", [D, T]); x2 = di("x2", [T, D]); wr = di("wr", [D, 36]); br = di("br", [1, 36])
    wgu = di("wgu", [32, D, 1024]); wd = di("wd", [32, 512, D])
    lng = di("lng", [1, D]); lnb = di("lnb", [1, D])
    out = nc.dram_tensor("x3", [T, D], F32, kind="ExternalOutput").ap()
    with ExitStack() as st:
        k = MK(nc, st)
        od = k.view("od", out)
        ps = [k.psum(f"ps{i}", [128, 512], F32) for i in range(8)]
        L = ln_setup(k, lng, lnb)
        XTb = k.sbuf("XTb", [128, 16, HT], BF16)
        yacc = k.sbuf("yacc", [128, HT // 128, D], F32)
        gates = k.sbuf("gates", [128, HT // 128, 32], F32)
        for half in range(2):
            h0 = half * HT
            k.dma("gpsimd", XTb[:], x2T[:, h0:h0 + HT].rearrange("(c p) t -> p c t", p=128), writes=[XTb])
            with ExitStack() as tst:
                S_ = lambda n, shp, dt=F32: k.sbuf(f"{n}_{half}", shp, dt, stack=tst)
                Wr = S_("Wr", [128, 16, 36]); k.dma("sync", Wr[:], wr.rearrange("(c p) n -> p c n", p=128), writes=[Wr])
                brB = S_("brB", [128, 36]); k.dma("sync", brB[:], br.to_broadcast([128, 36]), writes=[brB])
                xr = S_("xr", [128, 16, 128])
                lg = S_("lg", [128, 36]); m4 = S_("m4", [128, 1]); nm4 = S_("nm4", [128, 1]); oh4 = S_("oh4", [128, 4]); e4 = S_("e4", [128, 4])
                s4 = S_("s4", [128, 1]); pg = S_("pg", [128, 1]); sel = S_("sel", [128, 8]); sel2 = S_("sel2", [128, 8])
                m1 = S_("m1", [128, 1]); m2 = S_("m2", [128, 1]); oh1 = S_("oh1", [128, 8]); oh2 = S_("oh2", [128, 8])
                g1 = S_("g1", [128, 1]); g2 = S_("g2", [128, 1]); gate8 = S_("gate8", [128, 8])
                for tt in range(HT // 128):
                    t0 = h0 + tt * 128
                    k.dma("sync", xr[:], x2T[:, t0:t0 + 128].rearrange("(c p) t -> p c t", p=128), writes=[xr])
                    P = ps[tt % 2]
                    for kc in range(16):
                        k.op("tensor", lambda e: e.matmul(P[:, 0:36], lhsT=xr[:, kc, :], rhs=Wr[:, kc, :], start=(kc == 0), stop=(kc == 15)), reads=[xr, Wr], writes=[P])
                    k.op("vector", lambda e: e.tensor_tensor(out=lg[:], in0=P[:, 0:36], in1=brB[:], op=ALU.add), reads=[P, brB], writes=[lg])
                    k.op("vector", lambda e: e.tensor_reduce(out=m4[:], in_=lg[:, 0:4], axis=AX.X, op=ALU.max), reads=[lg], writes=[m4])
                    k.op("vector", lambda e: e.tensor_scalar(out=oh4[:], in0=lg[:, 0:4], scalar1=m4[:, 0:1], scalar2=None, op0=ALU.is_equal), reads=[lg, m4], writes=[oh4])
                    k.op("vector", lambda e: e.tensor_scalar(out=nm4[:], in0=m4[:], scalar1=-1.0, scalar2=None, op0=ALU.mult), reads=[m4], writes=[nm4])
                    k.op("scalar", lambda e: e.activation(out=e4[:], in_=lg[:, 0:4], func=AF.Exp, bias=nm4[:, 0:1], scale=1.0, accum_out=s4[:, 0:1]), reads=[lg, nm4], writes=[e4, s4])
                    k.op("vector", lambda e: e.reciprocal(out=pg[:], in_=s4[:]), reads=[s4], writes=[pg])
                    k.op("vector", lambda e: e.tensor_scalar(out=sel[:], in0=lg[:, 4:12], scalar1=oh4[:, 0:1], scalar2=None, op0=ALU.mult), reads=[lg, oh4], writes=[sel])
                    for g in range(1, 4):
                        k.op("vector", lambda e: e.scalar_tensor_tensor(out=sel[:], in0=lg[:, 4 + 8 * g:12 + 8 * g], scalar=oh4[:, g:g + 1], in1=sel[:], op0=ALU.mult, op1=ALU.add),
                             reads=[lg, oh4, sel], writes=[sel])
                    k.op("vector", lambda e: e.tensor_reduce(out=m1[:], in_=sel[:], axis=AX.X, op=ALU.max), reads=[sel], writes=[m1])
                    k.op("vector", lambda e: e.tensor_scalar(out=oh1[:], in0=sel[:], scalar1=m1[:, 0:1], scalar2=None, op0=ALU.is_equal), reads=[sel, m1], writes=[oh1])
                    k.op("vector", lambda e: e.scalar_tensor_tensor(out=sel2[:], in0=oh1[:], scalar=-1e30, in1=sel[:], op0=ALU.mult, op1=ALU.add), reads=[oh1, sel], writes=[sel2])
                    k.op("vector", lambda e: e.tensor_reduce(out=m2[:], in_=sel2[:], axis=AX.X, op=ALU.max), reads=[sel2], writes=[m2])
                    k.op("vector", lambda e: e.tensor_scalar(out=oh2[:], in0=sel2[:], scalar1=m2[:, 0:1], scalar2=None, op0=ALU.is_equal), reads=[sel2, m2], writes=[oh2])
                    k.op("vector", lambda e: e.tensor_tensor(out=g1[:], in0=m1[:], in1=m2[:], op=ALU.subtract), reads=[m1, m2], writes=[g1])
                    k.op("scalar", lambda e: e.activation(out=g1[:], in_=g1[:], func=AF.Sigmoid), reads=[g1], writes=[g1])
                    k.op("vector", lambda e: e.tensor_tensor(out=g1[:], in0=g1[:], in1=pg[:], op=ALU.mult), reads=[g1, pg], writes=[g1])
                    k.op("vector", lambda e: e.tensor_tensor(out=g2[:], in0=pg[:], in1=g1[:], op=ALU.subtract), reads=[g1, pg], writes=[g2])
                    k.op("vector", lambda e: e.tensor_scalar(out=gate8[:], in0=oh1[:], scalar1=g1[:, 0:1], scalar2=None, op0=ALU.mult), reads=[oh1, g1], writes=[gate8])
                    k.op("vector", lambda e: e.scalar_tensor_tensor(out=gate8[:], in0=oh2[:], scalar=g2[:, 0:1], in1=gate8[:], op0=ALU.mult, op1=ALU.add), reads=[oh2, g2, gate8], writes=[gate8])
                    for g in range(4):
                        k.op("vector", lambda e: e.tensor_scalar(out=gates[:, tt, g * 8:(g + 1) * 8], in0=gate8[:], scalar1=oh4[:, g:g + 1], scalar2=None, op0=ALU.mult),
                             reads=[gate8, oh4], writes=[gates])
                k.barrier()
            with ExitStack() as est:
                G = k.sbuf(f"G{half}", [128, 16, 512], BF16, stack=est); U = k.sbuf(f"U{half}", [128, 16, 512], BF16, stack=est)
                Wd = k.sbuf(f"Wd{half}", [128, 4, D], BF16, stack=est)
                hT = k.sbuf(f"hT{half}", [128, 4, 512], BF16, stack=est)
                sg = [k.sbuf(f"sg{i}_{half}", [128, 512], F32, stack=est) for i in range(2)]
                for ex in range(32):
                    k.dma("gpsimd", G[:], wgu[ex, :, 0:512].rearrange("(c p) n -> p c n", p=128), writes=[G])
                    k.dma("gpsimd", U[:], wgu[ex, :, 512:1024].rearrange("(c p) n -> p c n", p=128), writes=[U])
                    k.dma("gpsimd", Wd[:], wd[ex].rearrange("(c p) n -> p c n", p=128), writes=[Wd])
                    for blk in range(HT // 512):
                        bs = slice(blk * 512, (blk + 1) * 512)
                        for fc in range(4):
                            Pg, Pu = ps[(2 * fc) % 4], ps[(2 * fc + 1) % 4]
                            for kc in range(16):
                                k.op("tensor", lambda e: e.matmul(Pg[:], lhsT=G[:, kc, fc * 128:(fc + 1) * 128], rhs=XTb[:, kc, bs], start=(kc == 0), stop=(kc == 15)),
                                     reads=[G, XTb], writes=[Pg])
                            for kc in range(16):
                                k.op("tensor", lambda e: e.matmul(Pu[:], lhsT=U[:, kc, fc * 128:(fc + 1) * 128], rhs=XTb[:, kc, bs], start=(kc == 0), stop=(kc == 15)),
                                     reads=[U, XTb], writes=[Pu])
                            SG = sg[fc % 2]
                            k.op("scalar", lambda e: e.activation(out=SG[:], in_=Pg[:], func=AF.Silu, scale=1.0), reads=[Pg], writes=[SG])
                            k.op("vector", lambda e: e.tensor_tensor(out=hT[:, fc, :], in0=SG[:], in1=Pu[:], op=ALU.mult), reads=[SG, Pu], writes=[hT])
                        for tt in range(4):
                            tile_i = blk * 4 + tt
                            for nb in range(4):
                                Py = ps[4 + (tt * 4 + nb) % 4]
                                for fc in range(4):
                                    k.op("tensor", lambda e: e.matmul(Py[:], lhsT=hT[:, fc, tt * 128:(tt + 1) * 128], rhs=Wd[:, fc, nb * 512:(nb + 1) * 512],
                                                                      start=(fc == 0), stop=(fc == 3)), reads=[hT, Wd], writes=[Py])
                                ya = yacc[:, tile_i, nb * 512:(nb + 1) * 512]
                                if ex == 0:
                                    k.op("vector", lambda e: e.tensor_scalar(out=ya, in0=Py[:], scalar1=gates[:, tile_i, ex:ex + 1], scalar2=None, op0=ALU.mult),
                                         reads=[Py, gates], writes=[yacc])
                                else:
                                    k.op("vector", lambda e: e.scalar_tensor_tensor(out=ya, in0=Py[:], scalar=gates[:, tile_i, ex:ex + 1], in1=ya, op0=ALU.mult, op1=ALU.add),
                                         reads=[Py, gates, yacc], writes=[yacc])
                X = k.sbuf(f"Xt{half}", [128, D], F32, stack=est)
                for tt in range(HT // 128):
                    t0 = h0 + tt * 128
                    k.dma("sync", X[:], x2[t0:t0 + 128, :], writes=[X])
                    k.op("vector", lambda e: e.scalar_tensor_tensor(out=yacc[:, tt, :], in0=X[:], scalar=DN_ALPHA, in1=yacc[:, tt, :], op0=ALU.mult, op1=ALU.add),
                         reads=[X, yacc], writes=[yacc])
                    Rv = k.view("Rv", yacc[:, tt, :])
                    Rv.w = yacc.w; Rv.r = yacc.r
                    ln_rows(k, L, Rv, X)
                    yacc.r.update(Rv.r)
                    k.dma("sync", out[t0:t0 + 128, :], X[:], reads=[X], writes=[od])
                k.barrier()
        k.finish([od])
    return nc


def moe_inputs(x2, p, l):
    ins = []
    xt = x2.reshape(-1, D)
    wr = _c(np.concatenate([p["moe_w_group"][l], p["moe_w_expert"][l]], 1))
    br = _c(np.concatenate([p["moe_b_group"][l], p["moe_b_expert"][l]])[None])
    for c in range(NCORES):
        ts = slice(c * TPC, (c + 1) * TPC)
        ins.append({"x2T": _c(xt[ts].T), "x2": _c(xt[ts]), "wr": wr, "br": br,
                    "wgu": _c(p["moe_w_gate_up"][l]), "wd": _c(p["moe_w_down"][l]),
                    "lng": _c(p["ln3_g"][l][None]), "lnb": _c(p["ln3_b"][l][None])})
    return ins


_NC_CACHE = {}


def _nc(name, builder):
    if name not in _NC_CACHE:
        _NC_CACHE[name] = builder()
    return _NC_CACHE[name]


def kernel(**inputs):
    p = {k_: np.asarray(v) for k_, v in inputs.items()}
    x = np.ascontiguousarray(p["x"], dtype=np.float32)
    for l in range(2):
        xt = x.reshape(-1, D)
        res = run(_nc("gemm", lambda: build_gemm(TPC, D, IN_COLS)),
                  [{"xT": _c(xt[c * TPC:(c + 1) * TPC].T), "w": _c(p["w_in"][l])} for c in range(NCORES)])
        proj = np.concatenate([r["out"] for r in res], 0).reshape(BATCH, SEQ, IN_COLS)
        s5scan = s5_gather(run(_nc("s5", build_s5), s5_inputs(proj, p, l)))
        o_mla = np.zeros((BATCH, SEQ, 1024), np.float32)
        for part in range(2):
            mla_gather(o_mla, run(_nc("mla", build_mla), mla_inputs(proj, p, l, part)), part)
        y_gdn = gdn_gather(run(_nc("gdn", build_gdn), gdn_inputs(proj, p, l)))
        del proj
        res = run(_nc("mixout", build_mixout), mixout_inputs(x, s5scan, o_mla, y_gdn, p, l))
        x1 = np.concatenate([r["x1"] for r in res], 0).reshape(BATCH, SEQ, D)
        res = run(_nc("xattn", build_xattn), xattn_inputs(x1, p, l))
        x2 = np.concatenate([r["x2"] for r in res], 0).reshape(BATCH, SEQ, D)
        res = run(_nc("moe", build_moe), moe_inputs(x2, p, l))
        x = np.concatenate([r["x3"] for r in res], 0).reshape(BATCH, SEQ, D)
    return x.astype(np.float32)
```

```python
import math
from contextlib import ExitStack
from concourse.bass_utils import run_bass_kernel_spmd
import numpy as np
import concourse.bass as bass
import concourse.mybir as mybir

F32 = mybir.dt.float32
BF16 = mybir.dt.bfloat16
I32 = mybir.dt.int32
U32 = mybir.dt.uint32
AF = mybir.ActivationFunctionType
ALU = mybir.AluOpType
AX = mybir.AxisListType


SAME_ENGINE_SYNC = True


class Buf:
    def __init__(self, k, name, ap):
        self.k = k
        self.name = name
        self.ap = ap
        self.w = None
        self.r = {}
        self.dw = None
        self.dr = None

    def __getitem__(self, idx):
        return self.ap[idx]


class Eng:
    def __init__(self, k, name, eng, sem):
        self.k = k
        self.name = name
        self.eng = eng
        self.sem = sem
        self.count = 0
        self.seen = {}


class MK:
    def __init__(self, nc, stack, same_engine_sync=None, pe_self_sync=False):
        if same_engine_sync is None:
            same_engine_sync = SAME_ENGINE_SYNC
        self.nc = nc
        self.stack = stack
        self.sems = {}
        self.semval = {}
        self.same_engine_sync = same_engine_sync
        self.pe_self_sync = pe_self_sync
        self.engs = {}
        for name in ["tensor", "vector", "scalar", "gpsimd", "sync"]:
            s = self._newsem("e_" + name)
            self.engs[name] = Eng(self, name, getattr(nc, name), s)
        self.pe = self.engs["tensor"]
        self.dve = self.engs["vector"]
        self.act = self.engs["scalar"]
        self.pool = self.engs["gpsimd"]
        self.sp = self.engs["sync"]
        self.nbuf = 0
        self.ninst = 0

    def _newsem(self, key):
        h = self.stack.enter_context(self.nc.semaphore(key))
        self.sems[key] = h
        self.semval[key] = 0
        return key

    def sbuf(self, name, shape, dt, stack=None):
        t = (stack or self.stack).enter_context(self.nc.sbuf_tensor(name, list(shape), dt))
        return Buf(self, name, t)

    def psum(self, name, shape, dt):
        t = self.stack.enter_context(self.nc.psum_tensor(name, list(shape), dt))
        return Buf(self, name, t)

    def dram(self, name, shape, dt, kind="Internal"):
        t = self.nc.dram_tensor(name, list(shape), dt, kind=kind)
        return Buf(self, name, t.ap())

    def view(self, name, ap):
        return Buf(self, name, ap)

    def _wait(self, E, deps):
        for (sk, val) in deps:
            if sk == E.sem and (not self.same_engine_sync or (E.name == "tensor" and not self.pe_self_sync)):
                continue
            if E.seen.get(sk, 0) < val:
                E.eng.wait_ge(self.sems[sk], val)
                E.seen[sk] = val

    def _deps(self, reads, writes, skip=None):
        deps = []
        for b in reads:
            if b.w is not None:
                deps.append(b.w)
        for b in writes:
            if b.w is not None and b.w[0] != skip:
                deps.append(b.w)
            for sk, v in b.r.items():
                deps.append((sk, v))
        return deps

    def op(self, E, fn, reads=(), writes=()):
        if isinstance(E, str):
            E = self.engs[E]
        self._wait(E, self._deps(reads, writes))
        inst = fn(E.eng)
        E.count += 1
        inst.then_inc(self.sems[E.sem], 1)
        tok = (E.sem, E.count)
        for b in writes:
            b.w = tok
            b.r = {}
        for b in reads:
            if b not in writes:
                b.r[E.sem] = E.count
        self.ninst += 1
        return inst

    def dma(self, Q, out_ap, in_ap, reads=(), writes=(), indirect=None, **kw):
        if isinstance(Q, str):
            Q = self.engs[Q]
        assert len(writes) == 1
        wb = writes[0]
        if wb.dw is None:
            wb.dw = self._newsem("dw_" + wb.name)
        self._wait(Q, self._deps(reads, writes, skip=wb.dw))
        if Q.name == "gpsimd":
            prev = getattr(self, "_last_swdge", None)
            if prev is not None:
                self._wait(Q, [prev])
        if indirect is None:
            inst = Q.eng.dma_start(out=out_ap, in_=in_ap, **kw)
        else:
            inst = indirect(Q.eng)
        inst.then_inc(self.sems[wb.dw], 16)
        self.semval[wb.dw] += 16
        tok = (wb.dw, self.semval[wb.dw])
        if Q.name == "gpsimd":
            self._last_swdge = tok
        wb.w = tok
        wb.r = {}
        for b in reads:
            b.r[wb.dw] = self.semval[wb.dw]
        self.ninst += 1
        return inst

    def barrier(self):
        for E in self.engs.values():
            deps = [(o.sem, o.count) for o in self.engs.values() if o.count > 0]
            deps += [(sk, v) for sk, v in self.semval.items() if sk.startswith("dw_") and v > 0]
            self._wait(E, deps)

    def finish(self, bufs):
        for b in bufs:
            if b.w is not None:
                self._wait(self.sp, [b.w])


NCORES = 8
D = 2048
SEQ = 4096
BATCH = 4
TPC = 2048
IN_COLS = 3656
DN_ALPHA = 4.0 ** 0.25


def new_nc():
    return bass.Bass("TRN2", target_bir_lowering=False)


def run(nc, in_maps):
    res = run_bass_kernel_spmd(nc, in_maps, core_ids=list(range(NCORES)))
    return res.results


def build_gemm(T, K, N):
    nc = new_nc()
    xT = nc.dram_tensor("xT", [K, T], F32, kind="ExternalInput").ap()
    w = nc.dram_tensor("w", [K, N], F32, kind="ExternalInput").ap()
    out = nc.dram_tensor("out", [T, N], F32, kind="ExternalOutput").ap()
    KC = K // 128
    TT = T // 128
    with ExitStack() as st:
        k = MK(nc, st)
        xs = k.sbuf("xs", [128, KC, T], BF16)
        k.dma("gpsimd", xs[:], xT.rearrange("(c p) t -> p c t", p=128), writes=[xs])
        wb = [k.sbuf(f"wb{i}", [128, KC, 512], BF16) for i in range(2)]
        ps = [k.psum(f"ps{i}", [128, 512], F32) for i in range(4)]
        ob = [k.sbuf(f"ob{i}", [128, 512], F32) for i in range(4)]
        od = k.view("od", out)
        nblk = (N + 511) // 512
        it = 0
        for nb in range(nblk):
            n0 = nb * 512
            nw = min(512, N - n0)
            W = wb[nb % 2]
            k.dma("gpsimd", W[:, :, :nw], w[:, n0:n0 + nw].rearrange("(c p) n -> p c n", p=128), writes=[W])
            for tt in range(TT):
                P = ps[it % 4]
                O = ob[it % 4]
                for c in range(KC):
                    k.op("tensor", lambda e: e.matmul(P[:, :nw], lhsT=xs[:, c, tt * 128:(tt + 1) * 128], rhs=W[:, c, :nw],
                                                      start=(c == 0), stop=(c == KC - 1)), reads=[xs, W], writes=[P])
                if it % 2 == 0:
                    k.op("vector", lambda e: e.tensor_copy(out=O[:, :nw], in_=P[:, :nw]), reads=[P], writes=[O])
                else:
                    k.op("scalar", lambda e: e.activation(out=O[:, :nw], in_=P[:, :nw], func=AF.Copy), reads=[P], writes=[O])
                k.dma("sync", out[tt * 128:(tt + 1) * 128, n0:n0 + nw], O[:, :nw], reads=[O], writes=[od])
                it += 1
        k.finish([od])
    return nc


def make_ident(k, n=128, dt=F32, name="ident"):
    t = k.sbuf(name, [n, n], dt)
    k.op("gpsimd", lambda e: e.memset(t[:], 0.0), writes=[t])
    k.op("gpsimd", lambda e: e.affine_select(out=t[:], in_=t[:], pattern=[[-1, n]], compare_op=ALU.not_equal,
                                             fill=1.0, base=0, channel_multiplier=1), reads=[t], writes=[t])
    return t


def make_tri(k, name, n, keep_q_ge_k=True, dt=F32, strict=False):
    t = k.sbuf(name, [n, n], dt)
    k.op("gpsimd", lambda e: e.memset(t[:], 1.0), writes=[t])
    k.op("gpsimd", lambda e: e.affine_select(out=t[:], in_=t[:], pattern=[[1, n]], compare_op=ALU.is_ge,
                                             fill=0.0, base=(-1 if strict else 0), channel_multiplier=-1),
         reads=[t], writes=[t])
    return t


MLA_SCALE = 192.0 ** -0.5


def build_mla(stage=9):
    S = SEQ
    nc = new_nc()
    di = lambda n, shp, dt=F32: nc.dram_tensor(n, shp, dt, kind="ExternalInput").ap()
    cqT = di("cqT", [512, S]); ckvT = di("ckvT", [512, S])
    kr1 = di("kr1", [128, S]); kr2 = di("kr2", [128, S])
    msgn = di("msgn", [128, 1])
    pos = di("pos", [1, S], I32)
    invf = di("invf", [128, 1])
    qn = di("qn", [128, 4]); kvn = di("kvn", [128, 4])
    wqn = di("wqn", [512, 256]); wq1 = di("wq1", [512, 128]); wq2 = di("wq2", [512, 128])
    wk = di("wk", [512, 256]); wv = di("wv", [512, 256])
    out = nc.dram_tensor("o", [S, 256], F32, kind="ExternalOutput").ap()
    NB = S // 512
    with ExitStack() as st:
        k = MK(nc, st)
        od = k.view("od", out)
        ps = [k.psum(f"ps{i}", [128, 512], F32) for i in range(8)]
        ones_f = k.sbuf("ones_f", [128, 128], F32)
        k.op("vector", lambda e: e.memset(ones_f[:], 1.0), writes=[ones_f])
        ones_b = k.sbuf("ones_b", [128, 128], BF16)
        k.op("vector", lambda e: e.memset(ones_b[:], 1.0), writes=[ones_b])
        trimask = make_tri(k, "trimask", 128, dt=BF16)
        Wqn = k.sbuf("Wqn", [128, 4, 256], BF16); Wq1 = k.sbuf("Wq1", [128, 4, 128], BF16)
        Wq2 = k.sbuf("Wq2", [128, 4, 128], BF16); Wk = k.sbuf("Wk", [128, 4, 256], BF16)
        Wv = k.sbuf("Wv", [128, 4, 256], BF16)
        for Wt, wd in ((Wqn, wqn), (Wq1, wq1), (Wq2, wq2), (Wk, wk), (Wv, wv)):
            k.dma("gpsimd", Wt[:], wd.rearrange("(c p) n -> p c n", p=128), writes=[Wt])
        qn_s = k.sbuf("qn_s", [128, 4], F32); kvn_s = k.sbuf("kvn_s", [128, 4], F32)
        k.dma("sync", qn_s[:], qn, writes=[qn_s]); k.dma("sync", kvn_s[:], kvn, writes=[kvn_s])
        invf_s = k.sbuf("invf_s", [128, 1], F32)
        k.dma("sync", invf_s[:], invf, writes=[invf_s])
        msgn_s = k.sbuf("msgn_s", [128, 1], F32)
        k.dma("sync", msgn_s[:], msgn, writes=[msgn_s])
        mone_s = k.sbuf("mone_s", [128, 1], F32)
        k.op("vector", lambda e: e.memset(mone_s[:], -1.0), writes=[mone_s])
        cosT = k.sbuf("cosT", [128, S], BF16); sinT = k.sbuf("sinT", [128, S], BF16)
        negpi = k.sbuf("negpi", [128, 1], F32)
        k.op("vector", lambda e: e.memset(negpi[:], -math.pi), writes=[negpi])
        with ExitStack() as tst:
            posi = k.sbuf("posi", [128, S], I32, stack=tst)
            k.dma("sync", posi[:], pos.to_broadcast([128, S]), writes=[posi])
            ang = k.sbuf("ang", [128, S], F32, stack=tst)
            k.op("vector", lambda e: e.tensor_copy(out=ang[:], in_=posi[:]), reads=[posi], writes=[ang])
            k.op("vector", lambda e: e.tensor_scalar(out=ang[:], in0=ang[:], scalar1=invf_s[:, 0:1], scalar2=1.0 / (2 * math.pi),
                                                     op0=ALU.mult, op1=ALU.mult), reads=[ang, invf_s], writes=[ang])
            tmpf = k.sbuf("tmpf", [128, S], F32, stack=tst)
            tmpg = k.sbuf("tmpg", [128, S], F32, stack=tst)

            def sin_of_turns(dst, shift, mul):
                k.op("vector", lambda e: e.tensor_scalar(out=tmpf[:], in0=ang[:], scalar1=shift, scalar2=None, op0=ALU.add),
                     reads=[ang], writes=[tmpf])
                k.op("vector", lambda e: e.tensor_copy(out=posi[:], in_=tmpf[:]), reads=[tmpf], writes=[posi])
                k.op("vector", lambda e: e.tensor_copy(out=tmpg[:], in_=posi[:]), reads=[posi], writes=[tmpg])
                k.op("vector", lambda e: e.tensor_tensor(out=tmpf[:], in0=tmpf[:], in1=tmpg[:], op=ALU.subtract),
                     reads=[tmpf, tmpg], writes=[tmpf])
                k.op("vector", lambda e: e.tensor_scalar(out=tmpg[:], in0=tmpf[:], scalar1=0.0, scalar2=None, op0=ALU.is_lt),
                     reads=[tmpf], writes=[tmpg])
                k.op("vector", lambda e: e.tensor_tensor(out=tmpf[:], in0=tmpf[:], in1=tmpg[:], op=ALU.add),
                     reads=[tmpf, tmpg], writes=[tmpf])
                k.op("scalar", lambda e: e.activation(out=tmpg[:], in_=tmpf[:], func=AF.Sin, bias=negpi[:, 0:1], scale=2 * math.pi),
                     reads=[tmpf, negpi], writes=[tmpg])
                k.op("vector", lambda e: e.tensor_scalar(out=dst[:], in0=tmpg[:], scalar1=mul[:, 0:1], scalar2=None, op0=ALU.mult),
                     reads=[tmpg, mul], writes=[dst])

            sin_of_turns(sinT, 0.0, msgn_s)
            sin_of_turns(cosT, 0.25, mone_s)
            k.barrier()
        if stage == 0:
            k.dma("sync", out[0:128, 0:128], ones_f[:], reads=[ones_f, sinT, cosT], writes=[od]); k.finish([od]); return nc
        QN = [k.sbuf(f"QN{h}", [128, S], BF16) for h in range(2)]
        KN = [k.sbuf(f"KN{h}", [128, S], BF16) for h in range(2)]
        QR = [k.sbuf(f"QR{i}", [128, S], BF16) for i in range(1)]
        KR = k.sbuf("KR", [128, S], BF16)
        Va = k.sbuf("Va", [128, S // 128, 2, 129], BF16)
        k.op("gpsimd", lambda e: e.memset(Va[:], 1.0), writes=[Va])
        arow = k.sbuf("arow", [128, S], BF16)
        krow = k.sbuf("krow", [128, 2, 128], BF16)
        kmax = k.sbuf("kmax", [128, 2, NB], F32)
        krs = k.sbuf("krs", [128, 2, 512], F32)
        t1 = k.sbuf("t1", [128, 512], F32); t2 = k.sbuf("t2", [128, 512], F32)

        def rope(dst, x, xs, sl, rd):
            k.op("vector", lambda e: e.tensor_tensor(out=t1[:], in0=x, in1=cosT[:, sl], op=ALU.mult), reads=rd + [cosT], writes=[t1])
            k.op("vector", lambda e: e.tensor_tensor(out=t2[:], in0=xs, in1=sinT[:, sl], op=ALU.mult), reads=rd + [sinT], writes=[t2])
            k.op("vector", lambda e: e.tensor_tensor(out=dst[:, sl], in0=t1[:], in1=t2[:], op=ALU.add), reads=[t1, t2], writes=[dst])

        xin = [k.sbuf("xin0", [128, 4, 512], F32)]
        sq = k.sbuf("sq", [128, 4, 512], F32)
        xin.append(sq)
        rstd = k.sbuf("rstd", [128, 512], F32)
        xn0 = k.sbuf("xn0", [128, 4, 512], BF16)
        xn = [xn0, xn0]
        sqb = k.sbuf("sqb", [128, 512], F32)

        def rmsnorm_block(src_dram, sl, gam, XI, XN):
            k.dma("sync", XI[:], src_dram[:, sl].rearrange("(c p) t -> p c t", p=128), writes=[XI])
            k.op("scalar", lambda e: e.activation(out=sq[:], in_=XI[:], func=AF.Square), reads=[XI], writes=[sq])
            P = ps[7]
            for c in range(4):
                k.op("tensor", lambda e: e.matmul(P[:], lhsT=ones_f[:], rhs=sq[:, c, :], start=(c == 0), stop=(c == 3)),
                     reads=[ones_f, sq], writes=[P])
            k.op("scalar", lambda e: e.activation(out=rstd[:], in_=P[:], func=AF.Sqrt, bias=eps_t[:, 0:1], scale=1.0 / 512),
                 reads=[P, eps_t], writes=[rstd])
            k.op("vector", lambda e: e.reciprocal(out=rstd[:], in_=rstd[:]), reads=[rstd], writes=[rstd])
            for c in range(4):
                k.op("vector", lambda e: e.scalar_tensor_tensor(out=XN[:, c, :], in0=XI[:, c, :], scalar=gam[:, c:c + 1], in1=rstd[:],
                                                                op0=ALU.mult, op1=ALU.mult), reads=[XI, gam, rstd], writes=[XN])

        eps_t = k.sbuf("eps_t", [128, 1], F32)
        k.op("vector", lambda e: e.memset(eps_t[:], 1e-6), writes=[eps_t])

        for tb in range(NB):
            sl = slice(tb * 512, (tb + 1) * 512)
            k.dma("sync", krs[:, 0, :], kr1[:, sl], writes=[krs])
            k.dma("sync", krs[:, 1, :], kr2[:, sl], writes=[krs])
            rope(KR, krs[:, 0, :], krs[:, 1, :], sl, [krs])
            if stage == 1.1:
                k.barrier(); k.dma("sync", out[0:128, 0:128], ones_f[:], reads=[ones_f], writes=[od]); k.finish([od]); return nc
            XI, XN = xin[0], xn[0]
            rmsnorm_block(cqT, sl, qn_s, XI, XN)
            if stage == 1.2:
                k.barrier(); k.dma("sync", out[0:128, 0:128], ones_f[:], reads=[ones_f], writes=[od]); k.finish([od]); return nc
            for h in range(2):
                P = ps[h]
                for c in range(4):
                    k.op("tensor", lambda e: e.matmul(P[:], lhsT=Wqn[:, c, h * 128:(h + 1) * 128], rhs=XN[:, c, :], start=(c == 0), stop=(c == 3)),
                         reads=[Wqn, XN], writes=[P])
                k.op("scalar", lambda e: e.activation(out=QN[h][:, sl], in_=P[:], func=AF.Copy, scale=MLA_SCALE), reads=[P], writes=[QN[h]])
                k.op("scalar", lambda e: e.activation(out=XI[:, h, :], in_=P[:], func=AF.Square, scale=MLA_SCALE), reads=[P], writes=[XI])
            if stage == 1.3:
                k.barrier(); k.dma("sync", out[0:128, 0:128], ones_f[:], reads=[ones_f], writes=[od]); k.finish([od]); return nc
            qa = xin[1]
            for pr in range(1):
                P1, P2 = ps[4], ps[5]
                for c in range(4):
                    k.op("tensor", lambda e: e.matmul(P1[:], lhsT=Wq1[:, c, pr * 128:(pr + 1) * 128], rhs=XN[:, c, :], start=(c == 0), stop=(c == 3)), reads=[Wq1, XN], writes=[P1])
                for c in range(4):
                    k.op("tensor", lambda e: e.matmul(P2[:], lhsT=Wq2[:, c, pr * 128:(pr + 1) * 128], rhs=XN[:, c, :], start=(c == 0), stop=(c == 3)), reads=[Wq2, XN], writes=[P2])
                k.op("scalar", lambda e: e.activation(out=qa[:, 0, :], in_=P1[:], func=AF.Copy, scale=MLA_SCALE), reads=[P1], writes=[qa])
                k.op("scalar", lambda e: e.activation(out=qa[:, 1, :], in_=P2[:], func=AF.Copy, scale=MLA_SCALE), reads=[P2], writes=[qa])
                rope(QR[pr], qa[:, 0, :], qa[:, 1, :], sl, [qa])
                k.op("scalar", lambda e: e.activation(out=qa[:, 2 + pr, :], in_=qa[:, 0, :], func=AF.Square), reads=[qa], writes=[qa])
            if stage == 1.4:
                k.barrier(); k.dma("sync", out[0:128, 0:128], ones_f[:], reads=[ones_f], writes=[od]); k.finish([od]); return nc
            for h in range(2):
                P = ps[6]
                b64 = (h % 2) * 64
                k.op("tensor", lambda e: e.matmul(P[:], lhsT=ones_f[:], rhs=XI[:, h, :], start=True, stop=False), reads=[ones_f, XI], writes=[P])
                k.op("tensor", lambda e: e.matmul(P[:], lhsT=ones_f[b64:b64 + 64, :], rhs=qa[b64:b64 + 64, 2, :], start=False, stop=True),
                     reads=[ones_f, qa], writes=[P])
                hp = h * 32
                k.op("scalar", lambda e: e.activation(out=arow[hp:hp + 1, tb * 512:tb * 512 + 512], in_=P[hp:hp + 1, :], func=AF.Sqrt), reads=[P], writes=[arow])
            if stage == 1.5:
                k.barrier(); k.dma("sync", out[0:128, 0:128], ones_f[:], reads=[ones_f], writes=[od]); k.finish([od]); return nc
            XI, XN = xin[0], xn[1]
            rmsnorm_block(ckvT, sl, kvn_s, XI, XN)
            if stage == 1.55:
                k.barrier(); k.dma("sync", out[0:128, 0:128], ones_f[:], reads=[ones_f], writes=[od]); k.finish([od]); return nc
            for h in range(2):
                P = ps[h]
                for c in range(4):
                    k.op("tensor", lambda e: e.matmul(P[:], lhsT=Wk[:, c, h * 128:(h + 1) * 128], rhs=XN[:, c, :], start=(c == 0), stop=(c == 3)),
                         reads=[Wk, XN], writes=[P])
                k.op("vector", lambda e: e.tensor_copy(out=KN[h][:, sl], in_=P[:]), reads=[P], writes=[KN[h]])
                if stage == 1.56:
                    continue
                k.op("scalar", lambda e: e.activation(out=qa[:, h, :], in_=KN[h][:, sl], func=AF.Square), reads=[KN[h]], writes=[qa])
            if stage in (1.6, 1.61):
                k.barrier(); k.dma("sync", out[0:128, 0:128], ones_f[:], reads=[ones_f], writes=[od]); k.finish([od]); return nc
            if stage == 1.56:
                k.barrier(); k.dma("sync", out[0:128, 0:128], ones_f[:], reads=[ones_f], writes=[od]); k.finish([od]); return nc
            k.op("vector", lambda e: e.tensor_tensor(out=sqb[:], in0=KR[:, sl], in1=KR[:, sl], op=ALU.mult), reads=[KR], writes=[sqb])
            for h in range(2):
                P = ps[6]
                k.op("tensor", lambda e: e.matmul(P[:], lhsT=ones_f[:], rhs=qa[:, h, :], start=True, stop=False), reads=[ones_f, qa], writes=[P])
                k.op("tensor", lambda e: e.matmul(P[:], lhsT=ones_f[0:64, :], rhs=sqb[0:64, :], start=False, stop=True), reads=[ones_f, sqb], writes=[P])
                k.op("vector", lambda e: e.tensor_reduce(out=kmax[:, h, tb:tb + 1], in_=P[:], axis=AX.X, op=ALU.max), reads=[P], writes=[kmax])
            if stage == 1.7:
                k.barrier(); k.dma("sync", out[0:128, 0:128], ones_f[:], reads=[ones_f], writes=[od]); k.finish([od]); return nc
            for tt in range(4):
                P = ps[tt]
                for c in range(4):
                    k.op("tensor", lambda e: e.matmul(P[:, 0:256], lhsT=XN[:, c, tt * 128:(tt + 1) * 128], rhs=Wv[:, c, :], start=(c == 0), stop=(c == 3)),
                         reads=[XN, Wv], writes=[P])
                k.op("vector", lambda e: e.tensor_copy(out=Va[:, tb * 4 + tt, :, 0:128], in_=P[:, 0:256].rearrange("p (h d) -> p h d", h=2)),
                     reads=[P], writes=[Va])
        if stage == 1:
            k.dma("sync", out[0:128, 0:128], ones_f[:], reads=[ones_f, Va, KR, arow] + QN + KN + QR, writes=[od]); k.finish([od]); return nc
        km = k.sbuf("km", [128, 2], F32)
        for h in range(2):
            k.op("vector", lambda e: e.tensor_reduce(out=km[:, h:h + 1], in_=kmax[:, h, :], axis=AX.X, op=ALU.max), reads=[kmax], writes=[km])
        k.op("scalar", lambda e: e.activation(out=km[:], in_=km[:], func=AF.Sqrt), reads=[km], writes=[km])
        k.op("vector", lambda e: e.tensor_scalar(out=km[:], in0=km[:], scalar1=-1.0, scalar2=None, op0=ALU.mult), reads=[km], writes=[km])
        k.op("vector", lambda e: e.memset(krow[:], 1.0), writes=[krow])
        for h in range(2):
            hp = h * 32
            k.op("vector", lambda e: e.tensor_scalar(out=krow[hp:hp + 1, h, :], in0=krow[hp:hp + 1, h, :], scalar1=km[hp:hp + 1, h:h + 1],
                                                     scalar2=None, op0=ALU.mult), reads=[krow, km], writes=[krow])
        if stage == 2:
            k.dma("sync", out[0:128, 0:128], ones_f[:], reads=[ones_f, krow], writes=[od]); k.finish([od]); return nc
        PT = [k.sbuf(f"PT{i}", [128, 512], BF16) for i in range(3)]
        ot = [k.sbuf(f"ot{i}", [128, 128], F32) for i in range(2)]
        rc = [k.sbuf(f"rc{i}", [128, 1], F32) for i in range(2)]
        it = 0
        oi = 0
        for h in range(2):
            hs = slice((h % 2) * 64, (h % 2) * 64 + 64)
            hp = h * 32
            for qt in range(NB):
                qs = slice(qt * 512, (qt + 1) * 512)
                nkb = 4 * qt + 4
                for kb in range(nkb):
                    ks = slice(kb * 128, (kb + 1) * 128)
                    SP = ps[4 + it % 2]
                    Pt = PT[it % 3]
                    it += 1
                    k.op("tensor", lambda e: e.matmul(SP[:], lhsT=KN[h][:, ks], rhs=QN[h][:, qs], start=True, stop=False), reads=[KN[h], QN[h]], writes=[SP])
                    k.op("tensor", lambda e: e.matmul(SP[:], lhsT=KR[hs, ks], rhs=QR[h // 2][hs, qs], start=False, stop=False), reads=[KR, QR[h // 2]], writes=[SP])
                    k.op("tensor", lambda e: e.matmul(SP[:], lhsT=krow[hp:hp + 1, h, :], rhs=arow[hp:hp + 1, qt * 512:qt * 512 + 512], start=False, stop=True), reads=[krow, arow], writes=[SP])
                    k.op("scalar", lambda e: e.activation(out=Pt[:], in_=SP[:], func=AF.Exp), reads=[SP], writes=[Pt])
                    r = kb - 4 * qt
                    if r >= 0:
                        k.op("vector", lambda e: e.tensor_tensor(out=Pt[:, r * 128:(r + 1) * 128], in0=Pt[:, r * 128:(r + 1) * 128], in1=trimask[:], op=ALU.mult),
                             reads=[Pt, trimask], writes=[Pt])
                    for s in range(4):
                        if r >= 0 and s < r:
                            continue
                        O = ps[s]
                        k.op("tensor", lambda e: e.matmul(O[:, 0:129], lhsT=Pt[:, s * 128:(s + 1) * 128], rhs=Va[:, kb, h, :],
                                                          start=(kb == 0), stop=(kb == 4 * qt + s)), reads=[Pt, Va], writes=[O])
                for s in range(4):
                    O = ps[s]
                    R = rc[oi % 2]; OT = ot[oi % 2]; oi += 1
                    k.op("vector", lambda e: e.reciprocal(out=R[:], in_=O[:, 128:129]), reads=[O], writes=[R])
                    k.op("vector", lambda e: e.tensor_scalar(out=OT[:], in0=O[:, 0:128], scalar1=R[:, 0:1], scalar2=None, op0=ALU.mult),
                         reads=[O, R], writes=[OT])
                    q0 = qt * 512 + s * 128
                    k.dma("sync", out[q0:q0 + 128, h * 128:(h + 1) * 128], OT[:], reads=[OT], writes=[od])
        k.finish([od])
    return nc


C_U, C_CQ, C_CKV, C_KR, C_GQ, C_GK, C_GV, C_GZ, C_GA, C_GB = 0, 512, 1024, 1536, 1600, 2112, 2624, 3136, 3648, 3652


def _c(a):
    return np.ascontiguousarray(a)


def mla_inputs(proj, p, l, part):
    invf = (1.0 / (10000.0 ** (np.arange(32, dtype=np.float32) * (2.0 / 64)))).astype(np.float32)
    ins = []
    for c in range(NCORES):
        b, hh = c // 2, c % 2
        pb = proj[b]
        kr = pb[:, C_KR:C_KR + 64]
        heads = range(4 * hh + 2 * part, 4 * hh + 2 * part + 2)
        wuq = p["mla_w_uq"][l]; wukv = p["mla_w_ukv"][l]
        ins.append({
            "cqT": _c(pb[:, C_CQ:C_CQ + 512].T), "ckvT": _c(pb[:, C_CKV:C_CKV + 512].T),
            "kr1": _c(np.tile(kr.T, (2, 1))), "kr2": _c(np.tile(np.concatenate([kr[:, 32:], kr[:, :32]], 1).T, (2, 1))),
            "msgn": _c(np.tile(np.concatenate([np.ones(32), -np.ones(32)]), 2)[:, None].astype(np.float32)),
            "pos": _c(p["positions"][b][None].astype(np.int32)),
            "invf": _c(np.tile(invf, 4)[:, None]),
            "qn": _c(p["mla_q_norm"][l].reshape(4, 128).T), "kvn": _c(p["mla_kv_norm"][l].reshape(4, 128).T),
            "wqn": _c(np.concatenate([wuq[:, h * 192:h * 192 + 128] for h in heads], 1)),
            "wq1": _c(np.concatenate([wuq[:, h * 192 + 128:h * 192 + 192] for h in heads], 1)),
            "wq2": _c(np.concatenate([np.concatenate([wuq[:, h * 192 + 160:h * 192 + 192], wuq[:, h * 192 + 128:h * 192 + 160]], 1) for h in heads], 1)),
            "wk": _c(np.concatenate([wukv[:, h * 256:h * 256 + 128] for h in heads], 1)),
            "wv": _c(np.concatenate([wukv[:, h * 256 + 128:h * 256 + 256] for h in heads], 1)),
        })
    return ins


def mla_gather(o, res, part):
    for c in range(NCORES):
        b, hh = c // 2, c % 2
        c0 = hh * 512 + part * 256
        o[b, :, c0:c0 + 256] = res[c]["o"]
    return o


def build_s5():
    S = SEQ
    L = 128
    NCH = S // L
    nc = new_nc()
    di = lambda n, shp, dt=F32: nc.dram_tensor(n, shp, dt, kind="ExternalInput").ap()
    uT = di("uT", [256, S])
    prow = di("prow", [3, 1024])
    pcol = di("pcol", [128, 3, 8])
    BRc = di("BRc", [2, 128, 512]); BIc = di("BIc", [2, 128, 512])
    CRp = di("CRp", [128, 8, 32]); CIp = di("CIp", [128, 8, 32])
    Dd = di("Dd", [128, 2, 128])
    out = nc.dram_tensor("y", [S, 256], F32, kind="ExternalOutput").ap()
    TWO_PI = 2 * math.pi
    with ExitStack() as st:
        k = MK(nc, st)
        od = k.view("od", out)
        negpi = k.sbuf("negpi", [128, 1], F32)
        k.op("vector", lambda e: e.memset(negpi[:], -math.pi), writes=[negpi])
        us = k.sbuf("us", [128, 2, S], F32)
        k.dma("sync", us[:], uT.rearrange("(c p) t -> p c t", p=128), writes=[us])
        CR = k.sbuf("CR", [128, 8, 32], F32); CI = k.sbuf("CI", [128, 8, 32], F32); DD = k.sbuf("DD", [128, 2, 128], F32)
        k.dma("sync", CR[:], CRp, writes=[CR]); k.dma("sync", CI[:], CIp, writes=[CI]); k.dma("sync", DD[:], Dd, writes=[DD])
        k.op("vector", lambda e: e.tensor_scalar(out=CI[:], in0=CI[:], scalar1=-1.0, scalar2=None, op0=ALU.mult), reads=[CI], writes=[CI])
        triT = make_tri(k, "triT", 128, dt=F32)
        Bblk = k.sbuf("Bblk", [128, 2, 4, 2, 128], F32)
        Wn_re = k.sbuf("Wn_re", [128, 1024], F32); Wn_im = k.sbuf("Wn_im", [128, 1024], F32)
        Wp_re = k.sbuf("Wp_re", [128, 8, L + 1], F32); Wp_im = k.sbuf("Wp_im", [128, 8, L + 1], F32)
        iop = k.sbuf("iop", [128, 1], F32)
        iof = k.sbuf("iof", [128, L + 1], F32)
        k.op("gpsimd", lambda e: e.iota(iop[:], pattern=[[0, 1]], base=0, channel_multiplier=1, allow_small_or_imprecise_dtypes=True), writes=[iop])
        k.op("gpsimd", lambda e: e.iota(iof[:], pattern=[[1, L + 1]], base=0, channel_multiplier=0, allow_small_or_imprecise_dtypes=True), writes=[iof])

        def sincos(k, turns, n, dsin, dcos, tmpa, tmpb, tmpi_):
            for dst, shift in ((dsin, 0.0), (dcos, 0.25)):
                k.op("vector", lambda e: e.tensor_scalar(out=tmpa[:], in0=turns[:], scalar1=shift, scalar2=None, op0=ALU.add), reads=[turns], writes=[tmpa])
                k.op("vector", lambda e: e.tensor_copy(out=tmpi_[:], in_=tmpa[:]), reads=[tmpa], writes=[tmpi_])
                k.op("vector", lambda e: e.tensor_copy(out=tmpb[:], in_=tmpi_[:]), reads=[tmpi_], writes=[tmpb])
                k.op("vector", lambda e: e.tensor_tensor(out=tmpa[:], in0=tmpa[:], in1=tmpb[:], op=ALU.subtract), reads=[tmpa, tmpb], writes=[tmpa])
                k.op("vector", lambda e: e.tensor_scalar(out=tmpb[:], in0=tmpa[:], scalar1=0.0, scalar2=None, op0=ALU.is_lt), reads=[tmpa], writes=[tmpb])
                k.op("vector", lambda e: e.tensor_tensor(out=tmpa[:], in0=tmpa[:], in1=tmpb[:], op=ALU.add), reads=[tmpa, tmpb], writes=[tmpa])
                k.op("scalar", lambda e: e.activation(out=tmpb[:], in_=tmpa[:], func=AF.Sin, bias=negpi[:, 0:1], scale=TWO_PI), reads=[tmpa, negpi], writes=[tmpb])
                k.op("vector", lambda e: e.tensor_scalar(out=dst[:], in0=tmpb[:], scalar1=-1.0, scalar2=None, op0=ALU.mult), reads=[tmpb], writes=[dst])

        with ExitStack() as tst:
            T = lambda n, w=1024, dt=F32: k.sbuf(n, [128, w], dt, stack=tst)
            lr = T("lr"); li = T("li"); dtt = T("dtt")
            k.dma("sync", lr[:], prow[0:1, :].to_broadcast([128, 1024]), writes=[lr])
            k.dma("sync", li[:], prow[1:2, :].to_broadcast([128, 1024]), writes=[li])
            k.dma("sync", dtt[:], prow[2:3, :].to_broadcast([128, 1024]), writes=[dtt])
            k.op("vector", lambda e: e.tensor_scalar(out=lr[:], in0=lr[:], scalar1=-1e-4, scalar2=None, op0=ALU.min), reads=[lr], writes=[lr])
            k.op("scalar", lambda e: e.activation(out=dtt[:], in_=dtt[:], func=AF.Exp), reads=[dtt], writes=[dtt])
            a = T("a"); tu = T("tu")
            k.op("vector", lambda e: e.tensor_tensor(out=a[:], in0=lr[:], in1=dtt[:], op=ALU.mult), reads=[lr, dtt], writes=[a])
            k.op("vector", lambda e: e.tensor_tensor(out=tu[:], in0=li[:], in1=dtt[:], op=ALU.mult), reads=[li, dtt], writes=[tu])
            k.op("vector", lambda e: e.tensor_scalar(out=tu[:], in0=tu[:], scalar1=1.0 / TWO_PI, scalar2=None, op0=ALU.mult), reads=[tu], writes=[tu])
            sn = T("sn"); cs = T("cs"); ta = T("ta"); tb_ = T("tb_"); ti = T("ti", dt=I32)
            sincos(k, tu, 1024, sn, cs, ta, tb_, ti)
            mag = T("mag")
            k.op("scalar", lambda e: e.activation(out=mag[:], in_=a[:], func=AF.Exp), reads=[a], writes=[mag])
            k.op("vector", lambda e: e.tensor_tensor(out=cs[:], in0=cs[:], in1=mag[:], op=ALU.mult), reads=[cs, mag], writes=[cs])
            k.op("vector", lambda e: e.tensor_scalar(out=cs[:], in0=cs[:], scalar1=-1.0, scalar2=None, op0=ALU.add), reads=[cs], writes=[cs])
            k.op("vector", lambda e: e.tensor_tensor(out=sn[:], in0=sn[:], in1=mag[:], op=ALU.mult), reads=[sn, mag], writes=[sn])
            k.op("vector", lambda e: e.tensor_tensor(out=ta[:], in0=lr[:], in1=lr[:], op=ALU.mult), reads=[lr], writes=[ta])
            k.op("vector", lambda e: e.tensor_tensor(out=tb_[:], in0=li[:], in1=li[:], op=ALU.mult), reads=[li], writes=[tb_])
            k.op("vector", lambda e: e.tensor_tensor(out=ta[:], in0=ta[:], in1=tb_[:], op=ALU.add), reads=[ta, tb_], writes=[ta])
            k.op("vector", lambda e: e.reciprocal(out=ta[:], in_=ta[:]), reads=[ta], writes=[ta])
            fr = T("fr"); fi = T("fi")
            k.op("vector", lambda e: e.tensor_tensor(out=fr[:], in0=cs[:], in1=lr[:], op=ALU.mult), reads=[cs, lr], writes=[fr])
            k.op("vector", lambda e: e.tensor_tensor(out=tb_[:], in0=sn[:], in1=li[:], op=ALU.mult), reads=[sn, li], writes=[tb_])
            k.op("vector", lambda e: e.tensor_tensor(out=fr[:], in0=fr[:], in1=tb_[:], op=ALU.add), reads=[fr, tb_], writes=[fr])
            k.op("vector", lambda e: e.tensor_tensor(out=fr[:], in0=fr[:], in1=ta[:], op=ALU.mult), reads=[fr, ta], writes=[fr])
            k.op("vector", lambda e: e.tensor_tensor(out=fi[:], in0=sn[:], in1=lr[:], op=ALU.mult), reads=[sn, lr], writes=[fi])
            k.op("vector", lambda e: e.tensor_tensor(out=tb_[:], in0=cs[:], in1=li[:], op=ALU.mult), reads=[cs, li], writes=[tb_])
            k.op("vector", lambda e: e.tensor_tensor(out=fi[:], in0=fi[:], in1=tb_[:], op=ALU.subtract), reads=[fi, tb_], writes=[fi])
            k.op("vector", lambda e: e.tensor_tensor(out=fi[:], in0=fi[:], in1=ta[:], op=ALU.mult), reads=[fi, ta], writes=[fi])
            br = k.sbuf("br", [128, 2, 512], F32, stack=tst); bi = k.sbuf("bi", [128, 2, 512], F32, stack=tst)
            k.dma("sync", br[:], BRc.rearrange("c p n -> p c n"), writes=[br]); k.dma("sync", bi[:], BIc.rearrange("c p n -> p c n"), writes=[bi])
            for kc in range(2):
                frv = fr[:, kc * 512:(kc + 1) * 512].rearrange("p (a b) -> p a b", a=4)
                fiv = fi[:, kc * 512:(kc + 1) * 512].rearrange("p (a b) -> p a b", a=4)
                brv = br[:, kc, :].rearrange("p (a b) -> p a b", a=4); biv = bi[:, kc, :].rearrange("p (a b) -> p a b", a=4)
                tav = ta[:, 0:512].rearrange("p (a b) -> p a b", a=4); tbv = tb_[:, 0:512].rearrange("p (a b) -> p a b", a=4)
                k.op("vector", lambda e: e.tensor_tensor(out=tav, in0=brv, in1=frv, op=ALU.mult), reads=[br, fr], writes=[ta])
                k.op("vector", lambda e: e.tensor_tensor(out=tbv, in0=biv, in1=fiv, op=ALU.mult), reads=[bi, fi], writes=[tb_])
                k.op("vector", lambda e: e.tensor_tensor(out=Bblk[:, kc, :, 0, :], in0=tav, in1=tbv, op=ALU.subtract), reads=[ta, tb_], writes=[Bblk])
                k.op("vector", lambda e: e.tensor_tensor(out=tav, in0=brv, in1=fiv, op=ALU.mult), reads=[br, fi], writes=[ta])
                k.op("vector", lambda e: e.tensor_tensor(out=tbv, in0=biv, in1=frv, op=ALU.mult), reads=[bi, fr], writes=[tb_])
                k.op("vector", lambda e: e.tensor_tensor(out=Bblk[:, kc, :, 1, :], in0=tav, in1=tbv, op=ALU.add), reads=[ta, tb_], writes=[Bblk])
            k.op("vector", lambda e: e.tensor_scalar(out=ta[:], in0=tu[:], scalar1=iop[:, 0:1], scalar2=None, op0=ALU.mult), reads=[tu, iop], writes=[ta])
            sn2 = fr; cs2 = fi
            sincos(k, ta, 1024, sn2, cs2, mag, tb_, ti)
            nio = k.sbuf("nio", [128, 1], F32, stack=tst)
            k.op("vector", lambda e: e.tensor_scalar(out=nio[:], in0=iop[:], scalar1=-1.0, scalar2=None, op0=ALU.mult), reads=[iop], writes=[nio])
            k.op("scalar", lambda e: e.activation(out=mag[:], in_=a[:], func=AF.Exp, scale=nio[:, 0:1]), reads=[a, nio], writes=[mag])
            k.op("vector", lambda e: e.tensor_tensor(out=Wn_re[:], in0=cs2[:], in1=mag[:], op=ALU.mult), reads=[cs2, mag], writes=[Wn_re])
            k.op("vector", lambda e: e.tensor_tensor(out=Wn_im[:], in0=sn2[:], in1=mag[:], op=ALU.mult), reads=[sn2, mag], writes=[Wn_im])
            k.op("vector", lambda e: e.tensor_scalar(out=Wn_im[:], in0=Wn_im[:], scalar1=-1.0, scalar2=None, op0=ALU.mult), reads=[Wn_im], writes=[Wn_im])
            pc = k.sbuf("pc", [128, 3, 8], F32, stack=tst)
            k.dma("sync", pc[:], pcol, writes=[pc])
            k.op("vector", lambda e: e.tensor_scalar(out=pc[:, 0, :], in0=pc[:, 0, :], scalar1=-1e-4, scalar2=None, op0=ALU.min), reads=[pc], writes=[pc])
            k.op("scalar", lambda e: e.activation(out=pc[:, 2, :], in_=pc[:, 2, :], func=AF.Exp), reads=[pc], writes=[pc])
            ac = k.sbuf("ac", [128, 8], F32, stack=tst); tc_ = k.sbuf("tc_", [128, 8], F32, stack=tst)
            k.op("vector", lambda e: e.tensor_tensor(out=ac[:], in0=pc[:, 0, :], in1=pc[:, 2, :], op=ALU.mult), reads=[pc], writes=[ac])
            k.op("vector", lambda e: e.tensor_tensor(out=tc_[:], in0=pc[:, 1, :], in1=pc[:, 2, :], op=ALU.mult), reads=[pc], writes=[tc_])
            k.op("vector", lambda e: e.tensor_scalar(out=tc_[:], in0=tc_[:], scalar1=1.0 / TWO_PI, scalar2=None, op0=ALU.mult), reads=[tc_], writes=[tc_])
            W = L + 1
            tw = k.sbuf("tw", [128, W], F32, stack=tst); twa = k.sbuf("twa", [128, W], F32, stack=tst); twb = k.sbuf("twb", [128, W], F32, stack=tst)
            twi = k.sbuf("twi", [128, W], I32, stack=tst); tws = k.sbuf("tws", [128, W], F32, stack=tst); twc = k.sbuf("twc", [128, W], F32, stack=tst)
            twm = k.sbuf("twm", [128, W], F32, stack=tst)
            for pr in range(8):
                k.op("vector", lambda e: e.tensor_scalar(out=tw[:], in0=iof[:], scalar1=tc_[:, pr:pr + 1], scalar2=None, op0=ALU.mult), reads=[iof, tc_], writes=[tw])
                sincos(k, tw, W, tws, twc, twa, twb, twi)
                k.op("scalar", lambda e: e.activation(out=twm[:], in_=iof[:], func=AF.Exp, scale=ac[:, pr:pr + 1]), reads=[iof, ac], writes=[twm])
                k.op("vector", lambda e: e.tensor_tensor(out=Wp_re[:, pr, :], in0=twc[:], in1=twm[:], op=ALU.mult), reads=[twc, twm], writes=[Wp_re])
                k.op("vector", lambda e: e.tensor_tensor(out=Wp_im[:, pr, :], in0=tws[:], in1=twm[:], op=ALU.mult), reads=[tws, twm], writes=[Wp_im])
            k.barrier()
        psBU = [k.psum(f"psBU{j}", [128, 512], F32) for j in range(4)]
        psC = [k.psum(f"psC{j}", [128, 512], F32) for j in range(3)]
        psY = k.psum("psY", [128, 512], F32)
        V = k.sbuf("V", [128, 8, 2, 128], F32)
        X = [k.sbuf(f"X{i}", [128, 8, 2, 128], F32) for i in range(2)]
        cre = k.sbuf("cre", [128, 8], F32); cim = k.sbuf("cim", [128, 8], F32)
        k.op("vector", lambda e: e.memset(cre[:], 0.0), writes=[cre]); k.op("vector", lambda e: e.memset(cim[:], 0.0), writes=[cim])
        s1 = k.sbuf("s1", [128, 256], F32); s2 = k.sbuf("s2", [128, 256], F32)
        a1 = k.sbuf("a1", [128, 128], F32); a2 = k.sbuf("a2", [128, 128], F32)
        c1 = k.sbuf("c1", [128, 8], F32); c2 = k.sbuf("c2", [128, 8], F32)
        ys = [k.sbuf(f"ys{i}", [128, 256], F32) for i in range(2)]
        ci = 0
        for n in range(NCH):
            ts_ = slice(n * L, (n + 1) * L)
            Xc = X[n % 2]
            for kc in range(2):
                for hf in range(2):
                    P = psBU[kc * 2 + hf]
                    k.op("tensor", lambda e: e.matmul(P[:], lhsT=us[:, kc, ts_], rhs=Bblk[:, kc, hf * 2:hf * 2 + 2, :, :].rearrange("p a r b -> p (a r b)"),
                                                      start=True, stop=True), reads=[us, Bblk], writes=[P])
            for j in range(4):
                P = psBU[j]
                Pv = P[:].rearrange("p (a r b) -> p a r b", a=2, r=2)
                wr = Wn_re[:, j * 256:(j + 1) * 256].rearrange("p (a b) -> p a b", a=2)
                wi = Wn_im[:, j * 256:(j + 1) * 256].rearrange("p (a b) -> p a b", a=2)
                s1v = s1[:].rearrange("p (a b) -> p a b", a=2); s2v = s2[:].rearrange("p (a b) -> p a b", a=2)
                k.op("vector", lambda e: e.tensor_tensor(out=s1v, in0=Pv[:, :, 0, :], in1=wr, op=ALU.mult), reads=[P, Wn_re], writes=[s1])
                k.op("vector", lambda e: e.tensor_tensor(out=s2v, in0=Pv[:, :, 1, :], in1=wi, op=ALU.mult), reads=[P, Wn_im], writes=[s2])
                k.op("vector", lambda e: e.tensor_tensor(out=V[:, 2 * j:2 * j + 2, 0, :], in0=s1v, in1=s2v, op=ALU.subtract), reads=[s1, s2], writes=[V])
                k.op("vector", lambda e: e.tensor_tensor(out=s1v, in0=Pv[:, :, 0, :], in1=wi, op=ALU.mult), reads=[P, Wn_im], writes=[s1])
                k.op("vector", lambda e: e.tensor_tensor(out=s2v, in0=Pv[:, :, 1, :], in1=wr, op=ALU.mult), reads=[P, Wn_re], writes=[s2])
                k.op("vector", lambda e: e.tensor_tensor(out=V[:, 2 * j:2 * j + 2, 1, :], in0=s1v, in1=s2v, op=ALU.add), reads=[s1, s2], writes=[V])
            for pr in range(8):
                PC = psC[ci % 3]; ci += 1
                for ri in range(2):
                    k.op("tensor", lambda e: e.matmul(PC[:, ri * 128:(ri + 1) * 128], lhsT=V[:, pr, ri, :], rhs=triT[:], start=True, stop=True),
                         reads=[V, triT], writes=[PC])
                cr_, cm_ = PC[:, 0:128], PC[:, 128:256]
                wpr = Wp_re[:, pr, 0:L]; wpi = Wp_im[:, pr, 0:L]
                k.op("vector", lambda e: e.scalar_tensor_tensor(out=a1[:], in0=cr_, scalar=cre[:, pr:pr + 1], in1=wpr, op0=ALU.add, op1=ALU.mult), reads=[PC, cre, Wp_re], writes=[a1])
                k.op("vector", lambda e: e.scalar_tensor_tensor(out=a2[:], in0=cm_, scalar=cim[:, pr:pr + 1], in1=wpi, op0=ALU.add, op1=ALU.mult), reads=[PC, cim, Wp_im], writes=[a2])
                k.op("vector", lambda e: e.tensor_tensor(out=Xc[:, pr, 0, :], in0=a1[:], in1=a2[:], op=ALU.subtract), reads=[a1, a2], writes=[Xc])
                k.op("vector", lambda e: e.scalar_tensor_tensor(out=a1[:], in0=cm_, scalar=cim[:, pr:pr + 1], in1=wpr, op0=ALU.add, op1=ALU.mult), reads=[PC, cim, Wp_re], writes=[a1])
                k.op("vector", lambda e: e.scalar_tensor_tensor(out=a2[:], in0=cr_, scalar=cre[:, pr:pr + 1], in1=wpi, op0=ALU.add, op1=ALU.mult), reads=[PC, cre, Wp_im], writes=[a2])
                k.op("vector", lambda e: e.tensor_tensor(out=Xc[:, pr, 1, :], in0=a1[:], in1=a2[:], op=ALU.add), reads=[a1, a2], writes=[Xc])
            xr = Xc[:, :, 0, L - 1]; xi = Xc[:, :, 1, L - 1]
            l_re = Wp_re[:, :, 1]; l_im = Wp_im[:, :, 1]
            k.op("vector", lambda e: e.tensor_tensor(out=c1[:], in0=xr, in1=l_re, op=ALU.mult), reads=[Xc, Wp_re], writes=[c1])
            k.op("vector", lambda e: e.tensor_tensor(out=c2[:], in0=xi, in1=l_im, op=ALU.mult), reads=[Xc, Wp_im], writes=[c2])
            k.op("vector", lambda e: e.tensor_tensor(out=cre[:], in0=c1[:], in1=c2[:], op=ALU.subtract), reads=[c1, c2], writes=[cre])
            k.op("vector", lambda e: e.tensor_tensor(out=c1[:], in0=xr, in1=l_im, op=ALU.mult), reads=[Xc, Wp_im], writes=[c1])
            k.op("vector", lambda e: e.tensor_tensor(out=c2[:], in0=xi, in1=l_re, op=ALU.mult), reads=[Xc, Wp_re], writes=[c2])
            k.op("vector", lambda e: e.tensor_tensor(out=cim[:], in0=c1[:], in1=c2[:], op=ALU.add), reads=[c1, c2], writes=[cim])
            first = True
            for kc in range(2):
                k.op("tensor", lambda e: e.matmul(psY[:, kc * 128:(kc + 1) * 128], lhsT=us[:, kc, ts_], rhs=DD[:, kc, :], start=first, stop=False),
                     reads=[us, DD], writes=[psY])
                first = False
            for pr in range(8):
                k.op("tensor", lambda e: e.matmul(psY[:, pr * 32:(pr + 1) * 32], lhsT=Xc[:, pr, 0, :], rhs=CR[:, pr, :], start=False, stop=False),
                     reads=[Xc, CR], writes=[psY])
                k.op("tensor", lambda e: e.matmul(psY[:, pr * 32:(pr + 1) * 32], lhsT=Xc[:, pr, 1, :], rhs=CI[:, pr, :], start=False, stop=(pr == 7)),
                     reads=[Xc, CI], writes=[psY])
            Y = ys[n % 2]
            k.op("scalar", lambda e: e.activation(out=Y[:], in_=psY[:, 0:256], func=AF.Copy), reads=[psY], writes=[Y])
            k.dma("sync", out[ts_, :], Y[:], reads=[Y], writes=[od])
        k.finish([od])
    return nc


def s5_inputs(proj, p, l):
    ins = []
    for c in range(NCORES):
        b, gh = c // 2, c % 2
        G = slice(gh * 16, gh * 16 + 16)
        lam_re = p["s5_lambda_re"][l][G]; lam_im = p["s5_lambda_im"][l][G]
        ls = np.repeat(p["s5_log_step"][l][G][:, None], 64, 1)
        rows = np.stack([lam_re.reshape(-1), lam_im.reshape(-1), ls.reshape(-1)], 0).astype(np.float32)
        cols = np.stack([a.reshape(8, 128).T for a in (lam_re, lam_im, ls)], 1).astype(np.float32)
        b_re = p["s5_b_re"][l][G]; b_im = p["s5_b_im"][l][G]
        c_re = p["s5_c_re"][l][G]; c_im = p["s5_c_im"][l][G]
        dsk = p["s5_d"][l][G]
        BR = np.zeros((2, 128, 4, 128), np.float32); BI = np.zeros((2, 128, 4, 128), np.float32)
        CR = np.zeros((128, 8, 32), np.float32); CI = np.zeros((128, 8, 32), np.float32)
        DDm = np.zeros((128, 2, 128), np.float32)
        for g in range(16):
            kc, g8 = g // 8, g % 8
            pr4, gl = g8 // 2, g8 % 2
            BR[kc, g8 * 16:(g8 + 1) * 16, pr4, gl * 64:(gl + 1) * 64] = b_re[g].T
            BI[kc, g8 * 16:(g8 + 1) * 16, pr4, gl * 64:(gl + 1) * 64] = b_im[g].T
            pr = g // 2
            CR[gl * 64:(gl + 1) * 64, pr, gl * 16:(gl + 1) * 16] = c_re[g].T
            CI[gl * 64:(gl + 1) * 64, pr, gl * 16:(gl + 1) * 16] = c_im[g].T
            for h in range(16):
                DDm[g8 * 16 + h, kc, g8 * 16 + h] = dsk[g, h]
        ins.append({"uT": _c(proj[b][:, gh * 256:(gh + 1) * 256].T), "prow": _c(rows), "pcol": _c(cols),
                    "BRc": _c(BR.reshape(2, 128, 512)), "BIc": _c(BI.reshape(2, 128, 512)),
                    "CRp": CR, "CIp": CI, "Dd": DDm})
    return ins


def s5_gather(res):
    y = np.zeros((BATCH, SEQ, 512), np.float32)
    for c in range(NCORES):
        b, gh = c // 2, c % 2
        y[b, :, gh * 256:(gh + 1) * 256] = res[c]["y"]
    return y


def build_gdn():
    S = SEQ
    C = 64
    NCK = S // C
    nc = new_nc()
    di = lambda n, shp, dt=F32: nc.dram_tensor(n, shp, dt, kind="ExternalInput").ap()
    qkvT = di("qkvT", [768, S]); convw = di("convw", [128, 6, 4])
    z = di("z", [S, 256]); garow = di("garow", [2, S]); gbrow = di("gbrow", [2, S])
    hp_ = di("hp", [1, 4])
    gnorm = di("gnorm", [1, 128])
    out = nc.dram_tensor("o", [S, 256], F32, kind="ExternalOutput").ap()
    with ExitStack() as st:
        k = MK(nc, st)
        od = k.view("od", out)
        ps = [k.psum(f"ps{i}", [128, 512], F32) for i in range(8)]
        ident = make_ident(k, 128, F32)
        ones_f = k.sbuf("ones_f", [128, 128], F32)
        k.op("vector", lambda e: e.memset(ones_f[:], 1.0), writes=[ones_f])
        m_incl = make_tri(k, "m_incl", C, dt=F32)
        m_strict = make_tri(k, "m_strict", C, dt=F32, strict=True)
        negm = k.sbuf("negm", [C, C], F32)
        k.op("vector", lambda e: e.tensor_scalar(out=negm[:], in0=m_incl[:], scalar1=-1.0, scalar2=30000.0, op0=ALU.add, op1=ALU.mult),
             reads=[m_incl], writes=[negm])
        cw = k.sbuf("cw", [128, 6, 4], F32); k.dma("sync", cw[:], convw, writes=[cw])
        hp = k.sbuf("hps", [1, 4], F32); k.dma("sync", hp[:], hp_, writes=[hp])
        gn = k.sbuf("gn", [C, 128], F32); k.dma("sync", gn[:], gnorm.to_broadcast([C, 128]), writes=[gn])
        eps6 = k.sbuf("eps6", [128, 1], F32); k.op("vector", lambda e: e.memset(eps6[:], 1e-6), writes=[eps6])
        k.op("scalar", lambda e: e.activation(out=hp[:, 0:2], in_=hp[:, 0:2], func=AF.Exp), reads=[hp], writes=[hp])
        xt = k.sbuf("xt", [128, S + 3], F32)
        y = k.sbuf("yqkv", [128, 3, S], F32)
        Bb = k.sbuf("Bb", [128, S], F32); Beg = k.sbuf("Beg", [128, S], F32); Bend = k.sbuf("Bend", [128, S], F32)
        zc = [k.sbuf(f"zc{i}", [C, 128], F32) for i in range(2)]
        rA = k.sbuf("rA", [1, S], F32); rB = k.sbuf("rB", [1, S], F32); rC = k.sbuf("rC", [1, S], F32)
        negones = k.sbuf("negones", [1, C], F32)
        k.op("vector", lambda e: e.memset(negones[:], -1.0), writes=[negones])
        sqt = k.sbuf("sqt", [128, 512], F32); rst = k.sbuf("rst", [128, 512], F32)
        Sst = k.sbuf("Sst", [128, 128], F32)
        def two(name, shp):
            return [k.sbuf(f"{name}{i}", shp, F32) for i in range(2)]
        kb = k.sbuf("kb", [128, C], F32); kbg = k.sbuf("kbg", [128, C], F32); ke = k.sbuf("ke", [128, C], F32); vb = k.sbuf("vbT", [128, C], F32)
        qd2 = two("qd", [128, C]); tok2 = two("tok", [C, 3, 128]); AT2 = two("AT", [C, C]); TT2 = two("TT", [C, C]); nwT2 = two("nwT", [128, C])
        decT = k.sbuf("decT", [C, C], F32); decTs = k.sbuf("decTs", [C, C], F32)
        Pk = [k.sbuf(f"Pk{i}", [C, C], F32) for i in range(2)]; PTk = [k.sbuf(f"PTk{i}", [C, C], F32) for i in range(2)]
        vnew = k.sbuf("vnew", [C, 128], F32)
        osb = [k.sbuf(f"osb{i}", [C, 128], F32) for i in range(2)]
        o2 = k.sbuf("o2", [C, 128], F32); ssq = k.sbuf("ssq", [C, 1], F32)
        for j in range(2):
            for t3 in range(3):
                ti = j * 3 + t3
                k.op("vector", lambda e: e.memset(xt[:, 0:3], 0.0), writes=[xt])
                k.dma("sync", xt[:, 3:S + 3], qkvT[ti * 128:(ti + 1) * 128, :], writes=[xt])
                Y = y[:, t3, :]
                k.op("vector", lambda e: e.tensor_scalar(out=Y, in0=xt[:, 3:S + 3], scalar1=cw[:, ti, 3:4], scalar2=None, op0=ALU.mult), reads=[xt, cw], writes=[y])
                for tap in range(3):
                    k.op("vector", lambda e: e.scalar_tensor_tensor(out=Y, in0=xt[:, tap:S + tap], scalar=cw[:, ti, tap:tap + 1], in1=Y, op0=ALU.mult, op1=ALU.add),
                         reads=[xt, cw, y], writes=[y])
                k.op("scalar", lambda e: e.activation(out=Y, in_=Y, func=AF.Silu), reads=[y], writes=[y])
                if t3 < 2:
                    for tb in range(S // 512):
                        sl = slice(tb * 512, (tb + 1) * 512)
                        k.op("scalar", lambda e: e.activation(out=sqt[:], in_=y[:, t3, sl], func=AF.Square), reads=[y], writes=[sqt])
                        k.op("tensor", lambda e: e.matmul(ps[0][:], lhsT=ones_f[:], rhs=sqt[:], start=True, stop=True), reads=[ones_f, sqt], writes=[ps[0]])
                        k.op("scalar", lambda e: e.activation(out=rst[:], in_=ps[0][:], func=AF.Sqrt, bias=eps6[:, 0:1], scale=1.0), reads=[ps[0], eps6], writes=[rst])
                        k.op("vector", lambda e: e.reciprocal(out=rst[:], in_=rst[:]), reads=[rst], writes=[rst])
                        if t3 == 0:
                            k.op("vector", lambda e: e.scalar_tensor_tensor(out=y[:, t3, sl], in0=y[:, t3, sl], scalar=128.0 ** -0.5, in1=rst[:], op0=ALU.mult, op1=ALU.mult),
                                 reads=[y, rst], writes=[y])
                        else:
                            k.op("vector", lambda e: e.tensor_tensor(out=y[:, t3, sl], in0=y[:, t3, sl], in1=rst[:], op=ALU.mult), reads=[y, rst], writes=[y])
            qT, kT, vT = y[:, 0, :], y[:, 1, :], y[:, 2, :]
            k.dma("sync", rA[:], garow[j:j + 1, :], writes=[rA])
            k.op("scalar", lambda e: e.activation(out=rA[:], in_=rA[:], func=AF.Exp, bias=hp[0:1, 2 + j:3 + j], scale=1.0), reads=[rA, hp], writes=[rA])
            k.op("vector", lambda e: e.tensor_scalar(out=rA[:], in0=rA[:], scalar1=1.0, scalar2=None, op0=ALU.add), reads=[rA], writes=[rA])
            k.op("scalar", lambda e: e.activation(out=rA[:], in_=rA[:], func=AF.Ln), reads=[rA], writes=[rA])
            k.op("vector", lambda e: e.tensor_scalar(out=rA[:], in0=rA[:], scalar1=hp[0:1, j:j + 1], scalar2=-1.0, op0=ALU.mult, op1=ALU.mult), reads=[rA, hp], writes=[rA])
            k.op("vector", lambda e: e.memset(rC[:], 1.0), writes=[rC])
            k.op("vector", lambda e: e.memset(rC[:].rearrange("p (n c) -> p n c", c=C)[:, :, 0:1], 0.0), writes=[rC])
            k.op("vector", lambda e: e.tensor_tensor_scan(out=rB[:], data0=rC[:], data1=rA[:], initial=0.0, op0=ALU.mult, op1=ALU.add), reads=[rA, rC], writes=[rB])

            def bcast(row, B):
                for tb in range(S // 512):
                    sl = slice(tb * 512, (tb + 1) * 512)
                    P = ps[1 + tb % 2]
                    k.op("tensor", lambda e: e.matmul(P[:], lhsT=ones_f[0:1, :], rhs=row[0:1, sl], start=True, stop=True), reads=[ones_f, row], writes=[P])
                    k.op("scalar", lambda e: e.activation(out=B[:, sl], in_=P[:], func=AF.Copy), reads=[P], writes=[B])

            k.dma("sync", rA[:], gbrow[j:j + 1, :], writes=[rA])
            k.op("scalar", lambda e: e.activation(out=rA[:], in_=rA[:], func=AF.Sigmoid), reads=[rA], writes=[rA])
            bcast(rA, Bb)
            for n in range(NCK):
                cs = slice(n * C, (n + 1) * C)
                last = n * C + C - 1
                k.op("vector", lambda e: e.tensor_scalar(out=rC[0:1, cs], in0=rB[0:1, cs], scalar1=rB[0:1, last:last + 1], scalar2=-1.0,
                                                         op0=ALU.subtract, op1=ALU.mult), reads=[rB], writes=[rC])
            k.op("scalar", lambda e: e.activation(out=rC[:], in_=rC[:], func=AF.Exp), reads=[rC], writes=[rC])
            bcast(rC, Bend)
            k.op("scalar", lambda e: e.activation(out=rC[:], in_=rB[:], func=AF.Exp), reads=[rB], writes=[rC])
            bcast(rC, Beg)
            k.op("vector", lambda e: e.memset(Sst[:], 0.0), writes=[Sst])
            def phaseA(n):
                s_ = n % 2
                qd, tok, AT, TT, nwT = qd2[s_], tok2[s_], AT2[s_], TT2[s_], nwT2[s_]
                cs = slice(n * C, (n + 1) * C)
                k.op("vector", lambda e: e.tensor_tensor(out=kb[:], in0=kT[:, cs], in1=Bb[:, cs], op=ALU.mult), reads=[y, Bb], writes=[kb]); yield
                k.op("vector", lambda e: e.tensor_tensor(out=kbg[:], in0=kb[:], in1=Beg[:, cs], op=ALU.mult), reads=[kb, Beg], writes=[kbg]); yield
                k.op("vector", lambda e: e.tensor_tensor(out=qd[:], in0=qT[:, cs], in1=Beg[:, cs], op=ALU.mult), reads=[y, Beg], writes=[qd]); yield
                k.op("vector", lambda e: e.tensor_tensor(out=ke[:], in0=kT[:, cs], in1=Bend[:, cs], op=ALU.mult), reads=[y, Bend], writes=[ke]); yield
                k.op("vector", lambda e: e.tensor_tensor(out=vb[:], in0=vT[:, cs], in1=Bb[:, cs], op=ALU.mult), reads=[y, Bb], writes=[vb]); yield
                PD = ps[1]
                k.op("tensor", lambda e: e.matmul(PD[0:C, 0:C], lhsT=ones_f[0:1, 0:C], rhs=rB[0:1, cs], start=True, stop=False), reads=[ones_f, rB], writes=[PD]); yield
                k.op("tensor", lambda e: e.matmul(PD[0:C, 0:C], lhsT=rB[0:1, cs], rhs=negones[0:1, 0:C], start=False, stop=False), reads=[negones, rB], writes=[PD]); yield
                k.op("tensor", lambda e: e.matmul(PD[0:C, 0:C], lhsT=ident[0:C, 0:C], rhs=negm[:], start=False, stop=True), reads=[ident, negm], writes=[PD]); yield
                k.op("scalar", lambda e: e.activation(out=decT[:], in_=PD[0:C, 0:C], func=AF.Exp), reads=[PD], writes=[decT]); yield
                k.op("vector", lambda e: e.tensor_tensor(out=decTs[:], in0=decT[:], in1=m_strict[:], op=ALU.mult), reads=[decT, m_strict], writes=[decTs]); yield
                PK = ps[2]
                k.op("tensor", lambda e: e.matmul(PK[0:C, 0:C], lhsT=kT[:, cs], rhs=kb[:], start=True, stop=True), reads=[y, kb], writes=[PK]); yield
                k.op("tensor", lambda e: e.matmul(PK[0:C, C:2 * C], lhsT=kT[:, cs], rhs=qT[:, cs], start=True, stop=True), reads=[y], writes=[PK]); yield
                k.op("vector", lambda e: e.scalar_tensor_tensor(out=PTk[0][:], in0=PK[0:C, 0:C], scalar=-1.0, in1=decTs[:], op0=ALU.mult, op1=ALU.mult),
                     reads=[PK, decTs], writes=[PTk[0]]); yield
                k.op("vector", lambda e: e.tensor_tensor(out=AT[:], in0=PK[0:C, C:2 * C], in1=decT[:], op=ALU.mult), reads=[PK, decT], writes=[AT]); yield
                PX = ps[3]
                k.op("tensor", lambda e: e.transpose(out=PX[0:C, 0:C], in_=PTk[0][:], identity=ident[0:C, 0:C]), reads=[PTk[0], ident], writes=[PX]); yield
                k.op("scalar", lambda e: e.activation(out=Pk[0][:], in_=PX[0:C, 0:C], func=AF.Copy), reads=[PX], writes=[Pk[0]]); yield
                k.op("vector", lambda e: e.tensor_tensor(out=TT[:], in0=PTk[0][:], in1=ident[0:C, 0:C], op=ALU.add), reads=[PTk[0], ident], writes=[TT]); yield
                for lv in range(1, 6):
                    a_, b_ = (lv - 1) % 2, lv % 2
                    PP = ps[4]
                    k.op("tensor", lambda e: e.matmul(PP[0:C, 0:C], lhsT=PTk[a_][:], rhs=Pk[a_][:], start=True, stop=True), reads=[PTk[a_], Pk[a_]], writes=[PP]); yield
                    if lv < 5:
                        k.op("tensor", lambda e: e.matmul(PP[0:C, C:2 * C], lhsT=Pk[a_][:], rhs=PTk[a_][:], start=True, stop=True), reads=[PTk[a_], Pk[a_]], writes=[PP]); yield
                    k.op("scalar", lambda e: e.activation(out=Pk[b_][:], in_=PP[0:C, 0:C], func=AF.Copy), reads=[PP], writes=[Pk[b_]]); yield
                    if lv < 5:
                        k.op("vector", lambda e: e.tensor_copy(out=PTk[b_][:], in_=PP[0:C, C:2 * C]), reads=[PP], writes=[PTk[b_]]); yield
                    PU = ps[3]
                    k.op("tensor", lambda e: e.matmul(PU[0:C, 0:C], lhsT=Pk[b_][:], rhs=TT[:], start=True, stop=True), reads=[Pk[b_], TT], writes=[PU]); yield
                    k.op("vector", lambda e: e.tensor_tensor(out=TT[:], in0=TT[:], in1=PU[0:C, 0:C], op=ALU.add), reads=[TT, PU], writes=[TT]); yield
                for (ii, src) in ((0, vb), (1, kbg), (2, ke)):
                    PT_ = ps[1 + ii % 2]
                    k.op("tensor", lambda e: e.transpose(out=PT_[0:C, 0:128], in_=src[:], identity=ident[:]), reads=[src, ident], writes=[PT_]); yield
                    k.op("scalar", lambda e: e.activation(out=tok[:, ii, :], in_=PT_[0:C, 0:128], func=AF.Copy), reads=[PT_], writes=[tok]); yield
                PW = ps[4]
                k.op("tensor", lambda e: e.matmul(PW[:, 0:C], lhsT=tok[:, 1, :], rhs=TT[:], start=True, stop=True), reads=[tok, TT], writes=[PW]); yield
                k.op("vector", lambda e: e.tensor_scalar(out=nwT[:], in0=PW[:, 0:C], scalar1=-1.0, scalar2=None, op0=ALU.mult), reads=[PW], writes=[nwT]); yield

            def phaseB(n):
                s_ = n % 2
                qd, tok, AT, TT, nwT = qd2[s_], tok2[s_], AT2[s_], TT2[s_], nwT2[s_]
                cs = slice(n * C, (n + 1) * C)
                last = n * C + C - 1
                PV = ps[5]
                k.op("tensor", lambda e: e.matmul(PV[0:C, 0:128], lhsT=TT[:], rhs=tok[:, 0, :], start=True, stop=False), reads=[TT, tok], writes=[PV]); yield
                k.op("tensor", lambda e: e.matmul(PV[0:C, 0:128], lhsT=nwT[:], rhs=Sst[:], start=False, stop=True), reads=[nwT, Sst], writes=[PV]); yield
                k.op("scalar", lambda e: e.activation(out=vnew[:], in_=PV[0:C, 0:128], func=AF.Copy), reads=[PV], writes=[vnew]); yield
                PO = ps[6]
                k.op("tensor", lambda e: e.matmul(PO[0:C, 0:128], lhsT=qd[:], rhs=Sst[:], start=True, stop=False), reads=[qd, Sst], writes=[PO]); yield
                k.op("tensor", lambda e: e.matmul(PO[0:C, 0:128], lhsT=AT[:], rhs=vnew[:], start=False, stop=True), reads=[AT, vnew], writes=[PO]); yield
                PS_ = ps[7]
                k.op("tensor", lambda e: e.matmul(PS_[:, 0:128], lhsT=tok[:, 2, :], rhs=vnew[:], start=True, stop=True), reads=[tok, vnew], writes=[PS_]); yield
                k.op("vector", lambda e: e.scalar_tensor_tensor(out=Sst[:], in0=Sst[:], scalar=Beg[:, last:last + 1], in1=PS_[:, 0:128], op0=ALU.mult, op1=ALU.add),
                     reads=[Sst, Beg, PS_], writes=[Sst]); yield
                OS = osb[n % 2]
                k.op("scalar", lambda e: e.activation(out=o2[:], in_=PO[0:C, 0:128], func=AF.Square, accum_out=ssq[:, 0:1]), reads=[PO], writes=[o2, ssq]); yield
                k.op("scalar", lambda e: e.activation(out=ssq[:], in_=ssq[:], func=AF.Sqrt, bias=eps6[0:C, 0:1], scale=1.0 / 128), reads=[ssq, eps6], writes=[ssq]); yield
                k.op("vector", lambda e: e.reciprocal(out=ssq[:], in_=ssq[:]), reads=[ssq], writes=[ssq]); yield
                k.op("vector", lambda e: e.scalar_tensor_tensor(out=OS[:], in0=PO[0:C, 0:128], scalar=ssq[:, 0:1], in1=gn[:], op0=ALU.mult, op1=ALU.mult),
                     reads=[PO, ssq, gn], writes=[OS]); yield
                ZC = zc[n % 2]
                k.dma("sync", ZC[:], z[cs, j * 128:(j + 1) * 128], writes=[ZC]); yield
                k.op("scalar", lambda e: e.activation(out=ZC[:], in_=ZC[:], func=AF.Silu), reads=[ZC], writes=[ZC]); yield
                k.op("vector", lambda e: e.tensor_tensor(out=OS[:], in0=OS[:], in1=ZC[:], op=ALU.mult), reads=[OS, ZC], writes=[OS]); yield
                k.dma("sync", out[cs, j * 128:(j + 1) * 128], OS[:], reads=[OS], writes=[od]); yield

            for _ in phaseA(0):
                pass
            for n in range(NCK):
                gB = phaseB(n)
                gA = phaseA(n + 1) if n + 1 < NCK else iter(())
                doneA = doneB = False
                while not (doneA and doneB):
                    for _ in range(3):
                        if not doneA:
                            try:
                                next(gA)
                            except StopIteration:
                                doneA = True
                    if not doneB:
                        try:
                            next(gB)
                        except StopIteration:
                            doneB = True
        k.finish([od])
    return nc


def gdn_inputs(proj, p, l):
    ins = []
    cwf = p["gdn_conv"][l]
    for c in range(NCORES):
        b, hh = c // 2, c % 2
        pb = proj[b]
        tiles = []; cws = []
        for j in range(2):
            H = 2 * hh + j
            for base, off in ((C_GQ, 0), (C_GK, 512), (C_GV, 1024)):
                tiles.append(pb[:, base + H * 128: base + (H + 1) * 128].T)
                cws.append(cwf[:, off + H * 128: off + (H + 1) * 128].T)
        ins.append({
            "qkvT": _c(np.concatenate(tiles, 0)), "convw": _c(np.stack(cws, 1)),
            "z": _c(pb[:, C_GZ + hh * 256: C_GZ + (hh + 1) * 256]),
            "garow": _c(pb[:, C_GA + 2 * hh: C_GA + 2 * hh + 2].T), "gbrow": _c(pb[:, C_GB + 2 * hh: C_GB + 2 * hh + 2].T),
            "hp": _c(np.concatenate([p["gdn_a_log"][l][2 * hh:2 * hh + 2], p["gdn_dt_bias"][l][2 * hh:2 * hh + 2]])[None].astype(np.float32)),
            "gnorm": _c(p["gdn_out_norm"][l][None]),
        })
    return ins


def gdn_gather(res):
    y = np.zeros((BATCH, SEQ, 512), np.float32)
    for c in range(NCORES):
        b, hh = c // 2, c % 2
        y[b, :, hh * 256:(hh + 1) * 256] = res[c]["o"]
    return y


def ln_setup(k, g_dram, b_dram):
    gB = k.sbuf("ln_gB", [128, D], F32); bB = k.sbuf("ln_bB", [128, D], F32)
    k.dma("sync", gB[:], g_dram.to_broadcast([128, D]), writes=[gB])
    k.dma("sync", bB[:], b_dram.to_broadcast([128, D]), writes=[bB])
    st_ = {"gB": gB, "bB": bB,
           "junk": k.sbuf("ln_junk", [128, D], F32),
           "s1": k.sbuf("ln_s1", [128, 1], F32), "s2": k.sbuf("ln_s2", [128, 1], F32),
           "m2": k.sbuf("ln_m2", [128, 1], F32), "eps": k.sbuf("ln_eps", [128, 1], F32)}
    k.op("vector", lambda e: e.memset(st_["eps"][:], 1e-5), writes=[st_["eps"]])
    return st_


def ln_rows(k, L, r, dst):
    junk, s1, s2, m2 = L["junk"], L["s1"], L["s2"], L["m2"]
    k.op("scalar", lambda e: e.activation(out=junk[:], in_=r[:], func=AF.Copy, accum_out=s1[:, 0:1]), reads=[r], writes=[junk, s1])
    k.op("scalar", lambda e: e.activation(out=junk[:], in_=r[:], func=AF.Square, accum_out=s2[:, 0:1]), reads=[r], writes=[junk, s2])
    k.op("vector", lambda e: e.tensor_scalar(out=s1[:], in0=s1[:], scalar1=1.0 / D, scalar2=None, op0=ALU.mult), reads=[s1], writes=[s1])
    k.op("vector", lambda e: e.tensor_tensor(out=m2[:], in0=s1[:], in1=s1[:], op=ALU.mult), reads=[s1], writes=[m2])
    k.op("vector", lambda e: e.scalar_tensor_tensor(out=s2[:], in0=s2[:], scalar=1.0 / D, in1=m2[:], op0=ALU.mult, op1=ALU.subtract), reads=[s2, m2], writes=[s2])
    k.op("scalar", lambda e: e.activation(out=s2[:], in_=s2[:], func=AF.Sqrt, bias=L["eps"][:, 0:1], scale=1.0), reads=[s2, L["eps"]], writes=[s2])
    k.op("vector", lambda e: e.reciprocal(out=s2[:], in_=s2[:]), reads=[s2], writes=[s2])
    k.op("vector", lambda e: e.tensor_scalar(out=junk[:], in0=r[:], scalar1=s1[:, 0:1], scalar2=s2[:, 0:1], op0=ALU.subtract, op1=ALU.mult), reads=[r, s1, s2], writes=[junk])
    k.op("vector", lambda e: e.tensor_tensor(out=junk[:], in0=junk[:], in1=L["gB"][:], op=ALU.mult), reads=[junk, L["gB"]], writes=[junk])
    k.op("vector", lambda e: e.tensor_tensor(out=dst[:], in0=junk[:], in1=L["bB"][:], op=ALU.add), reads=[junk, L["bB"]], writes=[dst])


def build_mixout():
    T = TPC
    nc = new_nc()
    di = lambda n, shp, dt=F32: nc.dram_tensor(n, shp, dt, kind="ExternalInput").ap()
    s5T = di("s5T", [512, T]); mlaT = di("mlaT", [1024, T]); gdnT = di("gdnT", [512, T]); x = di("x", [T, D])
    wglu = di("wglu", [512, 512]); bglu = di("bglu", [128, 4]); gs5 = di("gs5", [128, 4]); gmla = di("gmla", [128, 8])
    wout = di("wout", [D, D]); lng = di("lng", [1, D]); lnb = di("lnb", [1, D])
    out = nc.dram_tensor("x1", [T, D], F32, kind="ExternalOutput").ap()
    with ExitStack() as st:
        k = MK(nc, st)
        od = k.view("od", out)
        ps = [k.psum(f"ps{i}", [128, 512], F32) for i in range(8)]
        ones_f = k.sbuf("ones_f", [128, 128], F32)
        k.op("vector", lambda e: e.memset(ones_f[:], 1.0), writes=[ones_f])
        eps6 = k.sbuf("eps6", [128, 1], F32); k.op("vector", lambda e: e.memset(eps6[:], 1e-6), writes=[eps6])
        Wout = k.sbuf("Wout", [128, 16, D], BF16)
        k.dma("gpsimd", Wout[:], wout.rearrange("(c p) n -> p c n", p=128), writes=[Wout])
        Wglu = k.sbuf("Wglu", [128, 4, 512], BF16)
        k.dma("gpsimd", Wglu[:], wglu.rearrange("(c p) n -> p c n", p=128), writes=[Wglu])
        bg = k.sbuf("bg", [128, 4], F32); g5 = k.sbuf("g5", [128, 4], F32); gm = k.sbuf("gm", [128, 8], F32)
        k.dma("sync", bg[:], bglu, writes=[bg]); k.dma("sync", g5[:], gs5, writes=[g5]); k.dma("sync", gm[:], gmla, writes=[gm])
        L = ln_setup(k, lng, lnb)
        cat = k.sbuf("cat", [128, 16, 512], BF16)
        a = k.sbuf("a", [128, 4, 512], F32); xin = k.sbuf("xin", [128, 4, 512], F32); yb = k.sbuf("yb", [128, 4, 512], BF16)
        y2 = k.sbuf("y2", [128, 4, 512], F32)
        mi = k.sbuf("mi", [128, 8, 512], F32)
        rstd = k.sbuf("rstd", [128, 512], F32)
        xt = [k.sbuf(f"xt{i}", [128, D], F32) for i in range(2)]
        r = [k.sbuf(f"r{i}", [128, D], F32) for i in range(2)]
        for tb in range(T // 512):
            sl = slice(tb * 512, (tb + 1) * 512)
            k.dma("sync", xin[:], s5T[:, sl].rearrange("(c p) t -> p c t", p=128), writes=[xin])
            k.op("scalar", lambda e: e.activation(out=a[:], in_=xin[:], func=AF.Square), reads=[xin], writes=[a])
            k.op("vector", lambda e: e.tensor_scalar(out=a[:], in0=a[:], scalar1=0.044715, scalar2=1.0, op0=ALU.mult, op1=ALU.add), reads=[a], writes=[a])
            k.op("vector", lambda e: e.tensor_tensor(out=a[:], in0=a[:], in1=xin[:], op=ALU.mult), reads=[a, xin], writes=[a])
            k.op("scalar", lambda e: e.activation(out=a[:], in_=a[:], func=AF.Tanh, scale=0.7978845608028654), reads=[a], writes=[a])
            k.op("vector", lambda e: e.tensor_scalar(out=a[:], in0=a[:], scalar1=1.0, scalar2=0.5, op0=ALU.add, op1=ALU.mult), reads=[a], writes=[a])
            k.op("vector", lambda e: e.tensor_tensor(out=a[:], in0=a[:], in1=xin[:], op=ALU.mult), reads=[a, xin], writes=[a])
            k.op("vector", lambda e: e.tensor_copy(out=yb[:], in_=a[:]), reads=[a], writes=[yb])
            for oc in range(4):
                P = ps[oc]
                for kc in range(4):
                    k.op("tensor", lambda e: e.matmul(P[:], lhsT=Wglu[:, kc, oc * 128:(oc + 1) * 128], rhs=yb[:, kc, :], start=(kc == 0), stop=(kc == 3)),
                         reads=[Wglu, yb], writes=[P])
                k.op("scalar", lambda e: e.activation(out=y2[:, oc, :], in_=P[:], func=AF.Sigmoid, bias=bg[:, oc:oc + 1], scale=1.0), reads=[P, bg], writes=[y2])
            k.op("vector", lambda e: e.tensor_tensor(out=y2[:], in0=y2[:], in1=a[:], op=ALU.mult), reads=[y2, a], writes=[y2])
            k.op("scalar", lambda e: e.activation(out=a[:], in_=y2[:], func=AF.Square), reads=[y2], writes=[a])
            P = ps[4]
            for kc in range(4):
                k.op("tensor", lambda e: e.matmul(P[:], lhsT=ones_f[:], rhs=a[:, kc, :], start=(kc == 0), stop=(kc == 3)), reads=[ones_f, a], writes=[P])
            k.op("scalar", lambda e: e.activation(out=rstd[:], in_=P[:], func=AF.Sqrt, bias=eps6[:, 0:1], scale=1.0 / 512), reads=[P, eps6], writes=[rstd])
            k.op("vector", lambda e: e.reciprocal(out=rstd[:], in_=rstd[:]), reads=[rstd], writes=[rstd])
            for kc in range(4):
                k.op("vector", lambda e: e.scalar_tensor_tensor(out=cat[:, kc, :], in0=y2[:, kc, :], scalar=g5[:, kc:kc + 1], in1=rstd[:], op0=ALU.mult, op1=ALU.mult),
                     reads=[y2, g5, rstd], writes=[cat])
            k.dma("sync", mi[:], mlaT[:, sl].rearrange("(c p) t -> p c t", p=128), writes=[mi])
            P = ps[5]
            for kc in range(8):
                k.op("scalar", lambda e: e.activation(out=a[:, kc % 4, :], in_=mi[:, kc, :], func=AF.Square), reads=[mi], writes=[a])
                k.op("tensor", lambda e: e.matmul(P[:], lhsT=ones_f[:], rhs=a[:, kc % 4, :], start=(kc == 0), stop=(kc == 7)), reads=[ones_f, a], writes=[P])
            k.op("scalar", lambda e: e.activation(out=rstd[:], in_=P[:], func=AF.Sqrt, bias=eps6[:, 0:1], scale=1.0 / 1024), reads=[P, eps6], writes=[rstd])
            k.op("vector", lambda e: e.reciprocal(out=rstd[:], in_=rstd[:]), reads=[rstd], writes=[rstd])
            for kc in range(8):
                k.op("vector", lambda e: e.scalar_tensor_tensor(out=cat[:, 4 + kc, :], in0=mi[:, kc, :], scalar=gm[:, kc:kc + 1], in1=rstd[:], op0=ALU.mult, op1=ALU.mult),
                     reads=[mi, gm, rstd], writes=[cat])
            k.dma("gpsimd", cat[:, 12:16, :], gdnT[:, sl].rearrange("(c p) t -> p c t", p=128), writes=[cat])
            for tt in range(4):
                t0 = tb * 512 + tt * 128
                X = xt[tt % 2]; R = r[tt % 2]
                k.dma("sync", X[:], x[t0:t0 + 128, :], writes=[X])
                for nb in range(4):
                    P = ps[nb]
                    for kc in range(16):
                        k.op("tensor", lambda e: e.matmul(P[:], lhsT=cat[:, kc, tt * 128:(tt + 1) * 128], rhs=Wout[:, kc, nb * 512:(nb + 1) * 512],
                                                          start=(kc == 0), stop=(kc == 15)), reads=[cat, Wout], writes=[P])
                    k.op("vector", lambda e: e.scalar_tensor_tensor(out=R[:, nb * 512:(nb + 1) * 512], in0=X[:, nb * 512:(nb + 1) * 512], scalar=DN_ALPHA, in1=P[:],
                                                                    op0=ALU.mult, op1=ALU.add), reads=[X, P], writes=[R])
                ln_rows(k, L, R, X)
                k.dma("sync", out[t0:t0 + 128, :], X[:], reads=[X], writes=[od])
        k.finish([od])
    return nc


def mixout_inputs(x, s5scan, o_mla, y_gdn, p, l):
    ins = []
    xt = x.reshape(-1, D); s5 = s5scan.reshape(-1, 512); om = o_mla.reshape(-1, 1024); yg = y_gdn.reshape(-1, 512)
    for c in range(NCORES):
        ts = slice(c * TPC, (c + 1) * TPC)
        ins.append({"s5T": _c(s5[ts].T), "mlaT": _c(om[ts].T), "gdnT": _c(yg[ts].T), "x": _c(xt[ts]),
                    "wglu": _c(p["s5_w_glu"][l]), "bglu": _c(p["s5_b_glu"][l].reshape(4, 128).T), "gs5": _c(p["s5_out_norm"][l].reshape(4, 128).T),
                    "gmla": _c(p["mla_out_norm"][l].reshape(8, 128).T), "wout": _c(p["w_out"][l]),
                    "lng": _c(p["ln1_g"][l][None]), "lnb": _c(p["ln1_b"][l][None])})
    return ins


XA_SCALE = 128.0 ** -0.5


def build_xattn(stage=9):
    T = TPC
    nc = new_nc()
    di = lambda n, shp, dt=F32: nc.dram_tensor(n, shp, dt, kind="ExternalInput").ap()
    x1T = di("x1T", [D, T]); x1 = di("x1", [T, D]); memT = di("memT", [D, 256])
    wq = di("wq", [D, 512]); wk = di("wk", [D, 512]); wv = di("wv", [D, 512]); wo = di("wo", [512, D])
    lng = di("lng", [1, D]); lnb = di("lnb", [1, D])
    out = nc.dram_tensor("x2", [T, D], F32, kind="ExternalOutput").ap()
    with ExitStack() as st:
        k = MK(nc, st, same_engine_sync=(stage != 1.5))
        od = k.view("od", out)
        ps = [k.psum(f"ps{i}", [128, 512], F32) for i in range(8)]
        ones_f = k.sbuf("ones_f", [128, 128], F32); k.op("vector", lambda e: e.memset(ones_f[:], 1.0), writes=[ones_f])
        ones_b = k.sbuf("ones_b", [128, 128], BF16); k.op("vector", lambda e: e.memset(ones_b[:], 1.0), writes=[ones_b])
        XT = k.sbuf("XT", [128, 16, T], BF16)
        k.dma("gpsimd", XT[:], x1T.rearrange("(c p) t -> p c t", p=128), writes=[XT])
        L = ln_setup(k, lng, lnb)
        KT = k.sbuf("KT", [128, 4, 256], BF16); V = k.sbuf("V", [128, 2, 512], BF16)
        kmx = k.sbuf("kmx", [128, 4], F32)
        krow = [k.sbuf(f"krow{h}", [1, 128], BF16) for h in range(4)]
        arow = [k.sbuf(f"arow{h}", [1, 512], BF16) for h in range(4)]
        sq = k.sbuf("sq", [128, 512], F32)
        with ExitStack() as tst:
            Wk = k.sbuf("Wk", [128, 16, 512], BF16, stack=tst); k.dma("gpsimd", Wk[:], wk.rearrange("(c p) n -> p c n", p=128), writes=[Wk])
            Wv = k.sbuf("Wv", [128, 16, 512], BF16, stack=tst); k.dma("gpsimd", Wv[:], wv.rearrange("(c p) n -> p c n", p=128), writes=[Wv])
            MT = k.sbuf("MT", [128, 16, 256], BF16, stack=tst); k.dma("gpsimd", MT[:], memT.rearrange("(c p) t -> p c t", p=128), writes=[MT])
            if stage == 0:
                k.barrier(); k.dma("sync", out[0:128, 0:128], ones_f[:], reads=[ones_f], writes=[od]); k.finish([od]); return nc
            for hp2 in ((1,) if stage == 1.6 else (0, 0) if stage == 1.7 else range(2)):
                P = ps[hp2]
                for hh in range(2):
                    h = 2 * hp2 + hh
                    for kc in range(16):
                        k.op("tensor", lambda e: e.matmul(P[:, hh * 256:(hh + 1) * 256], lhsT=Wk[:, kc, h * 128:(h + 1) * 128], rhs=MT[:, kc, :],
                                                          start=(kc == 0), stop=(kc == 15)), reads=[Wk, MT], writes=[P])
                k.op("vector", lambda e: e.tensor_copy(out=KT[:, 2 * hp2:2 * hp2 + 2, :], in_=P[:].rearrange("p (a b) -> p a b", a=2)), reads=[P], writes=[KT])
                if stage == 0.55:
                    k.barrier(); k.dma("sync", out[0:128, 0:128], ones_f[:], reads=[ones_f], writes=[od]); k.finish([od]); return nc
                KTv = KT[:, 2 * hp2:2 * hp2 + 2, :].rearrange("p a b -> p (a b)")
                k.op("vector", lambda e: e.tensor_tensor(out=sq[:], in0=KTv, in1=KTv, op=ALU.mult), reads=[KT], writes=[sq])
                if stage == 0.6:
                    k.barrier(); k.dma("sync", out[0:128, 0:128], ones_f[:], reads=[ones_f], writes=[od]); k.finish([od]); return nc
                P2 = ps[4]
                k.op("tensor", lambda e: e.matmul(P2[:], lhsT=ones_f[:], rhs=sq[:], start=True, stop=True), reads=[ones_f, sq], writes=[P2])
                if stage == 0.7:
                    k.barrier(); k.dma("sync", out[0:128, 0:128], ones_f[:], reads=[ones_f], writes=[od]); k.finish([od]); return nc
                for hh in range(2):
                    h = 2 * hp2 + hh
                    k.op("vector", lambda e: e.tensor_reduce(out=kmx[:, h:h + 1], in_=P2[:, hh * 256:(hh + 1) * 256], axis=AX.X, op=ALU.max), reads=[P2], writes=[kmx])
                    if stage == 0.8:
                        k.barrier(); k.dma("sync", out[0:128, 0:128], ones_f[:], reads=[ones_f], writes=[od]); k.finish([od]); return nc
                if stage == 0.9:
                    k.barrier(); k.dma("sync", out[0:128, 0:128], ones_f[:], reads=[ones_f], writes=[od]); k.finish([od]); return nc
            if stage in (1, 1.5, 1.6, 1.7):
                k.barrier(); k.dma("sync", out[0:128, 0:128], ones_f[:], reads=[ones_f], writes=[od]); k.finish([od]); return nc
            for mt in range(2):
                P = ps[5 + mt]
                for kc in range(16):
                    k.op("tensor", lambda e: e.matmul(P[:], lhsT=MT[:, kc, mt * 128:(mt + 1) * 128], rhs=Wv[:, kc, :], start=(kc == 0), stop=(kc == 15)),
                         reads=[Wv, MT], writes=[P])
                k.op("vector", lambda e: e.tensor_copy(out=V[:, mt, :], in_=P[:]), reads=[P], writes=[V])
            k.barrier()
        if stage == 2:
            k.barrier(); k.dma("sync", out[0:128, 0:128], ones_f[:], reads=[ones_f], writes=[od]); k.finish([od]); return nc
        k.op("scalar", lambda e: e.activation(out=kmx[:], in_=kmx[:], func=AF.Sqrt), reads=[kmx], writes=[kmx])
        k.op("vector", lambda e: e.tensor_scalar(out=kmx[:], in0=kmx[:], scalar1=-1.0, scalar2=None, op0=ALU.mult), reads=[kmx], writes=[kmx])
        for h in range(4):
            k.op("vector", lambda e: e.memset(krow[h][:], 1.0), writes=[krow[h]])
            k.op("vector", lambda e: e.tensor_scalar(out=krow[h][:], in0=krow[h][:], scalar1=kmx[0:1, h:h + 1], scalar2=None, op0=ALU.mult), reads=[krow[h], kmx], writes=[krow[h]])
        if stage == 3:
            k.barrier(); k.dma("sync", out[0:128, 0:128], ones_f[:], reads=[ones_f], writes=[od]); k.finish([od]); return nc
        Wq = k.sbuf("Wq", [128, 16, 512], BF16); k.dma("gpsimd", Wq[:], wq.rearrange("(c p) n -> p c n", p=128), writes=[Wq])
        Wo = k.sbuf("Wo", [128, 4, D], BF16); k.dma("gpsimd", Wo[:], wo.rearrange("(c p) n -> p c n", p=128), writes=[Wo])
        QT = k.sbuf("QT", [128, 4, 512], BF16)
        PT = [k.sbuf(f"PT{i}", [128, 512], BF16) for i in range(2)]
        OTn = k.sbuf("OTn", [128, 4, 512], BF16)
        rden = k.sbuf("rden", [128, 512], F32)
        X = k.sbuf("Xt", [128, D], F32); R = k.sbuf("Rt", [128, D], F32)
        for tb in range(T // 512):
            sl = slice(tb * 512, (tb + 1) * 512)
            for h in range(4):
                P = ps[h % 2]
                for kc in range(16):
                    k.op("tensor", lambda e: e.matmul(P[:], lhsT=Wq[:, kc, h * 128:(h + 1) * 128], rhs=XT[:, kc, sl], start=(kc == 0), stop=(kc == 15)),
                         reads=[Wq, XT], writes=[P])
                k.op("scalar", lambda e: e.activation(out=QT[:, h, :], in_=P[:], func=AF.Copy, scale=XA_SCALE), reads=[P], writes=[QT])
                k.op("scalar", lambda e: e.activation(out=sq[:], in_=P[:], func=AF.Square, scale=XA_SCALE), reads=[P], writes=[sq])
                P2 = ps[2]
                k.op("tensor", lambda e: e.matmul(P2[:], lhsT=ones_f[:], rhs=sq[:], start=True, stop=True), reads=[ones_f, sq], writes=[P2])
                k.op("scalar", lambda e: e.activation(out=arow[h][:], in_=P2[0:1, :], func=AF.Sqrt), reads=[P2], writes=[arow[h]])
            if stage == 4:
                k.barrier(); k.dma("sync", out[0:128, 0:128], ones_f[:], reads=[ones_f], writes=[od]); k.finish([od]); return nc
            for h in range(4):
                for mt in range(2):
                    SP = ps[3 + mt]
                    k.op("tensor", lambda e: e.matmul(SP[:], lhsT=KT[:, h, mt * 128:(mt + 1) * 128], rhs=QT[:, h, :], start=True, stop=False), reads=[KT, QT], writes=[SP])
                    k.op("tensor", lambda e: e.matmul(SP[:], lhsT=krow[h][0:1, :], rhs=arow[h][0:1, :], start=False, stop=True), reads=[krow[h], arow[h]], writes=[SP])
                    k.op("scalar", lambda e: e.activation(out=PT[mt][:], in_=SP[:], func=AF.Exp), reads=[SP], writes=[PT[mt]])
                PO, PDn = ps[5], ps[6]
                for mt in range(2):
                    k.op("tensor", lambda e: e.matmul(PO[:], lhsT=V[:, mt, h * 128:(h + 1) * 128], rhs=PT[mt][:], start=(mt == 0), stop=(mt == 1)), reads=[V, PT[mt]], writes=[PO])
                for mt in range(2):
                    k.op("tensor", lambda e: e.matmul(PDn[:], lhsT=ones_b[:], rhs=PT[mt][:], start=(mt == 0), stop=(mt == 1)), reads=[ones_b, PT[mt]], writes=[PDn])
                k.op("vector", lambda e: e.reciprocal(out=rden[:], in_=PDn[:]), reads=[PDn], writes=[rden])
                k.op("vector", lambda e: e.tensor_tensor(out=OTn[:, h, :], in0=PO[:], in1=rden[:], op=ALU.mult), reads=[PO, rden], writes=[OTn])
            if stage == 5:
                k.barrier(); k.dma("sync", out[0:128, 0:128], ones_f[:], reads=[ones_f], writes=[od]); k.finish([od]); return nc
            for tt in range(4):
                t0 = tb * 512 + tt * 128
                k.dma("sync", X[:], x1[t0:t0 + 128, :], writes=[X])
                for nb in range(4):
                    P = ps[nb % 2]
                    for h in range(4):
                        k.op("tensor", lambda e: e.matmul(P[:], lhsT=OTn[:, h, tt * 128:(tt + 1) * 128], rhs=Wo[:, h, nb * 512:(nb + 1) * 512], start=(h == 0), stop=(h == 3)),
                             reads=[OTn, Wo], writes=[P])
                    k.op("vector", lambda e: e.scalar_tensor_tensor(out=R[:, nb * 512:(nb + 1) * 512], in0=X[:, nb * 512:(nb + 1) * 512], scalar=DN_ALPHA, in1=P[:],
                                                                    op0=ALU.mult, op1=ALU.add), reads=[X, P], writes=[R])
                ln_rows(k, L, R, X)
                k.dma("sync", out[t0:t0 + 128, :], X[:], reads=[X], writes=[od])
        k.finish([od])
    return nc


def xattn_inputs(x1, p, l):
    ins = []
    xt = x1.reshape(-1, D)
    for c in range(NCORES):
        b = c // 2
        ts = slice(c * TPC, (c + 1) * TPC)
        ins.append({"x1T": _c(xt[ts].T), "x1": _c(xt[ts]), "memT": _c(p["mem"][b].T),
                    "wq": _c(p["xa_w_q"][l]), "wk": _c(p["xa_w_k"][l]), "wv": _c(p["xa_w_v"][l]), "wo": _c(p["xa_w_o"][l]),
                    "lng": _c(p["ln2_g"][l][None]), "lnb": _c(p["ln2_b"][l][None])})
    return ins


def build_moe():
    T = TPC
    HT = T // 2
    nc = new_nc()
    di = lambda n, shp, dt=F32: nc.dram_tensor(n, shp, dt, kind="ExternalInput").ap()
    x2T = di("x2T", [D, T]); x2 = di("x2", [T, D]); wr = di("wr", [D, 36]); br = di("br", [1, 36])
    wgu = di("wgu", [32, D, 1024]); wd = di("wd", [32, 512, D])
    lng = di("lng", [1, D]); lnb = di("lnb", [1, D])
    out = nc.dram_tensor("x3", [T, D], F32, kind="ExternalOutput").ap()
    with ExitStack() as st:
        k = MK(nc, st)
        od = k.view("od", out)
        ps = [k.psum(f"ps{i}", [128, 512], F32) for i in range(8)]
        L = ln_setup(k, lng, lnb)
        XTb = k.sbuf("XTb", [128, 16, HT], BF16)
        yacc = k.sbuf("yacc", [128, HT // 128, D], F32)
        gates = k.sbuf("gates", [128, HT // 128, 32], F32)
        for half in range(2):
            h0 = half * HT
            k.dma("gpsimd", XTb[:], x2T[:, h0:h0 + HT].rearrange("(c p) t -> p c t", p=128), writes=[XTb])
            with ExitStack() as tst:
                S_ = lambda n, shp, dt=F32: k.sbuf(f"{n}_{half}", shp, dt, stack=tst)
                Wr = S_("Wr", [128, 16, 36]); k.dma("sync", Wr[:], wr.rearrange("(c p) n -> p c n", p=128), writes=[Wr])
                brB = S_("brB", [128, 36]); k.dma("sync", brB[:], br.to_broadcast([128, 36]), writes=[brB])
                xr = S_("xr", [128, 16, 128])
                lg = S_("lg", [128, 36]); m4 = S_("m4", [128, 1]); nm4 = S_("nm4", [128, 1]); oh4 = S_("oh4", [128, 4]); e4 = S_("e4", [128, 4])
                s4 = S_("s4", [128, 1]); pg = S_("pg", [128, 1]); sel = S_("sel", [128, 8]); sel2 = S_("sel2", [128, 8])
                m1 = S_("m1", [128, 1]); m2 = S_("m2", [128, 1]); oh1 = S_("oh1", [128, 8]); oh2 = S_("oh2", [128, 8])
                g1 = S_("g1", [128, 1]); g2 = S_("g2", [128, 1]); gate8 = S_("gate8", [128, 8])
                for tt in range(HT // 128):
                    t0 = h0 + tt * 128
                    k.dma("sync", xr[:], x2T[:, t0:t0 + 128].rearrange("(c p) t -> p c t", p=128), writes=[xr])
                    P = ps[tt % 2]
                    for kc in range(16):
                        k.op("tensor", lambda e: e.matmul(P[:, 0:36], lhsT=xr[:, kc, :], rhs=Wr[:, kc, :], start=(kc == 0), stop=(kc == 15)), reads=[xr, Wr], writes=[P])
                    k.op("vector", lambda e: e.tensor_tensor(out=lg[:], in0=P[:, 0:36], in1=brB[:], op=ALU.add), reads=[P, brB], writes=[lg])
                    k.op("vector", lambda e: e.tensor_reduce(out=m4[:], in_=lg[:, 0:4], axis=AX.X, op=ALU.max), reads=[lg], writes=[m4])
                    k.op("vector", lambda e: e.tensor_scalar(out=oh4[:], in0=lg[:, 0:4], scalar1=m4[:, 0:1], scalar2=None, op0=ALU.is_equal), reads=[lg, m4], writes=[oh4])
                    k.op("vector", lambda e: e.tensor_scalar(out=nm4[:], in0=m4[:], scalar1=-1.0, scalar2=None, op0=ALU.mult), reads=[m4], writes=[nm4])
                    k.op("scalar", lambda e: e.activation(out=e4[:], in_=lg[:, 0:4], func=AF.Exp, bias=nm4[:, 0:1], scale=1.0, accum_out=s4[:, 0:1]), reads=[lg, nm4], writes=[e4, s4])
                    k.op("vector", lambda e: e.reciprocal(out=pg[:], in_=s4[:]), reads=[s4], writes=[pg])
                    k.op("vector", lambda e: e.tensor_scalar(out=sel[:], in0=lg[:, 4:12], scalar1=oh4[:, 0:1], scalar2=None, op0=ALU.mult), reads=[lg, oh4], writes=[sel])
                    for g in range(1, 4):
                        k.op("vector", lambda e: e.scalar_tensor_tensor(out=sel[:], in0=lg[:, 4 + 8 * g:12 + 8 * g], scalar=oh4[:, g:g + 1], in1=sel[:], op0=ALU.mult, op1=ALU.add),
                             reads=[lg, oh4, sel], writes=[sel])
                    k.op("vector", lambda e: e.tensor_reduce(out=m1[:], in_=sel[:], axis=AX.X, op=ALU.max), reads=[sel], writes=[m1])
                    k.op("vector", lambda e: e.tensor_scalar(out=oh1[:], in0=sel[:], scalar1=m1[:, 0:1], scalar2=None, op0=ALU.is_equal), reads=[sel, m1], writes=[oh1])
                    k.op("vector", lambda e: e.scalar_tensor_tensor(out=sel2[:], in0=oh1[:], scalar=-1e30, in1=sel[:], op0=ALU.mult, op1=ALU.add), reads=[oh1, sel], writes=[sel2])
                    k.op("vector", lambda e: e.tensor_reduce(out=m2[:], in_=sel2[:], axis=AX.X, op=ALU.max), reads=[sel2], writes=[m2])
                    k.op("vector", lambda e: e.tensor_scalar(out=oh2[:], in0=sel2[:], scalar1=m2[:, 0:1], scalar2=None, op0=ALU.is_equal), reads=[sel2, m2], writes=[oh2])
                    k.op("vector", lambda e: e.tensor_tensor(out=g1[:], in0=m1[:], in1=m2[:], op=ALU.subtract), reads=[m1, m2], writes=[g1])
                    k.op("scalar", lambda e: e.activation(out=g1[:], in_=g1[:], func=AF.Sigmoid), reads=[g1], writes=[g1])
                    k.op("vector", lambda e: e.tensor_tensor(out=g1[:], in0=g1[:], in1=pg[:], op=ALU.mult), reads=[g1, pg], writes=[g1])
                    k.op("vector", lambda e: e.tensor_tensor(out=g2[:], in0=pg[:], in1=g1[:], op=ALU.subtract), reads=[g1, pg], writes=[g2])
                    k.op("vector", lambda e: e.tensor_scalar(out=gate8[:], in0=oh1[:], scalar1=g1[:, 0:1], scalar2=None, op0=ALU.mult), reads=[oh1, g1], writes=[gate8])
                    k.op("vector", lambda e: e.scalar_tensor_tensor(out=gate8[:], in0=oh2[:], scalar=g2[:, 0:1], in1=gate8[:], op0=ALU.mult, op1=ALU.add), reads=[oh2, g2, gate8], writes=[gate8])
                    for g in range(4):
                        k.op("vector", lambda e: e.tensor_scalar(out=gates[:, tt, g * 8:(g + 1) * 8], in0=gate8[:], scalar1=oh4[:, g:g + 1], scalar2=None, op0=ALU.mult),
                             reads=[gate8, oh4], writes=[gates])
                k.barrier()
            with ExitStack() as est:
                G2 = [k.sbuf(f"G{half}_{i}", [128, 16, 256], BF16, stack=est) for i in range(2)]
                U2 = [k.sbuf(f"U{half}_{i}", [128, 16, 256], BF16, stack=est) for i in range(2)]
                W2 = [k.sbuf(f"Wd{half}_{i}", [128, 4, 1024], BF16, stack=est) for i in range(2)]
                hT = k.sbuf(f"hT{half}", [128, 4, HT], BF16, stack=est)
                sg = [k.sbuf(f"sg{i}_{half}", [128, 512], F32, stack=est) for i in range(2)]
                for ex in range(32):
                    for fp in range(2):
                        k.dma("gpsimd", G2[fp][:], wgu[ex, :, fp * 256:(fp + 1) * 256].rearrange("(c p) n -> p c n", p=128), writes=[G2[fp]])
                        k.dma("gpsimd", U2[fp][:], wgu[ex, :, 512 + fp * 256:512 + (fp + 1) * 256].rearrange("(c p) n -> p c n", p=128), writes=[U2[fp]])
                    for ch in range(2):
                        k.dma("gpsimd", W2[ch][:], wd[ex, :, ch * 1024:(ch + 1) * 1024].rearrange("(c p) n -> p c n", p=128), writes=[W2[ch]])
                    for fp in range(2):
                        for blk in range(HT // 512):
                            bs = slice(blk * 512, (blk + 1) * 512)
                            for fcl in range(2):
                                fc = fp * 2 + fcl
                                Pg, Pu = ps[(2 * fcl) % 4], ps[(2 * fcl + 1) % 4]
                                for kc in range(16):
                                    k.op("tensor", lambda e: e.matmul(Pg[:], lhsT=G2[fp][:, kc, fcl * 128:(fcl + 1) * 128], rhs=XTb[:, kc, bs], start=(kc == 0), stop=(kc == 15)),
                                         reads=[G2[fp], XTb], writes=[Pg])
                                for kc in range(16):
                                    k.op("tensor", lambda e: e.matmul(Pu[:], lhsT=U2[fp][:, kc, fcl * 128:(fcl + 1) * 128], rhs=XTb[:, kc, bs], start=(kc == 0), stop=(kc == 15)),
                                         reads=[U2[fp], XTb], writes=[Pu])
                                SG = sg[fcl]
                                k.op("scalar", lambda e: e.activation(out=SG[:], in_=Pg[:], func=AF.Silu, scale=1.0), reads=[Pg], writes=[SG])
                                k.op("vector", lambda e: e.tensor_tensor(out=hT[:, fc, bs], in0=SG[:], in1=Pu[:], op=ALU.mult), reads=[SG, Pu], writes=[hT])
                    for ch in range(2):
                        for tile_i in range(HT // 128):
                            for nbl in range(2):
                                nb = ch * 2 + nbl
                                Py = ps[4 + (tile_i * 2 + nbl) % 4]
                                for fc in range(4):
                                    k.op("tensor", lambda e: e.matmul(Py[:], lhsT=hT[:, fc, tile_i * 128:(tile_i + 1) * 128], rhs=W2[ch][:, fc, nbl * 512:(nbl + 1) * 512],
                                                                      start=(fc == 0), stop=(fc == 3)), reads=[hT, W2[ch]], writes=[Py])
                                ya = yacc[:, tile_i, nb * 512:(nb + 1) * 512]
                                if ex == 0:
                                    k.op("vector", lambda e: e.tensor_scalar(out=ya, in0=Py[:], scalar1=gates[:, tile_i, ex:ex + 1], scalar2=None, op0=ALU.mult),
                                         reads=[Py, gates], writes=[yacc])
                                else:
                                    k.op("vector", lambda e: e.scalar_tensor_tensor(out=ya, in0=Py[:], scalar=gates[:, tile_i, ex:ex + 1], in1=ya, op0=ALU.mult, op1=ALU.add),
                                         reads=[Py, gates, yacc], writes=[yacc])
                X = k.sbuf(f"Xt{half}", [128, D], F32, stack=est)
                for tt in range(HT // 128):
                    t0 = h0 + tt * 128
                    k.dma("sync", X[:], x2[t0:t0 + 128, :], writes=[X])
                    k.op("vector", lambda e: e.scalar_tensor_tensor(out=yacc[:, tt, :], in0=X[:], scalar=DN_ALPHA, in1=yacc[:, tt, :], op0=ALU.mult, op1=ALU.add),
                         reads=[X, yacc], writes=[yacc])
                    Rv = k.view("Rv", yacc[:, tt, :])
                    Rv.w = yacc.w; Rv.r = yacc.r
                    ln_rows(k, L, Rv, X)
                    yacc.r.update(Rv.r)
                    k.dma("sync", out[t0:t0 + 128, :], X[:], reads=[X], writes=[od])
                k.barrier()
        k.finish([od])
    return nc


def moe_inputs(x2, p, l):
    ins = []
    xt = x2.reshape(-1, D)
    wr = _c(np.concatenate([p["moe_w_group"][l], p["moe_w_expert"][l]], 1))
    br = _c(np.concatenate([p["moe_b_group"][l], p["moe_b_expert"][l]])[None])
    for c in range(NCORES):
        ts = slice(c * TPC, (c + 1) * TPC)
        ins.append({"x2T": _c(xt[ts].T), "x2": _c(xt[ts]), "wr": wr, "br": br,
                    "wgu": _c(p["moe_w_gate_up"][l]), "wd": _c(p["moe_w_down"][l]),
                    "lng": _c(p["ln3_g"][l][None]), "lnb": _c(p["ln3_b"][l][None])})
    return ins


_NC_CACHE = {}


def _nc(name, builder):
    if name not in _NC_CACHE:
        _NC_CACHE[name] = builder()
    return _NC_CACHE[name]


def kernel(**inputs):
    p = {k_: np.asarray(v) for k_, v in inputs.items()}
    x = np.ascontiguousarray(p["x"], dtype=np.float32)
    for l in range(2):
        xt = x.reshape(-1, D)
        res = run(_nc("gemm", lambda: build_gemm(TPC, D, IN_COLS)),
                  [{"xT": _c(xt[c * TPC:(c + 1) * TPC].T), "w": _c(p["w_in"][l])} for c in range(NCORES)])
        proj = np.concatenate([r["out"] for r in res], 0).reshape(BATCH, SEQ, IN_COLS)
        s5scan = s5_gather(run(_nc("s5", build_s5), s5_inputs(proj, p, l)))
        o_mla = np.zeros((BATCH, SEQ, 1024), np.float32)
        for part in range(2):
            mla_gather(o_mla, run(_nc("mla", build_mla), mla_inputs(proj, p, l, part)), part)
        y_gdn = gdn_gather(run(_nc("gdn", build_gdn), gdn_inputs(proj, p, l)))
        del proj
        res = run(_nc("mixout", build_mixout), mixout_inputs(x, s5scan, o_mla, y_gdn, p, l))
        x1 = np.concatenate([r["x1"] for r in res], 0).reshape(BATCH, SEQ, D)
        res = run(_nc("xattn", build_xattn), xattn_inputs(x1, p, l))
        x2 = np.concatenate([r["x2"] for r in res], 0).reshape(BATCH, SEQ, D)
        res = run(_nc("moe", build_moe), moe_inputs(x2, p, l))
        x = np.concatenate([r["x3"] for r in res], 0).reshape(BATCH, SEQ, D)
    return x.astype(np.float32)
```
